# Optimizing a Trainium2 kernel written in Bass

```python
import math
import jax
import jax.numpy as jnp
from jax import lax
import numpy as np

D_MODEL = 4096
BATCH = 4
SEQ = 2048
DEPTH = 4

N_MIXERS = 4
N_HEADS = 32
HEAD_DIM = 128
ATT_WIDTH = N_HEADS * HEAD_DIM
Q_LORA = 1024
KV_LORA = 512
IDX_HEADS = 64
IDX_DIM = 128
TOPK_MAX = 256
Q_BLOCK = 128
ATT_IN = Q_LORA + KV_LORA + IDX_DIM + IDX_HEADS
REL_BUCKETS = 32
REL_MAX_DIST = 128
SCONV_WIDTH = 3
CONF_WIDTH = 31
LRU_WIDTH = D_MODEL
LRU_HEADS = 16
LRU_BLOCK = LRU_WIDTH // LRU_HEADS
LRU_CONV_WIDTH = 4
LRU_C = 8.0
D_FF = 2 * D_MODEL
N_EXPERTS = 8
MOE_TOPK = 2
D_FF_EXPERT = D_MODEL // 2
N_ADA = 6
NORM_EPS = 1e-6

N_ATT = (DEPTH + 3) // 4
N_SCONV = (DEPTH + 2) // 4
N_CONF = (DEPTH + 1) // 4
N_LRU = DEPTH // 4
N_DENSE = (DEPTH + 1) // 2
N_MOE = DEPTH // 2

kernel_name = "hybrid_dsa_conv_conformer_rglru_moe_trunk"


def rms_norm(x, g):
    xf = x.astype(jnp.float32)
    y = xf * lax.rsqrt(jnp.mean(xf * xf, axis=-1, keepdims=True) + NORM_EPS)
    return (y * g.astype(jnp.float32)).astype(x.dtype)


def layer_norm(x, g, b):
    xf = x.astype(jnp.float32)
    mu = jnp.mean(xf, axis=-1, keepdims=True)
    var = jnp.mean(jnp.square(xf - mu), axis=-1, keepdims=True)
    y = (xf - mu) * lax.rsqrt(var + NORM_EPS)
    return (y * g.astype(jnp.float32) + b.astype(jnp.float32)).astype(x.dtype)


def causal_depthwise_conv(x, conv_w):
    width, ch = conv_w.shape
    return lax.conv_general_dilated(
        x, conv_w[:, None, :].astype(x.dtype), window_strides=(1,),
        padding=[(width - 1, 0)], dimension_numbers=("NWC", "WIO", "NWC"),
        feature_group_count=ch)


def causal_rel_bucket(dist):
    dist = jnp.maximum(dist, 0)
    max_exact = REL_BUCKETS // 2
    large = max_exact + (jnp.log(jnp.maximum(dist, 1).astype(jnp.float32) / max_exact)
                         / math.log(REL_MAX_DIST / max_exact)
                         * (REL_BUCKETS - max_exact)).astype(jnp.int32)
    large = jnp.minimum(large, REL_BUCKETS - 1)
    return jnp.where(dist < max_exact, dist, large)


def dsa_attention(h, rel_bias, w_in, g_cq, g_ckv, w_uq, w_uk, w_uv, w_qidx, w_out):
    bsz, seq, _ = h.shape
    k_sel = min(TOPK_MAX, seq // 4)
    n_blk = seq // Q_BLOCK
    proj = h @ w_in
    c_q, c_kv, k_idx, w_head = jnp.split(
        proj, [Q_LORA, Q_LORA + KV_LORA, Q_LORA + KV_LORA + IDX_DIM], axis=-1)
    c_q = rms_norm(c_q, g_cq)
    c_kv = rms_norm(c_kv, g_ckv)
    w_head = w_head * (IDX_HEADS ** -0.5)
    key_pos = jnp.arange(seq, dtype=jnp.int32)
    q_pos = key_pos.reshape(n_blk, Q_BLOCK)
    rel_bias_f = rel_bias.astype(jnp.float32)

    def to_blocks(t):
        return t.reshape(bsz, n_blk, Q_BLOCK, t.shape[-1]).swapaxes(0, 1)

    def one_block(args):
        cq_b, wh_b, qp = args
        q_idx = (cq_b @ w_qidx).reshape(bsz, Q_BLOCK, IDX_HEADS, IDX_DIM)
        dots = jnp.einsum("bqhd,bsd->bqhs", q_idx, k_idx) * (IDX_DIM ** -0.5)
        score = jnp.einsum("bqh,bqhs->bqs", wh_b.astype(jnp.float32),
                           jax.nn.relu(dots).astype(jnp.float32))
        causal = key_pos[None, :] <= qp[:, None]
        score = jnp.where(causal[None], score, -jnp.inf)
        _, idx = lax.top_k(score, k_sel)
        valid = idx <= qp[None, :, None]
        kv_sel = jax.vmap(lambda c, i: c[i])(c_kv, idx)
        q = (cq_b @ w_uq).reshape(bsz, Q_BLOCK, N_HEADS, HEAD_DIM)
        q_lat = jnp.einsum("bqhd,chd->bqhc", q, w_uk)
        logits = jnp.einsum("bqhc,bqkc->bqhk", q_lat, kv_sel).astype(jnp.float32) * (HEAD_DIM ** -0.5)
        bucket = causal_rel_bucket(qp[None, :, None] - idx)
        logits = logits + jnp.transpose(rel_bias_f[bucket], (0, 1, 3, 2))
        logits = jnp.where(valid[:, :, None, :], logits, -jnp.inf)
        p = jax.nn.softmax(logits, axis=-1).astype(h.dtype)
        o_lat = jnp.einsum("bqhk,bqkc->bqhc", p, kv_sel)
        o = jnp.einsum("bqhc,chd->bqhd", o_lat, w_uv)
        return o.reshape(bsz, Q_BLOCK, ATT_WIDTH)

    out = lax.map(one_block, (to_blocks(c_q), to_blocks(w_head), q_pos))
    out = out.swapaxes(0, 1).reshape(bsz, seq, ATT_WIDTH)
    return out @ w_out


def short_conv_mixer(h, w_in, conv_w, w_out):
    gate_b, gate_c, xin = jnp.split(h @ w_in, 3, axis=-1)
    y = gate_b * causal_depthwise_conv(gate_c * xin, conv_w)
    return y @ w_out


def conformer_conv_mixer(h, w_in, conv_w, conv_b, ln_g, ln_b, w_out):
    a, g = jnp.split(h @ w_in, 2, axis=-1)
    u = a * jax.nn.sigmoid(g)
    u = causal_depthwise_conv(u, conv_w) + conv_b
    u = layer_norm(u, ln_g, ln_b)
    return jax.nn.silu(u) @ w_out


def rglru_mixer(h, w_in, conv_w, conv_b, w_a, b_a, w_x, b_x, lam, w_out):
    bsz, seq, _ = h.shape
    gate_br, x_br = jnp.split(h @ w_in, 2, axis=-1)
    gate_br = jax.nn.gelu(gate_br)
    xc = causal_depthwise_conv(x_br, conv_w) + conv_b
    xb = xc.reshape(bsz, seq, LRU_HEADS, LRU_BLOCK)
    r = jax.nn.sigmoid(jnp.einsum("bshi,hij->bshj", xb, w_a).reshape(bsz, seq, LRU_WIDTH) + b_a)
    i_g = jax.nn.sigmoid(jnp.einsum("bshi,hij->bshj", xb, w_x).reshape(bsz, seq, LRU_WIDTH) + b_x)
    log_a = -LRU_C * r.astype(jnp.float32) * jax.nn.softplus(-lam.astype(jnp.float32))
    a = jnp.exp(log_a)
    u = jnp.sqrt(-jnp.expm1(2.0 * log_a)) * (i_g * xc).astype(jnp.float32)

    def combine(left, right):
        a1, b1 = left
        a2, b2 = right
        return a1 * a2, a2 * b1 + b2

    _, hs = lax.associative_scan(combine, (a, u), axis=1)
    return (hs.astype(h.dtype) * gate_br) @ w_out


def swiglu(t, w13, w2):
    g, u = jnp.split(t @ w13, 2, axis=-1)
    return (jax.nn.silu(g) * u) @ w2


def moe_swiglu(h, router, w13, w2):
    d = h.shape[-1]
    t = h.reshape(-1, d)
    logits = (t @ router).astype(jnp.float32)
    top_v, top_i = lax.top_k(logits, MOE_TOPK)
    top_w = jax.nn.softmax(top_v, axis=-1)
    gates = jnp.sum(jax.nn.one_hot(top_i, N_EXPERTS, dtype=jnp.float32) * top_w[..., None],
                    axis=1).astype(t.dtype)
    out = jnp.zeros_like(t)
    for e in range(N_EXPERTS):
        out = out + gates[:, e:e + 1] * swiglu(t, w13[e], w2[e])
    return out.reshape(h.shape)


def modulate(h, shift, scale):
    return h * (1.0 + scale) + shift


def setup_inputs(seed: int = 0) -> dict:
    key = jax.random.key(seed)
    ks = iter(jax.random.split(key, 64))
    f32 = jnp.float32

    def nrm(shape, fan_in, scale=1.0):
        return jax.random.normal(next(ks), shape, f32) * (scale * fan_in ** -0.5)

    def gain(shape):
        return 1.0 + 0.02 * jax.random.normal(next(ks), shape, f32)

    def small(shape, s=0.02):
        return s * jax.random.normal(next(ks), shape, f32)

    x = jax.random.normal(next(ks), (BATCH, SEQ, D_MODEL), f32)
    c = jax.random.normal(next(ks), (BATCH, D_MODEL), f32)
    ada_w = nrm((D_MODEL, N_ADA * D_MODEL), D_MODEL, 0.5)
    ada_b = small((N_ADA * D_MODEL,))
    ada_table = small((DEPTH, N_ADA, D_MODEL), 0.1)
    norm_mix = gain((DEPTH, D_MODEL))
    norm_ffn = gain((DEPTH, D_MODEL))
    norm_final = gain((D_MODEL,))
    rel_bias = small((REL_BUCKETS, N_HEADS), 0.5)

    att_w_in = nrm((N_ATT, D_MODEL, ATT_IN), D_MODEL)
    att_g_cq = gain((N_ATT, Q_LORA))
    att_g_ckv = gain((N_ATT, KV_LORA))
    att_w_uq = nrm((N_ATT, Q_LORA, ATT_WIDTH), Q_LORA)
    att_w_uk = nrm((N_ATT, KV_LORA, N_HEADS, HEAD_DIM), KV_LORA)
    att_w_uv = nrm((N_ATT, KV_LORA, N_HEADS, HEAD_DIM), KV_LORA)
    att_w_qidx = nrm((N_ATT, Q_LORA, IDX_HEADS * IDX_DIM), Q_LORA)
    att_w_out = nrm((N_ATT, ATT_WIDTH, D_MODEL), ATT_WIDTH)

    sconv_w_in = nrm((N_SCONV, D_MODEL, 3 * D_MODEL), D_MODEL)
    sconv_conv_w = nrm((N_SCONV, SCONV_WIDTH, D_MODEL), SCONV_WIDTH)
    sconv_w_out = nrm((N_SCONV, D_MODEL, D_MODEL), D_MODEL)

    conf_w_in = nrm((N_CONF, D_MODEL, 2 * D_MODEL), D_MODEL)
    conf_conv_w = nrm((N_CONF, CONF_WIDTH, D_MODEL), CONF_WIDTH)
    conf_conv_b = small((N_CONF, D_MODEL))
    conf_ln_g = gain((N_CONF, D_MODEL))
    conf_ln_b = small((N_CONF, D_MODEL))
    conf_w_out = nrm((N_CONF, D_MODEL, D_MODEL), D_MODEL)

    lru_w_in = nrm((N_LRU, D_MODEL, 2 * LRU_WIDTH), D_MODEL)
    lru_conv_w = nrm((N_LRU, LRU_CONV_WIDTH, LRU_WIDTH), LRU_CONV_WIDTH)
    lru_conv_b = small((N_LRU, LRU_WIDTH))
    lru_w_a = nrm((N_LRU, LRU_HEADS, LRU_BLOCK, LRU_BLOCK), LRU_BLOCK)
    lru_b_a = small((N_LRU, LRU_WIDTH))
    lru_w_x = nrm((N_LRU, LRU_HEADS, LRU_BLOCK, LRU_BLOCK), LRU_BLOCK)
    lru_b_x = small((N_LRU, LRU_WIDTH))
    a_pow_c = jax.random.uniform(next(ks), (N_LRU, LRU_WIDTH), f32, minval=0.9, maxval=0.999)
    s = a_pow_c ** (1.0 / LRU_C)
    lru_lambda = jnp.log(s) - jnp.log1p(-s)
    lru_w_out = nrm((N_LRU, LRU_WIDTH, D_MODEL), LRU_WIDTH)

    ffn_w13 = nrm((N_DENSE, D_MODEL, 2 * D_FF), D_MODEL)
    ffn_w2 = nrm((N_DENSE, D_FF, D_MODEL), D_FF)
    moe_router = nrm((N_MOE, D_MODEL, N_EXPERTS), D_MODEL)
    moe_w13 = nrm((N_MOE, N_EXPERTS, D_MODEL, 2 * D_FF_EXPERT), D_MODEL)
    moe_w2 = nrm((N_MOE, N_EXPERTS, D_FF_EXPERT, D_MODEL), D_FF_EXPERT)

    return {
        "x": x, "c": c, "ada_w": ada_w, "ada_b": ada_b, "ada_table": ada_table,
        "norm_mix": norm_mix, "norm_ffn": norm_ffn, "norm_final": norm_final, "rel_bias": rel_bias,
        "att_w_in": att_w_in, "att_g_cq": att_g_cq, "att_g_ckv": att_g_ckv, "att_w_uq": att_w_uq,
        "att_w_uk": att_w_uk, "att_w_uv": att_w_uv, "att_w_qidx": att_w_qidx, "att_w_out": att_w_out,
        "sconv_w_in": sconv_w_in, "sconv_conv_w": sconv_conv_w, "sconv_w_out": sconv_w_out,
        "conf_w_in": conf_w_in, "conf_conv_w": conf_conv_w, "conf_conv_b": conf_conv_b,
        "conf_ln_g": conf_ln_g, "conf_ln_b": conf_ln_b, "conf_w_out": conf_w_out,
        "lru_w_in": lru_w_in, "lru_conv_w": lru_conv_w, "lru_conv_b": lru_conv_b,
        "lru_w_a": lru_w_a, "lru_b_a": lru_b_a, "lru_w_x": lru_w_x, "lru_b_x": lru_b_x,
        "lru_lambda": lru_lambda, "lru_w_out": lru_w_out,
        "ffn_w13": ffn_w13, "ffn_w2": ffn_w2, "moe_router": moe_router,
        "moe_w13": moe_w13, "moe_w2": moe_w2,
    }


def reference(x, c, ada_w, ada_b, ada_table, norm_mix, norm_ffn, norm_final, rel_bias,
              att_w_in, att_g_cq, att_g_ckv, att_w_uq, att_w_uk, att_w_uv, att_w_qidx, att_w_out,
              sconv_w_in, sconv_conv_w, sconv_w_out,
              conf_w_in, conf_conv_w, conf_conv_b, conf_ln_g, conf_ln_b, conf_w_out,
              lru_w_in, lru_conv_w, lru_conv_b, lru_w_a, lru_b_a, lru_w_x, lru_b_x, lru_lambda, lru_w_out,
              ffn_w13, ffn_w2, moe_router, moe_w13, moe_w2):
    bsz = x.shape[0]
    mod_all = (jax.nn.silu(c) @ ada_w + ada_b).reshape(bsz, N_ADA, D_MODEL)
    for i in range(DEPTH):
        mod = mod_all + ada_table[i]
        shift_m, scale_m, gate_m = mod[:, 0, None, :], mod[:, 1, None, :], mod[:, 2, None, :]
        shift_f, scale_f, gate_f = mod[:, 3, None, :], mod[:, 4, None, :], mod[:, 5, None, :]

        h = modulate(rms_norm(x, norm_mix[i]), shift_m, scale_m)
        kind, j = i % N_MIXERS, i // N_MIXERS
        if kind == 0:
            y = dsa_attention(h, rel_bias, att_w_in[j], att_g_cq[j], att_g_ckv[j], att_w_uq[j],
                              att_w_uk[j], att_w_uv[j], att_w_qidx[j], att_w_out[j])
        elif kind == 1:
            y = short_conv_mixer(h, sconv_w_in[j], sconv_conv_w[j], sconv_w_out[j])
        elif kind == 2:
            y = conformer_conv_mixer(h, conf_w_in[j], conf_conv_w[j], conf_conv_b[j],
                                     conf_ln_g[j], conf_ln_b[j], conf_w_out[j])
        else:
            y = rglru_mixer(h, lru_w_in[j], lru_conv_w[j], lru_conv_b[j], lru_w_a[j], lru_b_a[j],
                            lru_w_x[j], lru_b_x[j], lru_lambda[j], lru_w_out[j])
        x = x + gate_m * y

        h = modulate(rms_norm(x, norm_ffn[i]), shift_f, scale_f)
        if i % 2 == 0:
            y = swiglu(h, ffn_w13[i // 2], ffn_w2[i // 2])
        else:
            y = moe_swiglu(h, moe_router[i // 2], moe_w13[i // 2], moe_w2[i // 2])
        x = x + gate_f * y
    return rms_norm(x, norm_final)
```

```python
import types
import numpy as np
import ml_dtypes
from contextlib import ExitStack
import concourse.bass as bass
import concourse.mybir as mybir
from concourse.bass_utils import run_bass_kernel_spmd

F32 = mybir.dt.float32
BF16 = mybir.dt.bfloat16
AF = mybir.ActivationFunctionType
ALU = mybir.AluOpType
AX = mybir.AxisListType
NEG = -1.0e30
DEBUG = False
HALO = 32


class Cfg:
    def __init__(s, D=4096, SEQ=2048, NH=32, QL=1024, KVL=512, IH=64, TOPK=256, LH=16, DFF=8192, NE=8,
                 DFE=2048, DEPTH=4):
        s.D, s.SEQ, s.NH, s.QL, s.KVL, s.IH, s.LH, s.DFF, s.NE, s.DFE, s.DEPTH = D, SEQ, NH, QL, KVL, IH, LH, DFF, NE, DFE, DEPTH
        s.T = SEQ // 2
        s.S = SEQ
        s.KSEL = min(TOPK, SEQ // 4)
        s.DC = D // 128
        s.AW = NH * 128
        s.ATT_IN = QL + KVL + 128 + IH
        s.LB = D // LH
        s.TT = min(512, s.T)
        s.NT = s.T // s.TT
        s.TB = s.T // 128


class StopBuild(Exception):
    pass


class Res:
    def __init__(s, name):
        s.name, s.w, s.r = name, None, []


class Q:
    def __init__(s, nc, name, sems, is_dma):
        s.nc, s.name, s.sems, s.is_dma = nc, name, sems, is_dma
        s.n = 0
        s.seen = {}
        s.prog = []

    def wait_tok(s, tok):
        kind = tok[0]
        if kind == "c":
            _, F, n = tok
            if F is s and s.name == "pe":
                return
            if s.seen.get(F.name, 0) >= n:
                return
            s.seen[F.name] = n
            sem = F.sems[0]
            s.prog.append(lambda e, sem=sem, n=n: e.wait_ge(sem, n))
        else:
            _, sem, val, key = tok
            if s.seen.get(key, 0) >= val:
                return
            s.seen[key] = val
            s.prog.append(lambda e, sem=sem, val=val: e.wait_ge(sem, val))

    def issue(s, fn, own_sem=None):
        if own_sem is not None:
            s.prog.append(lambda e, fn=fn, sem=own_sem: fn(e).then_inc(sem))
            tok = ("d", own_sem, 1, id(own_sem))
            s.cc_toks = getattr(s, "cc_toks", []) + [tok]
            return tok
        if s.is_dma:
            K = len(s.sems)
            i = s.n
            s.n += 1
            slot = i % K
            sem = s.sems[slot]
            if i >= K:
                prev = 16 * (i // K)
                key = (s.name, slot)
                if s.seen.get(key, 0) < prev:
                    s.seen[key] = prev
                    s.prog.append(lambda e, sem=sem, prev=prev: e.wait_ge(sem, prev))
            s.prog.append(lambda e, fn=fn, sem=sem: fn(e).then_inc(sem, 16))
            return ("d", sem, 16 * (i // K + 1), (s.name, slot))
        s.n += 1
        sem = s.sems[0]
        s.prog.append(lambda e, fn=fn, sem=sem: fn(e).then_inc(sem, 1))
        return ("c", s, s.n)


def _freeze(fn):
    if fn.__closure__ is None:
        return fn
    cells = []
    for cell in fn.__closure__:
        try:
            cells.append(types.CellType(cell.cell_contents))
        except ValueError:
            cells.append(cell)
    return types.FunctionType(fn.__code__, fn.__globals__, fn.__name__, fn.__defaults__, tuple(cells))


def emit(q, fn, reads=(), writes=(), own_sem=None):
    fn = _freeze(fn)
    deps = []
    for t in reads:
        if t.w is not None:
            deps.append(t.w)
    for t in writes:
        if t.w is not None:
            deps.append(t.w)
        deps.extend(t.r)
    for d in deps:
        q.wait_tok(d)
    tok = q.issue(fn, own_sem)
    for t in reads:
        if tok[0] == "c":
            t.r = [x for x in t.r if not (x[0] == "c" and x[1] is tok[1])]
        t.r.append(tok)
    for t in writes:
        t.w = tok
        t.r = []
    return tok


class B:
    def __init__(s, cfg, stop_after=None, only=None):
        s.c = cfg
        s.stop_after = stop_after
        s.only = only
        s.debug = DEBUG
        s.nc = bass.Bass("TRN2", target_bir_lowering=False)
        s.es = ExitStack()
        s.din = {}
        s.nsem = 0

    def sem(s):
        s.nsem += 1
        return s.es.enter_context(s.nc.semaphore(f"s{s.nsem}"))

    def inp(s, name, shape, dt=F32):
        h = s.nc.dram_tensor(name, list(shape), dt, kind="ExternalInput").ap()
        s.din[name] = (tuple(shape), dt)
        return h

    def dram(s, name, shape, dt):
        return s.nc.dram_tensor(name, list(shape), dt).ap()

    def sb(s, name, shape, dt):
        return s.es.enter_context(s.nc.sbuf_tensor("sb_" + name, list(shape), dt))

    def weight(s, name, K, Fd):
        src = s.inp(name, [K, Fd])
        full = s.dram(name + "_f", [K, Fd], BF16)
        step = max(1, min(K, (1 << 20) // Fd))
        chunks = [(r0, min(K, r0 + step)) for r0 in range(0, K, step)]
        rls = [Res(name + "_f") for _ in chunks]
        s.wts[name] = (src, full, chunks, rls)
        return full, rls

    def gather(s, name):
        src, full, chunks, rls = s.wts[name]
        for (r0, r1), rl in zip(chunks, rls):
            emit(s.pq, lambda e: e.dma_start(out=full[r0:r1, :], in_=src[r0:r1, :]), [], [rl])

    def gather2(s, loc, rls, full, rf, rows, Fd, dt, name):
        quad = s.dram(name + "_q", [4 * rows, Fd], dt)
        rq = Res(name + "_q")
        emit(s.pq, lambda e: e.collective_compute("AllGather", ALU.bypass, replica_groups=[[0, 1, 2, 3], [4, 5, 6, 7]],
                                                  ins=[loc.opt()], outs=[quad.opt()]), rls, [rq], own_sem=s.sem())
        emit(s.pq, lambda e: e.collective_compute("AllGather", ALU.bypass, replica_groups=[[0, 4], [1, 5], [2, 6], [3, 7]],
                                                  ins=[quad.opt()], outs=[full.opt()]), [rq], [rf], own_sem=s.sem())

    def pair_exchange(s, loc, rl, pair, rp):
        emit(s.pq, lambda e: e.collective_compute("AllGather", ALU.bypass, replica_groups=[[0, 1], [2, 3], [4, 5], [6, 7]],
                                                  ins=[loc.opt()], outs=[pair.opt()]), [rl], [rp], own_sem=s.sem())

    def linear(s, Wd, Wres, k0, KC, cols, xfn, xres, epi, N, pre=None):
        PF = len(s.wt) - 1
        n = len(cols)
        slots = {}
        xr = list(xres)

        def load(j):
            c0, w = cols[j]
            sl = s.wcount % len(s.wt)
            s.wcount += 1
            slots[j] = sl
            wt, wr = s.wt[sl], s.wt_r[sl]
            emit(s.sq, lambda e, wt=wt, c0=c0, w=w: e.dma_start(
                out=wt[:, 0:KC, 0:w], in_=Wd[k0:k0 + KC * 128, c0:c0 + w].rearrange("(kc p) f -> p kc f", p=128)),
                list(Wres), [wr])
            if pre is not None:
                pre(j)

        for j in range(min(PF, n)):
            load(j)
        for j in range(n):
            if j + PF < n:
                load(j + PF)
            c0, w = cols[j]
            sl = slots[j]
            wt, wr = s.wt[sl], s.wt_r[sl]
            if N > 512:
                pi = s.pcount % 3
                ps = s.psum[:, pi * 1024:(pi + 1) * 1024]
                pr = [s.bank_r[2 * pi], s.bank_r[2 * pi + 1]]
            else:
                pi = 4 + s.pcount % 2
                ps = s.bank[pi]
                pr = [s.bank_r[pi]]
            s.pcount += 1
            for n0 in range(0, N, 512):
                n1 = min(N, n0 + 512)
                for kc in range(KC):
                    rhs = xfn(kc, n0, n1)
                    emit(s.pe, lambda e, ps=ps, wt=wt, kc=kc, w=w, n0=n0, n1=n1, rhs=rhs: e.matmul(
                        ps[0:w, n0:n1], lhsT=wt[:, kc, 0:w], rhs=rhs, start=(kc == 0), stop=(kc == KC - 1)),
                        [wr] + xr, pr)
            epi(j, ps, pr, w)

    def next_xt(s):
        sl = s.xcount % len(s.xt)
        s.xcount += 1
        return s.xt[sl], s.xt_r[sl]

    def resid(s, gate):
        c = s.c
        slots = {}

        def pre(j):
            xt, xr = s.next_xt()
            slots[j] = (xt, xr)
            emit(s.sq, lambda e: e.dma_start(out=xt, in_=s.xs[j * 128:(j + 1) * 128, :]), [s.xs_r[j]], [xr])

        def epi(j, ps, pr, w):
            xt, xr = slots[j]
            emit(s.dve, lambda e: e.scalar_tensor_tensor(out=xt, in0=ps[:, 0:c.T], scalar=gate[:, j:j + 1], in1=xt,
                                                         op0=ALU.mult, op1=ALU.add), pr + [xr, s.mod_r], [xr])
            emit(s.sq, lambda e: e.dma_start(out=s.xs[j * 128:(j + 1) * 128, :], in_=xt), [xr], [s.xs_r[j]])

        return pre, epi

    def colsum_bc(s, acc, acc_r, out, out_r, scale, eps, power):
        c = s.c
        ps, pr = s.bank[6], [s.bank_r[6]]
        for n0 in range(0, c.T, 512):
            n1 = min(c.T, n0 + 512)
            emit(s.pe, lambda e, n0=n0, n1=n1: e.matmul(ps[:, 0:n1 - n0], lhsT=s.ones32[:, :], rhs=acc[:, n0:n1], start=True, stop=True),
                 acc_r + [s.const_r], pr)
            emit(s.dve, lambda e, n0=n0, n1=n1: e.tensor_scalar(out=out[:, n0:n1], in0=ps[:, 0:n1 - n0], scalar1=scale, scalar2=eps,
                                                                  op0=ALU.mult, op1=ALU.add), pr, out_r)
        if power is not None:
            s.rpow(out, out_r, power)

    def rpow(s, out, out_r, power):
        emit(s.act, lambda e: e.activation(out=out, in_=out, func=AF.Ln), out_r, out_r)
        emit(s.act, lambda e: e.activation(out=out, in_=out, func=AF.Exp, scale=power), out_r, out_r)

    def norm(s, gs, sh, out_fn=None, hook=None):
        c = s.c
        acc, acc_r = s.st[0], [s.st_r[0]]
        for dc in range(c.DC):
            xt, xr = s.next_xt()
            emit(s.sq, lambda e: e.dma_start(out=xt, in_=s.xs[dc * 128:(dc + 1) * 128, :]), [s.xs_r[dc]], [xr])
            if dc == 0:
                emit(s.act, lambda e: e.activation(out=acc, in_=xt, func=AF.Square), [xr], acc_r)
            else:
                tq, tr = s.tmp[dc % 2], s.tmp_r[dc % 2]
                emit(s.act, lambda e: e.activation(out=tq, in_=xt, func=AF.Square), [xr], [tr])
                emit(s.dve, lambda e: e.tensor_tensor(out=acc, in0=acc, in1=tq, op=ALU.add), [tr] + acc_r, acc_r)
        rstd, rstd_r = s.st[1], [s.st_r[1]]
        s.colsum_bc(acc, acc_r, rstd, rstd_r, 1.0 / c.D, 1e-6, -0.5)
        if "rstd" not in s.dbg_map:
            s.dbg("acc", acc, acc_r, c.T)
            s.dbg("rstd", rstd, rstd_r, c.T)
        for dc in range(c.DC):
            xt, xr = s.next_xt()
            emit(s.sq, lambda e: e.dma_start(out=xt, in_=s.xs[dc * 128:(dc + 1) * 128, :]), [s.xs_r[dc]], [xr])
            emit(s.dve, lambda e: e.tensor_tensor(out=xt, in0=xt, in1=rstd, op=ALU.mult), [xr] + rstd_r, [xr])
            bias = sh[:, dc:dc + 1] if sh is not None else 0.0
            if out_fn is not None:
                out_fn(dc, xt, xr, gs)
            elif hook is not None:
                tq, tr = s.tmp[dc % 2], s.tmp_r[dc % 2]
                emit(s.act, lambda e: e.activation(out=tq, in_=xt, func=AF.Identity, bias=bias, scale=gs[:, dc:dc + 1]),
                     [xr, s.mod_r], [tr])
                emit(s.dve, lambda e: e.tensor_copy(out=s.hT[:, dc, :], in_=tq), [tr], [s.hT_r[dc]])
                hook(dc, tq, tr)
            else:
                emit(s.act, lambda e: e.activation(out=s.hT[:, dc, :], in_=xt, func=AF.Identity, bias=bias, scale=gs[:, dc:dc + 1]),
                     [xr, s.mod_r], [s.hT_r[dc]])
                if dc == 0 and "h0" not in s.dbg_map:
                    s.dbg("h0", s.hT[:, 0, :], [s.hT_r[0]], c.T)

    def hx(s, kc, n0, n1):
        return s.hT[:, kc, n0:n1]

    def midx(s, kc, n0, n1):
        return s.mid[:, kc, HALO + n0:HALO + n1]

    def midc(s, dc):
        return s.mid[:, dc, HALO:HALO + s.c.T]

    def halo_exchange(s, tag):
        c = s.c
        loc = s.dram(f"halo_l{tag}", [c.D, HALO], BF16)
        pair = s.dram(f"halo_p{tag}", [2 * c.D, HALO], BF16)
        rl, rp = Res("hl"), Res("hp")
        emit(s.pq, lambda e: e.dma_start(out=loc.rearrange("(dc p) h -> p dc h", p=128), in_=s.mid[:, :, c.T:c.T + HALO]), s.mid_r, [rl])
        s.pair_exchange(loc, rl, pair, rp)
        emit(s.sq, lambda e: e.dma_start(out=s.mid[:, :, 0:HALO], in_=pair[0:c.D, :].rearrange("(dc p) h -> p dc h", p=128)), [rp], s.mid_r)
        emit(s.dve, lambda e: e.tensor_scalar(out=s.mid[:, :, 0:HALO], in0=s.mid[:, :, 0:HALO], scalar1=s.flag[:, 0:1], scalar2=None, op0=ALU.mult),
             s.mid_r + [s.const_r], s.mid_r)

    def dwconv(s, dc, cw, K, out, out_r, bias=None):
        c = s.c
        for k in range(K):
            off = HALO - (K - 1) + k
            src = s.mid[:, dc, off:off + c.T]
            wk = cw[:, k * c.DC + dc:k * c.DC + dc + 1]
            if k == 0:
                if bias is not None:
                    emit(s.dve, lambda e, src=src, wk=wk: e.tensor_scalar(out=out, in0=src, scalar1=wk, scalar2=bias[:, dc:dc + 1],
                                                                        op0=ALU.mult, op1=ALU.add), [s.mid_r[dc], s.cv_r], out_r)
                else:
                    emit(s.dve, lambda e, src=src, wk=wk: e.tensor_scalar(out=out, in0=src, scalar1=wk, scalar2=None, op0=ALU.mult),
                         [s.mid_r[dc], s.cv_r], out_r)
            else:
                emit(s.dve, lambda e, src=src, wk=wk: e.scalar_tensor_tensor(out=out, in0=src, scalar=wk, in1=out,
                                                                           op0=ALU.mult, op1=ALU.add), [s.mid_r[dc], s.cv_r] + out_r, out_r)

    def load_small(s, dst, src, res):
        emit(s.sq, lambda e: e.dma_start(out=dst, in_=src), [], [res])

    def build(s):
        c = s.c
        nc = s.nc
        es = s.es
        D, T, DC = c.D, c.T, c.DC
        s.pe = Q(nc, "pe", [s.sem()], False)
        s.act = Q(nc, "act", [s.sem()], False)
        s.dve = Q(nc, "dve", [s.sem()], False)
        s.sq = Q(nc, "sq", [s.sem() for _ in range(8)], True)
        s.pq = Q(nc, "pq", [s.sem() for _ in range(8)], True)
        s.wts = {}
        s.wcount = s.pcount = s.xcount = 0

        xT_in = s.inp("xT", [D, T])
        out_d = nc.dram_tensor("outT", [D, T], F32, kind="ExternalOutput").ap()
        s.dbg_d = nc.dram_tensor("dbg", [128, 8192], F32, kind="ExternalOutput").ap() if s.debug else None
        s.dbg_off = 0
        s.dbg_map = {}
        cT_in = s.inp("cT", [128, DC * 4])
        adaw_in = s.inp("ada_w", [D, 6 * D // 8])
        adab_in = s.inp("ada_b", [128, 6 * DC // 8])
        adat_in = s.inp("ada_t", [128, c.DEPTH * 6 * DC])
        nmix_in = s.inp("norm_mix", [128, c.DEPTH * DC])
        nffn_in = s.inp("norm_ffn", [128, c.DEPTH * DC])
        nfin_in = s.inp("norm_final", [128, DC])
        bsel_in = s.inp("bsel", [128, 4])
        flag_in = s.inp("flag", [128, 2])
        ident_in = s.inp("ident", [128, 128])
        tri_in = s.inp("tri", [128, 128])
        s.ohs_in = s.inp("ohs", [128, 2 * 32 * 128])
        s.rbb_in = s.inp("rbb", [128, 32 * c.NH])
        s.gcq_in = s.inp("g_cq", [128, c.QL // 128])
        s.gckv_in = s.inp("g_ckv", [128, c.KVL // 128])
        s.scw_in = s.inp("sconv_cw", [128, 3 * DC])
        s.cfw_in = s.inp("conf_cw", [128, 31 * DC])
        s.cfv_in = s.inp("conf_vec", [128, 3 * DC])
        s.lrw_in = s.inp("lru_cw", [128, 4 * DC])
        s.lrv_in = s.inp("lru_vec", [128, 4 * DC])
        s.rt_in = s.inp("router", [128, 2 * DC * 8])

        s.xs = s.dram("xs", [D, T], F32)
        s.xs_r = [Res(f"xs{i}") for i in range(DC)]

        s.full = (c.D == 4096)
        s.hT = s.sb("hT", [128, DC, T], BF16)
        s.hT_r = [Res(f"hT{i}") for i in range(DC)]
        s.mid = s.sb("mid", [128, DC, T + HALO], BF16)
        s.mid_r = [Res(f"mid{i}") for i in range(DC)]
        NW = 2 if s.full else 3
        s.wt = [s.sb(f"wt{i}", [128, DC, 128], BF16) for i in range(NW)]
        s.wt_r = [Res(f"wt{i}") for i in range(NW)]
        s.scr = s.sb("scr", [128, 7 * T], F32)
        s.xt = [s.scr[:, i * T:(i + 1) * T] for i in range(2)]
        s.xt_r = [Res(f"xt{i}") for i in range(2)]
        s.tmp = [s.scr[:, (2 + i) * T:(3 + i) * T] for i in range(2)]
        s.tmp_r = [Res(f"tmp{i}") for i in range(2)]
        s.st = [s.scr[:, (4 + i) * T:(5 + i) * T] for i in range(2)]
        s.st_r = [Res(f"st{i}") for i in range(2)]
        s.px = s.scr[:, 6 * T:7 * T]
        s.px_r = Res("px")
        s.xt = s.xt + [s.px]
        s.xt_r = s.xt_r + [s.px_r]
        psum = es.enter_context(nc.psum_tensor("psum", [128, 4096], F32))
        s.bank = [psum[:, i * 512:(i + 1) * 512] for i in range(8)]
        s.bank_r = [Res(f"bank{i}") for i in range(8)]
        s.psum = psum
        s.const_r = Res("const")
        s.ones32 = s.sb("ones32", [128, 128], F32)
        s.ident32 = s.sb("ident32", [128, 128], F32)
        s.identb = s.sb("identb", [128, 128], BF16)
        s.onesb = s.sb("onesb", [128, 128], BF16)
        s.tri = s.sb("tri", [128, 128], F32)
        s.flag = s.sb("flag", [128, 2], F32)
        bsel = s.sb("bsel", [128, 4], F32)
        modsel = s.sb("modsel", [128, 6 * DC], F32)
        s.modL = s.sb("modL", [128, c.DEPTH * 6 * DC], F32)
        nmix = s.sb("nmix", [128, c.DEPTH * DC], F32)
        nffn = s.sb("nffn", [128, c.DEPTH * DC], F32)
        s.nfin = s.sb("nfin", [128, DC], F32)
        s.rtf = s.sb("rtf", [128, DC * 8], F32)
        s.rtf_r = Res("rtf")
        s.gtok = s.sb("gtok", [128, c.TB, 8], F32)
        s.gtok_r = Res("gtok")
        s.sm = [s.sb(f"sm{i}", [128, 16], F32) for i in range(3)]
        s.sm_r = Res("sm")
        s.gb = s.sb("gb", [128, 128], F32)
        s.gb_r = Res("gb")
        s.EB = s.sb("EB", [128, c.NH * 256], BF16)
        s.EB_r = Res("EB")
        s.cvw = s.sb("cvw", [128, 31 * DC], F32)
        s.cvv = s.sb("cvv", [128, 4 * DC], F32)
        s.cv_r = Res("cv")
        s.vec = s.sb("vec", [128, 4 * DC], F32)
        s.vec_r = Res("vec")
        s.mod_r = Res("modL")
        small_r = Res("small")

        for dst, src in ((s.ident32[:, :], ident_in), (s.tri[:, :], tri_in), (s.flag[:, :], flag_in), (bsel[:, :], bsel_in),
                         (s.modL[:, :], adat_in), (nmix[:, :], nmix_in), (nffn[:, :], nffn_in), (s.nfin[:, :], nfin_in)):
            s.load_small(dst, src, small_r)
        emit(s.dve, lambda e: e.memset(s.ones32[:, :], 1.0), [], [s.const_r])
        emit(s.dve, lambda e: e.memset(s.onesb[:, :], 1.0), [s.const_r], [s.const_r])
        emit(s.dve, lambda e: e.tensor_copy(out=s.identb[:, :], in_=s.ident32[:, :]), [small_r, s.const_r], [s.const_r])

        for dc in range(DC):
            emit(s.sq, lambda e, dc=dc: e.dma_start(out=s.xs[dc * 128:(dc + 1) * 128, :], in_=xT_in[dc * 128:(dc + 1) * 128, :]), [], [s.xs_r[dc]])

        W = {}
        s.W = W
        decls = [("att_w_in", D, c.ATT_IN), ("att_w_qidx", c.QL, c.IH * 128), ("att_w_uq", c.QL, c.AW), ("att_w_ukT", c.AW, c.KVL),
                 ("att_w_uv", c.KVL, c.AW), ("att_w_out", c.AW, D), ("ffn_w13_0", D, 2 * c.DFF), ("ffn_w2_0", c.DFF, D),
                 ("sconv_w_in", D, 3 * D), ("sconv_w_out", D, D), ("moe_w13_0", c.NE * D, 2 * c.DFE), ("moe_w2_0", c.NE * c.DFE, D),
                 ("conf_w_in", D, 2 * D), ("conf_w_out", D, D), ("ffn_w13_1", D, 2 * c.DFF), ("ffn_w2_1", c.DFF, D),
                 ("lru_w_in", D, 2 * D), ("lru_w_a", D, c.LB), ("lru_w_x", D, c.LB), ("lru_w_out", D, D),
                 ("moe_w13_1", c.NE * D, 2 * c.DFE), ("moe_w2_1", c.NE * c.DFE, D)]
        for (name, K, Fd) in decls:
            W[name] = s.weight(name, K, Fd)
        order = [d[0] for d in decls]
        gi = [0]

        def gather_upto(name):
            while gi[0] < len(order) and gi[0] <= order.index(name):
                s.gather(order[gi[0]])
                gi[0] += 1

        gather_upto("ffn_w2_0")

        NCL = 6 * DC // 8
        cT = s.st[0]
        emit(s.sq, lambda e: e.dma_start(out=cT[:, 0:DC * 4], in_=cT_in), [], [s.st_r[0]])
        scT = s.sb("scT", [128, DC * 4], BF16)
        scT_r = Res("scT")
        emit(s.act, lambda e: e.activation(out=scT[:, :], in_=cT[:, 0:DC * 4], func=AF.Silu), [s.st_r[0]], [scT_r])
        adab = s.sb("adab", [128, NCL], F32)
        s.load_small(adab[:, :], adab_in, small_r)
        modp = s.sb("modp", [128, NCL * 4], F32)
        modp_r = Res("modp")
        for j in range(NCL):
            sl = s.wcount % len(s.wt)
            s.wcount += 1
            wt, wr = s.wt[sl], s.wt_r[sl]
            emit(s.pq, lambda e, wt=wt, j=j: e.dma_start(out=wt[:, 0:DC, :], in_=adaw_in[:, j * 128:(j + 1) * 128].rearrange("(kc p) f -> p kc f", p=128)),
                 [], [wr])
            ps, pr = s.bank[6 + j % 2], [s.bank_r[6 + j % 2]]
            for kc in range(DC):
                emit(s.pe, lambda e, ps=ps, wt=wt, kc=kc: e.matmul(ps[:, 0:4], lhsT=wt[:, kc, :], rhs=scT[:, kc * 4:(kc + 1) * 4],
                                                                   start=(kc == 0), stop=(kc == DC - 1)), [wr, scT_r], pr)
            emit(s.dve, lambda e, ps=ps, j=j: e.tensor_scalar(out=modp[:, j * 4:(j + 1) * 4], in0=ps[:, 0:4], scalar1=adab[:, j:j + 1], scalar2=None,
                                                              op0=ALU.add), pr + [small_r], [modp_r])
        modl_d = s.dram("modl", [NCL * 128, 4], F32)
        modf_d = s.dram("modf", [6 * D, 4], F32)
        rl, rf = Res("modl"), Res("modf")
        emit(s.pq, lambda e: e.dma_start(out=modl_d.rearrange("(j p) b -> p j b", p=128), in_=modp[:, :].rearrange("p (j b) -> p j b", b=4)), [modp_r], [rl])
        s.gather2(modl_d, [rl], modf_d, rf, NCL * 128, 4, F32, "modg")
        modall = s.st[1]
        m3 = modall[:, 0:6 * DC * 4].rearrange("p (j b) -> p j b", b=4)
        emit(s.sq, lambda e: e.dma_start(out=m3, in_=modf_d.rearrange("(j p) b -> p j b", p=128)), [rf], [s.st_r[1]])
        ms_r = Res("modsel")
        emit(s.dve, lambda e: e.tensor_scalar(out=modsel[:, :], in0=m3[:, :, 0], scalar1=bsel[:, 0:1], scalar2=None, op0=ALU.mult), [s.st_r[1], small_r], [ms_r])
        for b in range(1, 4):
            emit(s.dve, lambda e, b=b: e.scalar_tensor_tensor(out=modsel[:, :], in0=m3[:, :, b], scalar=bsel[:, b:b + 1], in1=modsel[:, :],
                                                              op0=ALU.mult, op1=ALU.add), [s.st_r[1], ms_r], [ms_r])
        mod_r = s.mod_r
        for i in range(c.DEPTH):
            o = i * 6 * DC
            emit(s.dve, lambda e, o=o: e.tensor_tensor(out=s.modL[:, o:o + 6 * DC], in0=s.modL[:, o:o + 6 * DC],
                                                       in1=modsel[:, :], op=ALU.add), [ms_r, small_r, mod_r], [mod_r])
            for (j, nw) in ((1, nmix), (4, nffn)):
                emit(s.dve, lambda e, o=o, j=j, nw=nw, i=i: e.scalar_tensor_tensor(
                    out=s.modL[:, o + j * DC:o + (j + 1) * DC], in0=s.modL[:, o + j * DC:o + (j + 1) * DC], scalar=1.0,
                    in1=nw[:, i * DC:(i + 1) * DC], op0=ALU.add, op1=ALU.mult), [mod_r, small_r], [mod_r])

        def M(i, j):
            o = i * 6 * DC + j * DC
            return s.modL[:, o:o + DC]

        s.dbg("modL", s.modL[:, :], [mod_r], c.DEPTH * 6 * DC)
        s.dbg("modsel", modsel[:, :], [ms_r], 6 * DC)
        s.dbg("modp", modp[:, :], [modp_r], NCL * 4)

        done = False
        try:
            s.layers(M, gather_upto)
        except StopBuild:
            done = True
        for i in range(0):
            if s.stop_after is not None and s.stop_after[0] == "pre":
                done = True
                break
            kind = i % 4
            if kind == 0:
                s.attention(M(i, 1), M(i, 0), M(i, 2))
                gather_upto("moe_w2_0")
            elif kind == 1:
                s.sconv(M(i, 1), M(i, 0), M(i, 2))
                gather_upto("ffn_w2_1")
            elif kind == 2:
                s.conformer(M(i, 1), M(i, 0), M(i, 2))
                gather_upto("moe_w2_1")
            else:
                s.rglru(M(i, 1), M(i, 0), M(i, 2))
            if s.stop_after == ("mix", i):
                done = True
                break
            if i % 2 == 0:
                s.norm(M(i, 4), M(i, 3))
                s.ffn(W[f"ffn_w13_{i // 2}"], W[f"ffn_w2_{i // 2}"], M(i, 5))
            else:
                s.moe(W[f"moe_w13_{i // 2}"], W[f"moe_w2_{i // 2}"], M(i, 4), M(i, 3), M(i, 5), i // 2)
            if s.stop_after == ("ffn", i):
                done = True
                break

        s.out_r = Res("out")

        def fin(dc, xt, xr, gs):
            emit(s.act, lambda e: e.activation(out=xt, in_=xt, func=AF.Identity, scale=gs[:, dc:dc + 1]), [xr, small_r], [xr])
            emit(s.sq, lambda e: e.dma_start(out=out_d[dc * 128:(dc + 1) * 128, :], in_=xt), [xr], [s.out_r])

        if not done:
            s.norm(s.nfin, None, out_fn=fin)
        else:
            for dc in range(DC):
                xt, xr = s.next_xt()
                emit(s.sq, lambda e: e.dma_start(out=xt, in_=s.xs[dc * 128:(dc + 1) * 128, :]), [s.xs_r[dc]], [xr])
                emit(s.pq, lambda e: e.dma_start(out=out_d[dc * 128:(dc + 1) * 128, :], in_=xt), [xr], [s.out_r])
        for tok in getattr(s.pq, "cc_toks", []):
            s.pq.wait_tok(tok)
        for q in (s.pe, s.act, s.dve):
            if q.n > 0:
                s.pq.wait_tok(("c", q, q.n))
        for q in (s.pq, s.sq):
            K = len(q.sems)
            for i in range(max(0, q.n - K), q.n):
                q.wait_tok(("d", q.sems[i % K], 16 * (i // K + 1), (q.name, i % K)))

        with nc.Block() as block:
            @block.tensor
            def _(e):
                for f in s.pe.prog:
                    f(e)

            @block.scalar
            def _(e):
                for f in s.act.prog:
                    f(e)

            @block.vector
            def _(e):
                for f in s.dve.prog:
                    f(e)

            @block.sync
            def _(e):
                for f in s.sq.prog:
                    f(e)

            @block.gpsimd
            def _(e):
                for f in s.pq.prog:
                    f(e)
        es.close()
        return nc

    def dbg(s, name, ap, res, n):
        if not s.debug or s.dbg_off + n > 8192:
            return
        o = s.dbg_off
        s.dbg_off += n
        s.dbg_map[name] = (o, n)
        emit(s.pq, lambda e: e.dma_start(out=s.dbg_d[:, o:o + n], in_=ap), res, [Res("dbg")])

    def chk(s, tag, kind="att"):
        if s.stop_after == (kind, tag):
            raise StopBuild()

    def layers(s, M, gather_upto):
        c = s.c
        W = s.W
        for i in range(c.DEPTH):
            if s.stop_after is not None and s.stop_after[0] == "pre":
                raise StopBuild()
            kind = i % 4
            gather_upto(["moe_w2_0", "ffn_w2_1", "moe_w2_1", "moe_w2_1"][kind])
            if s.only is None or f"mix{i}" in s.only:
                if kind == 0:
                    s.attention(M(i, 1), M(i, 0), M(i, 2))
                elif kind == 1:
                    s.sconv(M(i, 1), M(i, 0), M(i, 2))
                elif kind == 2:
                    s.conformer(M(i, 1), M(i, 0), M(i, 2))
                else:
                    s.rglru(M(i, 1), M(i, 0), M(i, 2))
            if s.stop_after == ("mix", i):
                raise StopBuild()
            if s.only is None or f"ffn{i}" in s.only:
                if i % 2 == 0:
                    s.norm(M(i, 4), M(i, 3))
                    s.ffn(W[f"ffn_w13_{i // 2}"], W[f"ffn_w2_{i // 2}"], M(i, 5))
                else:
                    s.moe(W[f"moe_w13_{i // 2}"], W[f"moe_w2_{i // 2}"], M(i, 4), M(i, 3), M(i, 5), i // 2)
            if s.stop_after == ("ffn", i):
                raise StopBuild()

    def swiglu_epi(s, gbc=None, gbc_r=None):
        c = s.c

        def epi(idx, ps, pr, w):
            jj, which = idx // 2, idx % 2
            tq, tr = s.tmp[jj % 2], s.tmp_r[jj % 2]
            if which == 0:
                emit(s.act, lambda e: e.activation(out=tq, in_=ps[:, 0:c.T], func=AF.Silu), pr, [tr])
                if gbc is not None:
                    emit(s.dve, lambda e: e.tensor_tensor(out=tq, in0=tq, in1=gbc, op=ALU.mult), [tr] + gbc_r, [tr])
            else:
                emit(s.dve, lambda e: e.tensor_tensor(out=s.midc(jj), in0=ps[:, 0:c.T], in1=tq, op=ALU.mult), pr + [tr], [s.mid_r[jj]])
        return epi

    def ffn(s, W13, W2, gate):
        c = s.c
        (w13, r13), (w2, r2) = W13, W2
        G = c.DC
        HC = c.DFF // 128
        for g in range(HC // G):
            cols = []
            for jj in range(G):
                j = g * G + jj
                cols.append((j * 128, 128))
                cols.append((c.DFF + j * 128, 128))
            s.linear(w13, r13, 0, c.DC, cols, s.hx, s.hT_r, s.swiglu_epi(), c.T)
            if "act0" not in s.dbg_map:
                s.dbg("act0", s.midc(0), [s.mid_r[0]], c.T)
            pre, epi2 = s.resid(gate)
            s.linear(w2, r2, g * G * 128, G, [(d * 128, 128) for d in range(c.DC)], s.midx, s.mid_r, epi2, c.T, pre=pre)

    def moe(s, W13, W2, gs, sh, gate, li):
        c = s.c
        (w13, r13), (w2, r2) = W13, W2
        DC, T = c.DC, c.T
        rtf = s.rtf
        emit(s.sq, lambda e: e.dma_start(out=rtf[:, :], in_=s.rt_in[:, li * DC * 8:(li + 1) * DC * 8]), [], [s.rtf_r])
        lgps = s.psum[:, 2048:3072]
        lgps_r = [s.bank_r[4], s.bank_r[5]]

        def hook(dc, tq, tr):
            for n0 in range(0, T, 512):
                n1 = min(T, n0 + 512)
                emit(s.pe, lambda e, n0=n0, n1=n1: e.matmul(lgps[0:8, n0:n1], lhsT=rtf[:, dc * 8:(dc + 1) * 8], rhs=tq[:, n0:n1],
                                                            start=(dc == 0), stop=(dc == DC - 1)), [tr, s.rtf_r], lgps_r)

        s.norm(gs, sh, hook=hook)
        s.chk(1, "moe")
        lgT = s.st[0]
        emit(s.dve, lambda e: e.tensor_copy(out=lgT[0:8, :], in_=lgps[0:8, 0:T]), lgps_r, [s.st_r[0]])
        gtok = s.gtok
        l8, m8, ex = s.sm[0], s.sm[1], s.sm[2]
        r8 = [s.sm_r]
        for tb in range(c.TB):
            ps, pr = s.bank[6], [s.bank_r[6]]
            emit(s.pe, lambda e, tb=tb: e.matmul(ps[:, 0:8], lhsT=lgT[0:8, tb * 128:(tb + 1) * 128], rhs=s.ident32[0:8, 0:8], start=True, stop=True),
                 [s.st_r[0], s.const_r], pr)
            emit(s.dve, lambda e: e.tensor_copy(out=l8[:, 0:8], in_=ps[:, 0:8]), pr, r8)
            emit(s.dve, lambda e: e.max(out=m8[:, 0:8], in_=l8[:, 0:8]), r8, r8)
            emit(s.dve, lambda e: e.tensor_scalar(out=ex[:, 8:9], in0=m8[:, 0:1], scalar1=-1.0, scalar2=None, op0=ALU.mult), r8, r8)
            emit(s.act, lambda e: e.activation(out=ex[:, 0:8], in_=l8[:, 0:8], func=AF.Exp, bias=ex[:, 8:9], scale=1.0), r8, r8)
            emit(s.dve, lambda e: e.scalar_tensor_tensor(out=ex[:, 0:8], in0=l8[:, 0:8], scalar=m8[:, 1:2], in1=ex[:, 0:8], op0=ALU.is_ge, op1=ALU.mult),
                 r8, r8)
            emit(s.dve, lambda e: e.tensor_reduce(out=ex[:, 9:10], in_=ex[:, 0:8], axis=AX.X, op=ALU.add), r8, r8)
            emit(s.dve, lambda e: e.reciprocal(out=ex[:, 9:10], in_=ex[:, 9:10]), r8, r8)
            emit(s.dve, lambda e, tb=tb: e.tensor_scalar(out=gtok[:, tb, :], in0=ex[:, 0:8], scalar1=ex[:, 9:10], scalar2=None, op0=ALU.mult),
                 r8, [s.gtok_r])
        s.chk(2, "moe")
        HCE = c.DFE // 128
        for ei in range(c.NE):
            gps = s.psum[:, 2048:3072]
            gpr = [s.bank_r[4], s.bank_r[5]]
            for tb in range(c.TB):
                emit(s.dve, lambda e, tb=tb: e.tensor_copy(out=s.gb[:, :], in_=gtok[:, tb, ei:ei + 1].to_broadcast([128, 128])), [s.gtok_r], [s.gb_r])
                emit(s.pe, lambda e, tb=tb: e.matmul(gps[:, tb * 128:(tb + 1) * 128], lhsT=s.gb[:, :], rhs=s.ident32[:, :], start=True, stop=True),
                     [s.gb_r, s.const_r], gpr)
            gbc, gbc_r = s.st[1], [s.st_r[1]]
            emit(s.act, lambda e: e.activation(out=gbc, in_=gps[:, 0:T], func=AF.Identity), gpr, gbc_r)
            s.chk(3, "moe")
            cols = []
            for jj in range(HCE):
                cols.append((jj * 128, 128))
                cols.append((c.DFE + jj * 128, 128))
            s.linear(w13, r13, ei * c.D, DC, cols, s.hx, s.hT_r, s.swiglu_epi(gbc, gbc_r), T)
            s.chk(4, "moe")
            pre, epi2 = s.resid(gate)
            s.linear(w2, r2, ei * c.DFE, HCE, [(d * 128, 128) for d in range(DC)], s.midx, s.mid_r[0:HCE], epi2, T, pre=pre)

    def sconv(s, gs, sh, gate):
        c = s.c
        D, DC, T = c.D, c.DC, c.T
        (w_in, r_in), (w_out, r_out) = s.W["sconv_w_in"], s.W["sconv_w_out"]
        s.load_small(s.cvw[:, 0:3 * DC], s.scw_in, s.cv_r)
        s.norm(gs, sh)
        cols = []
        for dc in range(DC):
            cols += [(D + dc * 128, 128), (2 * D + dc * 128, 128)]

        def epi1(idx, ps, pr, w):
            dc, which = idx // 2, idx % 2
            tq, tr = s.tmp[dc % 2], s.tmp_r[dc % 2]
            if which == 0:
                emit(s.act, lambda e: e.activation(out=tq, in_=ps[:, 0:T], func=AF.Identity), pr, [tr])
            else:
                emit(s.dve, lambda e: e.tensor_tensor(out=s.midc(dc), in0=ps[:, 0:T], in1=tq, op=ALU.mult), pr + [tr], [s.mid_r[dc]])

        s.linear(w_in, r_in, 0, DC, cols, s.hx, s.hT_r, epi1, T)
        s.halo_exchange("sc")

        def epi2(dc, ps, pr, w):
            tq, tr = s.tmp[dc % 2], [s.tmp_r[dc % 2]]
            s.dwconv(dc, s.cvw, 3, tq, tr)
            emit(s.dve, lambda e: e.tensor_tensor(out=s.midc(dc), in0=ps[:, 0:T], in1=tq, op=ALU.mult), pr + tr, [s.mid_r[dc]])

        s.linear(w_in, r_in, 0, DC, [(dc * 128, 128) for dc in range(DC)], s.hx, s.hT_r, epi2, T)
        pre, epi3 = s.resid(gate)
        s.linear(w_out, r_out, 0, DC, [(d * 128, 128) for d in range(DC)], s.midx, s.mid_r, epi3, T, pre=pre)

    def conformer(s, gs, sh, gate):
        c = s.c
        D, DC, T = c.D, c.DC, c.T
        (w_in, r_in), (w_out, r_out) = s.W["conf_w_in"], s.W["conf_w_out"]
        s.load_small(s.cvw[:, 0:31 * DC], s.cfw_in, s.cv_r)
        s.load_small(s.cvv[:, 0:3 * DC], s.cfv_in, s.cv_r)
        s.norm(gs, sh)
        cols = []
        for dc in range(DC):
            cols += [(D + dc * 128, 128), (dc * 128, 128)]

        def epi1(idx, ps, pr, w):
            dc, which = idx // 2, idx % 2
            tq, tr = s.tmp[dc % 2], s.tmp_r[dc % 2]
            if which == 0:
                emit(s.act, lambda e: e.activation(out=tq, in_=ps[:, 0:T], func=AF.Sigmoid), pr, [tr])
            else:
                emit(s.dve, lambda e: e.tensor_tensor(out=s.midc(dc), in0=ps[:, 0:T], in1=tq, op=ALU.mult), pr + [tr], [s.mid_r[dc]])

        s.linear(w_in, r_in, 0, DC, cols, s.hx, s.hT_r, epi1, T)
        s.halo_exchange("cf")
        acc1, acc2 = s.st[0], s.st[1]
        a1r, a2r = [s.st_r[0]], [s.st_r[1]]
        for dc in range(DC):
            tq, tr = s.tmp[dc % 2], [s.tmp_r[dc % 2]]
            s.dwconv(dc, s.cvw, 31, tq, tr, bias=s.cvv[:, 0:DC])
            sq, sqr = s.next_xt()
            emit(s.act, lambda e: e.activation(out=sq, in_=tq, func=AF.Square), tr, [sqr])
            emit(s.act, lambda e: e.activation(out=s.hT[:, dc, :], in_=tq, func=AF.Identity), tr, [s.hT_r[dc]])
            if dc == 0:
                emit(s.dve, lambda e: e.tensor_copy(out=acc1, in_=tq), tr, a1r)
                emit(s.dve, lambda e: e.tensor_copy(out=acc2, in_=sq), [sqr], a2r)
            else:
                emit(s.dve, lambda e: e.tensor_tensor(out=acc1, in0=acc1, in1=tq, op=ALU.add), tr + a1r, a1r)
                emit(s.dve, lambda e: e.tensor_tensor(out=acc2, in0=acc2, in1=sq, op=ALU.add), [sqr] + a2r, a2r)
        mean, mr = s.xt[0], [s.xt_r[0]]
        rstd, rr = s.xt[1], [s.xt_r[1]]
        s.colsum_bc(acc1, a1r, mean, mr, 1.0 / D, 0.0, None)
        s.colsum_bc(acc2, a2r, rstd, rr, 1.0 / D, 0.0, None)
        m2, m2r = s.px, [s.px_r]
        emit(s.dve, lambda e: e.tensor_tensor(out=m2, in0=mean, in1=mean, op=ALU.mult), mr, m2r)
        emit(s.dve, lambda e: e.tensor_tensor(out=rstd, in0=rstd, in1=m2, op=ALU.subtract), rr + m2r, rr)
        emit(s.dve, lambda e: e.tensor_scalar(out=rstd, in0=rstd, scalar1=1e-6, scalar2=None, op0=ALU.add), rr, rr)
        s.rpow(rstd, rr, -0.5)
        for dc in range(DC):
            tq, tr = s.tmp[dc % 2], [s.tmp_r[dc % 2]]
            emit(s.dve, lambda e: e.tensor_tensor(out=tq, in0=s.hT[:, dc, :], in1=mean, op=ALU.subtract), [s.hT_r[dc]] + mr, tr)
            emit(s.dve, lambda e: e.tensor_tensor(out=tq, in0=tq, in1=rstd, op=ALU.mult), tr + rr, tr)
            emit(s.act, lambda e: e.activation(out=s.midc(dc), in_=tq, func=AF.Silu, bias=s.cvv[:, 2 * DC + dc:2 * DC + dc + 1],
                                               scale=s.cvv[:, DC + dc:DC + dc + 1]), tr + [s.cv_r], [s.mid_r[dc]])
        pre, epi3 = s.resid(gate)
        s.linear(w_out, r_out, 0, DC, [(d * 128, 128) for d in range(DC)], s.midx, s.mid_r, epi3, T, pre=pre)

    def rglru(s, gs, sh, gate):
        c = s.c
        D, DC, T = c.D, c.DC, c.T
        CH = c.LB // 128
        (w_in, r_in), (w_out, r_out) = s.W["lru_w_in"], s.W["lru_w_out"]
        (w_a, rw_a), (w_x, rw_x) = s.W["lru_w_a"], s.W["lru_w_x"]
        s.load_small(s.cvw[:, 0:4 * DC], s.lrw_in, s.cv_r)
        s.load_small(s.cvv[:, 0:4 * DC], s.lrv_in, s.cv_r)
        vec, vr = s.vec, [s.vec_r]
        emit(s.act, lambda e: e.activation(out=vec[:, 0:DC], in_=s.cvv[:, 3 * DC:4 * DC], func=AF.Exp, scale=-1.0), [s.cv_r], vr)
        emit(s.dve, lambda e: e.tensor_scalar(out=vec[:, 0:DC], in0=vec[:, 0:DC], scalar1=1.0, scalar2=None, op0=ALU.add), vr, vr)
        emit(s.act, lambda e: e.activation(out=vec[:, 0:DC], in_=vec[:, 0:DC], func=AF.Ln), vr, vr)
        emit(s.dve, lambda e: e.tensor_scalar(out=vec[:, DC:2 * DC], in0=vec[:, 0:DC], scalar1=-16.0, scalar2=None, op0=ALU.mult), vr, vr)
        emit(s.dve, lambda e: e.tensor_scalar(out=vec[:, 0:DC], in0=vec[:, 0:DC], scalar1=-8.0, scalar2=None, op0=ALU.mult), vr, vr)
        s.chk(1, "lru")
        s.norm(gs, sh)

        def epi1(dc, ps, pr, w):
            emit(s.act, lambda e: e.activation(out=s.midc(dc), in_=ps[:, 0:T], func=AF.Identity), pr, [s.mid_r[dc]])

        s.linear(w_in, r_in, 0, DC, [(D + dc * 128, 128) for dc in range(DC)], s.hx, s.hT_r, epi1, T)
        s.chk(2, "lru")
        s.halo_exchange("lr")
        for dc in range(DC):
            tq, tr = s.tmp[dc % 2], [s.tmp_r[dc % 2]]
            s.dwconv(dc, s.cvw, 4, tq, tr, bias=s.cvv[:, 0:DC])
            emit(s.act, lambda e: e.activation(out=s.midc(dc), in_=tq, func=AF.Identity), tr, [s.mid_r[dc]])

        t_r, t_a2, t_a, t_u, t_g = s.xt[0], s.xt[1], s.tmp[0], s.tmp[1], s.st[0]
        r_r, r_a2, r_a, r_u, r_g = [s.xt_r[0]], [s.xt_r[1]], [s.tmp_r[0]], [s.tmp_r[1]], [s.st_r[0]]
        ob = s.st[1].bitcast(BF16)
        ob_r = [s.st_r[1]]
        assert CH <= 2

        def lru_pass(final_only):
            for hh in range(c.LH):
                for jc in range(CH):
                    j = hh * CH + jc
                    xfn = lambda kc, n0, n1, hh=hh: s.mid[:, hh * CH + kc, HALO + n0:HALO + n1]
                    xres = s.mid_r[hh * CH:(hh + 1) * CH]

                    def epi_r(_, ps, pr, w):
                        emit(s.act, lambda e: e.activation(out=t_r, in_=ps[:, 0:T], func=AF.Sigmoid, bias=s.cvv[:, DC + j:DC + j + 1], scale=1.0),
                             pr + [s.cv_r], r_r)
                        emit(s.act, lambda e: e.activation(out=t_a, in_=t_r, func=AF.Exp, scale=vec[:, j:j + 1]), r_r + vr, r_a)
                        emit(s.act, lambda e: e.activation(out=t_a2, in_=t_r, func=AF.Exp, scale=vec[:, DC + j:DC + j + 1]), r_r + vr, r_a2)
                        emit(s.dve, lambda e: e.tensor_scalar(out=t_a2, in0=t_a2, scalar1=-1.0, scalar2=1.0, op0=ALU.mult, op1=ALU.add), r_a2, r_a2)
                        emit(s.act, lambda e: e.activation(out=t_a2, in_=t_a2, func=AF.Sqrt), r_a2, r_a2)

                    def epi_i(_, ps, pr, w):
                        emit(s.act, lambda e: e.activation(out=t_u, in_=ps[:, 0:T], func=AF.Sigmoid, bias=s.cvv[:, 2 * DC + j:2 * DC + j + 1], scale=1.0),
                             pr + [s.cv_r], r_u)
                        emit(s.dve, lambda e: e.tensor_tensor(out=t_u, in0=t_u, in1=s.midc(j), op=ALU.mult), r_u + [s.mid_r[j]], r_u)
                        emit(s.dve, lambda e: e.tensor_tensor(out=t_u, in0=t_u, in1=t_a2, op=ALU.mult), r_u + r_a2, r_u)
                        init = 0.0 if final_only else vec[:, 3 * DC + j:3 * DC + j + 1]
                        emit(s.dve, lambda e: e.tensor_tensor_scan(out=t_r, data0=t_a, data1=t_u, initial=init, op0=ALU.mult, op1=ALU.add),
                             r_a + r_u + vr + r_r, r_r)
                        if final_only:
                            emit(s.dve, lambda e: e.tensor_copy(out=vec[:, 2 * DC + j:2 * DC + j + 1], in_=t_r[:, T - 1:T]), r_r + vr, vr)

                    s.linear(w_a, rw_a, hh * c.LB, CH, [(jc * 128, 128)], xfn, xres, epi_r, T)
                    s.linear(w_x, rw_x, hh * c.LB, CH, [(jc * 128, 128)], xfn, xres, epi_i, T)
                    if not final_only:
                        def epi_g(_, ps, pr, w):
                            emit(s.act, lambda e: e.activation(out=t_g, in_=ps[:, 0:T], func=AF.Square), pr, r_g)
                            emit(s.dve, lambda e: e.tensor_scalar(out=t_g, in0=t_g, scalar1=0.044715, scalar2=1.0, op0=ALU.mult, op1=ALU.add), r_g, r_g)
                            emit(s.dve, lambda e: e.tensor_tensor(out=t_g, in0=t_g, in1=ps[:, 0:T], op=ALU.mult), r_g + pr, r_g)
                            emit(s.act, lambda e: e.activation(out=t_g, in_=t_g, func=AF.Sigmoid, scale=1.5957691216057308), r_g, r_g)
                            emit(s.dve, lambda e: e.tensor_tensor(out=t_g, in0=t_g, in1=ps[:, 0:T], op=ALU.mult), r_g + pr, r_g)
                            emit(s.dve, lambda e: e.tensor_tensor(out=ob[:, jc * T:(jc + 1) * T], in0=t_g, in1=t_r, op=ALU.mult), r_g + r_r, ob_r)
                        s.linear(w_in, r_in, 0, DC, [(j * 128, 128)], s.hx, s.hT_r, epi_g, T)
                if not final_only:
                    for jc in range(CH):
                        j = hh * CH + jc
                        emit(s.act, lambda e, j=j, jc=jc: e.activation(out=s.midc(j), in_=ob[:, jc * T:(jc + 1) * T], func=AF.Identity), ob_r, [s.mid_r[j]])

        s.chk(3, "lru")
        lru_pass(True)
        s.chk(4, "lru")
        loc = s.dram("lru_l", [128, DC], F32)
        pair = s.dram("lru_p", [256, DC], F32)
        rl, rp = Res("ll"), Res("lp")
        emit(s.pq, lambda e: e.dma_start(out=loc, in_=vec[:, 2 * DC:3 * DC]), vr, [rl])
        s.pair_exchange(loc, rl, pair, rp)
        emit(s.sq, lambda e: e.dma_start(out=vec[:, 3 * DC:4 * DC], in_=pair[0:128, :]), [rp], vr)
        emit(s.dve, lambda e: e.tensor_scalar(out=vec[:, 3 * DC:4 * DC], in0=vec[:, 3 * DC:4 * DC], scalar1=s.flag[:, 0:1], scalar2=None, op0=ALU.mult),
             vr + [s.const_r], vr)
        s.chk(5, "lru")
        lru_pass(False)
        s.chk(6, "lru")
        pre, epi3 = s.resid(gate)
        s.linear(w_out, r_out, 0, DC, [(d * 128, 128) for d in range(DC)], s.midx, s.mid_r, epi3, T, pre=pre)

    def attention(s, gs, sh, gate):
        c = s.c
        D, DC, T, S, TB = c.D, c.DC, c.T, c.S, c.TB
        QC, KVC, NH, IH = c.QL // 128, c.KVL // 128, c.NH, c.IH
        SB = S // 128
        W = s.W
        (w_in, r_in), (w_qidx, r_qidx), (w_uq, r_uq) = W["att_w_in"], W["att_w_qidx"], W["att_w_uq"]
        (w_ukT, r_ukT), (w_uv, r_uv), (w_out, r_out) = W["att_w_ukT"], W["att_w_uv"], W["att_w_out"]
        s.load_small(s.cvv[:, 0:QC], s.gcq_in, s.cv_r)
        s.load_small(s.cvv[:, QC:QC + KVC], s.gckv_in, s.cv_r)

        if s.full:
            midflat = s.mid[:, :, :].rearrange("p a b -> p (a b)")
            ohs = midflat[:, 0:2 * 32 * 128]
            ohs_r = s.mid_r
            rbb = s.st[0]
            rbb_r = [s.st_r[0]]
        else:
            ohs = s.sb("ohs_sb", [128, 2 * 32 * 128], BF16)[:, :]
            ohs_r = [Res("ohs")]
            rbb = s.sb("rbb_sb", [128, 32 * NH], F32)[:, :]
            rbb_r = [Res("rbb")]
        emit(s.pq, lambda e: e.dma_start(out=ohs, in_=s.ohs_in), [], ohs_r)
        emit(s.sq, lambda e: e.dma_start(out=rbb[:, 0:32 * NH], in_=s.rbb_in), [], rbb_r)
        assert NH <= 32
        negb = s.gb[:, 0:NH]
        nb_r = [s.gb_r]
        emit(s.dve, lambda e: e.tensor_scalar(out=negb, in0=rbb[:, 31 * NH:32 * NH], scalar1=-1.0, scalar2=None, op0=ALU.mult), rbb_r, nb_r)
        eacc = s.tmp[0][:, 0:128]
        ea_r = [s.tmp_r[0]]
        for h in range(NH):
            for dl in range(2):
                for b in range(32):
                    src = ohs[:, (dl * 32 + b) * 128:(dl * 32 + b + 1) * 128]
                    sc = rbb[:, b * NH + h:b * NH + h + 1]
                    if b == 0:
                        emit(s.dve, lambda e, src=src, sc=sc: e.tensor_scalar(out=eacc, in0=src, scalar1=sc, scalar2=None, op0=ALU.mult),
                             ohs_r + rbb_r, ea_r)
                    else:
                        emit(s.dve, lambda e, src=src, sc=sc: e.scalar_tensor_tensor(out=eacc, in0=src, scalar=sc, in1=eacc, op0=ALU.mult, op1=ALU.add),
                             ohs_r + rbb_r + ea_r, ea_r)
                o = (h * 2 + dl) * 128
                emit(s.act, lambda e, o=o, h=h: e.activation(out=s.EB[:, o:o + 128], in_=eacc, func=AF.Exp, bias=negb[:, h:h + 1], scale=1.0),
                     ea_r + nb_r, [s.EB_r])

        s.chk(1)
        s.norm(gs, sh)
        s.chk(2)
        accq, aq_r = s.st[0], [s.st_r[0]]
        acck, ak_r = s.st[1], [s.st_r[1]]

        def epi_in(j, ps, pr, w):
            if j < QC + KVC:
                acc, ar = (accq, aq_r) if j < QC else (acck, ak_r)
                first = (j == 0) or (j == QC)
                if first:
                    emit(s.act, lambda e: e.activation(out=acc, in_=ps[:, 0:T], func=AF.Square), pr, ar)
                else:
                    tq, tr = s.tmp[j % 2], [s.tmp_r[j % 2]]
                    emit(s.act, lambda e: e.activation(out=tq, in_=ps[:, 0:T], func=AF.Square), pr, tr)
                    emit(s.dve, lambda e: e.tensor_tensor(out=acc, in0=acc, in1=tq, op=ALU.add), tr + ar, ar)
            emit(s.act, lambda e: e.activation(out=s.midc(j), in_=ps[:, 0:T], func=AF.Identity), pr, [s.mid_r[j]])

        s.linear(w_in, r_in, 0, DC, [(j * 128, 128) for j in range(QC + KVC + 1)], s.hx, s.hT_r, epi_in, T)

        s.chk(3)
        if s.full:
            hflat = s.hT[:, :, :].rearrange("p a b -> p (a b)")
        else:
            hflat = s.sb("att_scratch", [128, QC * T + KVC * S + SB * c.KVL + S + 16 * 128 + TB * IH * 2 + KVC * 256 + 128 + 256 + 64], BF16)[:, :]
        off = [0]

        def carve(n):
            a = hflat[:, off[0]:off[0] + n]
            off[0] += n
            return a

        assert TB * IH <= 31 * DC
        wtok = s.cvw[:, 0:TB * IH]
        wtok_r = [s.cv_r]
        sl = s.wcount % len(s.wt)
        s.wcount += 1
        wt, wr = s.wt[sl], s.wt_r[sl]
        wcol = c.QL + c.KVL + 128
        emit(s.sq, lambda e: e.dma_start(out=wt[:, 0:DC, 0:IH], in_=w_in[:, wcol:wcol + IH].rearrange("(kc p) f -> p kc f", p=128)), list(r_in), [wr])
        for tb in range(TB):
            ps, pr = s.bank[7], [s.bank_r[7]]
            for kc in range(DC):
                emit(s.pe, lambda e, tb=tb, kc=kc: e.matmul(ps[:, 0:IH], lhsT=s.hT[:, kc, tb * 128:(tb + 1) * 128], rhs=wt[:, kc, 0:IH],
                                                            start=(kc == 0), stop=(kc == DC - 1)), [wr, s.hT_r[kc]], pr)
            emit(s.dve, lambda e, tb=tb: e.tensor_copy(out=wtok[:, tb * IH:(tb + 1) * IH], in_=ps[:, 0:IH]), pr, wtok_r)

        cqn = carve(QC * T)
        ckv = carve(KVC * S)
        kvt = carve(SB * c.KVL)
        kidx = carve(S)
        GI = min(16, IH)
        qig = carve(GI * 128)
        qh = carve(128)
        qlat = carve(KVC * 128)
        olat = carve(KVC * 128)
        views = {n: [Res(n)] for n in ("cqn", "ckv", "kvt", "kidx", "qig", "qh", "qlat", "olat")}
        if s.full:
            base = []
            for r in s.hT_r:
                base += r.r + ([r.w] if r.w is not None else [])
            for v in views.values():
                v[0].r = list(base)
        rinv = carve(256).bitcast(F32)
        rinv_r = [Res("rinv")]
        views["rinv"] = rinv_r
        if s.full:
            rinv_r[0].r = list(base)

        rq, rq_r = s.xt[0], [s.xt_r[0]]
        rk, rk_r = s.xt[1], [s.xt_r[1]]
        s.colsum_bc(accq, aq_r, rq, rq_r, 1.0 / c.QL, 1e-6, -0.5)
        s.colsum_bc(acck, ak_r, rk, rk_r, 1.0 / c.KVL, 1e-6, -0.5)
        for j in range(QC + KVC):
            tq, tr = s.tmp[j % 2], [s.tmp_r[j % 2]]
            rs, rs_r = (rq, rq_r) if j < QC else (rk, rk_r)
            emit(s.dve, lambda e: e.tensor_tensor(out=tq, in0=s.midc(j), in1=rs, op=ALU.mult), [s.mid_r[j]] + rs_r, tr)
            if j < QC:
                dst, dr = cqn[:, j * T:(j + 1) * T], views["cqn"]
            else:
                cc = j - QC
                dst, dr = ckv[:, cc * S + T:cc * S + 2 * T], views["ckv"]
            emit(s.act, lambda e, dst=dst: e.activation(out=dst, in_=tq, func=AF.Identity, scale=s.cvv[:, j:j + 1]), tr + [s.cv_r], dr)
        emit(s.dve, lambda e: e.tensor_copy(out=kidx[:, T:2 * T], in_=s.midc(QC + KVC)), [s.mid_r[QC + KVC]], views["kidx"])

        s.chk(4)
        NR = (KVC + 1) * 128
        loc = s.dram("kv_l", [NR, T], BF16)
        pair = s.dram("kv_p", [2 * NR, T], BF16)
        rl, rp = Res("kvl"), Res("kvp")
        ckv3 = ckv.rearrange("p (a b) -> p a b", b=S)
        emit(s.pq, lambda e: e.dma_start(out=loc[0:KVC * 128, :].rearrange("(cc p) t -> p cc t", p=128), in_=ckv3[:, :, T:2 * T]), views["ckv"], [rl])
        emit(s.pq, lambda e: e.dma_start(out=loc[KVC * 128:NR, :], in_=kidx[:, T:2 * T]), views["kidx"], [rl])
        s.pair_exchange(loc, rl, pair, rp)
        emit(s.sq, lambda e: e.dma_start(out=ckv3[:, :, 0:T], in_=pair[0:KVC * 128, :].rearrange("(cc p) t -> p cc t", p=128)), [rp], views["ckv"])
        emit(s.sq, lambda e: e.dma_start(out=kidx[:, 0:T], in_=pair[KVC * 128:NR, :]), [rp], views["kidx"])

        s.chk(5)
        for sb_ in range(SB):
            ps, pr = s.bank[6 + sb_ % 2], [s.bank_r[6 + sb_ % 2]]
            for cc in range(KVC):
                emit(s.pe, lambda e, ps=ps, cc=cc, sb_=sb_: e.matmul(ps[:, cc * 128:(cc + 1) * 128], lhsT=ckv[:, cc * S + sb_ * 128:cc * S + (sb_ + 1) * 128],
                                                                  rhs=s.identb[:, :], start=True, stop=True), views["ckv"] + [s.const_r], pr)
            emit(s.act, lambda e, ps=ps, sb_=sb_: e.activation(out=kvt[:, sb_ * c.KVL:(sb_ + 1) * c.KVL], in_=ps[:, 0:c.KVL], func=AF.Identity), pr, views["kvt"])

        s.chk(6)
        big = s.psum[:, 0:2048]
        big_r = s.bank_r[0:4]
        Sc = s.scr[:, 0:S]
        Sc_r = [s.xt_r[0], s.xt_r[1]]
        Sw = s.scr[:, 2 * T:2 * T + S]
        Sw_r = [s.tmp_r[0], s.tmp_r[1]]
        Mb = s.scr[:, 2 * T:3 * T].bitcast(BF16)
        PTs = [s.st[0].bitcast(BF16), s.px.bitcast(BF16)]
        PT_rs = [[s.st_r[0]], [s.px_r]]
        MT = s.st[1].bitcast(BF16)
        MT_r = [s.st_r[1]]
        m8, thr = s.sm[0], s.sm[1]
        sm_r = [s.sm_r]
        scale = 128 ** -0.5

        for qb in range(TB):
            NKC = TB + qb + 1
            NK = NKC * 128
            t0 = qb * 128
            xq = lambda kc, n0, n1: cqn[:, kc * T + t0 + n0:kc * T + t0 + n1]
            first = True
            for g0 in range(0, IH, GI):
                def epi_qi(jl, ps, pr, w):
                    emit(s.act, lambda e: e.activation(out=qig[:, jl * 128:(jl + 1) * 128], in_=ps[:, 0:128], func=AF.Identity), pr, views["qig"])
                s.linear(w_qidx, r_qidx, 0, QC, [((g0 + jl) * 128, 128) for jl in range(GI)], xq, views["cqn"], epi_qi, 128)
                for jl in range(GI):
                    hh = g0 + jl
                    for n0 in range(0, NK, 512):
                        n1 = min(NK, n0 + 512)
                        emit(s.pe, lambda e, jl=jl, n0=n0, n1=n1: e.matmul(big[:, n0:n1], lhsT=qig[:, jl * 128:(jl + 1) * 128], rhs=kidx[:, n0:n1],
                                                                          start=True, stop=True), views["qig"] + views["kidx"], big_r)
                    emit(s.act, lambda e: e.activation(out=Sw[:, 0:NK], in_=big[:, 0:NK], func=AF.Relu), big_r, Sw_r)
                    wsc = wtok[:, qb * IH + hh:qb * IH + hh + 1]
                    if first:
                        emit(s.dve, lambda e, wsc=wsc: e.tensor_scalar(out=Sc[:, 0:NK], in0=Sw[:, 0:NK], scalar1=wsc, scalar2=None, op0=ALU.mult),
                             Sw_r + wtok_r, Sc_r)
                        first = False
                    else:
                        emit(s.dve, lambda e, wsc=wsc: e.scalar_tensor_tensor(out=Sc[:, 0:NK], in0=Sw[:, 0:NK], scalar=wsc, in1=Sc[:, 0:NK],
                                                                             op0=ALU.mult, op1=ALU.add), Sw_r + wtok_r + Sc_r, Sc_r)
            s.chk(7)
            emit(s.dve, lambda e: e.tensor_scalar(out=Sc[:, 0:T], in0=Sc[:, 0:T], scalar1=s.flag[:, 1:2], scalar2=None, op0=ALU.add), Sc_r + [s.const_r], Sc_r)
            emit(s.dve, lambda e: e.tensor_tensor(out=Sc[:, NK - 128:NK], in0=Sc[:, NK - 128:NK], in1=s.tri[:, :], op=ALU.add), Sc_r + [s.const_r], Sc_r)
            rounds = c.KSEL // 8
            for r in range(rounds):
                srcv = Sc if r == 0 else Sw
                src_r = Sc_r if r == 0 else Sw_r
                emit(s.dve, lambda e, srcv=srcv: e.max(out=m8[:, 0:8], in_=srcv[:, 0:NK]), src_r + sm_r, sm_r)
                if r < rounds - 1:
                    emit(s.dve, lambda e, srcv=srcv: e.match_replace(out=Sw[:, 0:NK], in_to_replace=m8[:, 0:8], in_values=srcv[:, 0:NK], imm_value=NEG),
                         src_r + sm_r + Sw_r, Sw_r)
            emit(s.dve, lambda e: e.tensor_scalar(out=thr[:, 0:1], in0=m8[:, 7:8], scalar1=-1.0e29, scalar2=None, op0=ALU.max), sm_r, sm_r)
            emit(s.dve, lambda e: e.tensor_scalar(out=Mb[:, 0:NK], in0=Sc[:, 0:NK], scalar1=thr[:, 0:1], scalar2=None, op0=ALU.is_ge), Sc_r + sm_r + Sw_r, Sw_r)
            s.chk(8)
            for j0 in range(0, NKC, 4):
                j1 = min(NKC, j0 + 4)
                ps, pr = s.bank[7], [s.bank_r[7]]
                for j in range(j0, j1):
                    emit(s.pe, lambda e, j=j, j0=j0: e.matmul(ps[:, (j - j0) * 128:(j - j0 + 1) * 128], lhsT=Mb[:, j * 128:(j + 1) * 128], rhs=s.identb[:, :],
                                                             start=True, stop=True), Sw_r + [s.const_r], pr)
                emit(s.act, lambda e, j0=j0, j1=j1: e.activation(out=MT[:, j0 * 128:j1 * 128], in_=ps[:, 0:(j1 - j0) * 128], func=AF.Identity), pr, MT_r)
            s.chk(9)
            for h in range(NH):
                PT, PT_r = PTs[h % 2], PT_rs[h % 2]

                def epi_q(_, ps, pr, w):
                    emit(s.act, lambda e: e.activation(out=qh, in_=ps[:, 0:128], func=AF.Identity), pr, views["qh"])
                s.linear(w_uq, r_uq, 0, QC, [(h * 128, 128)], xq, views["cqn"], epi_q, 128)
                sl = s.wcount % len(s.wt)
                s.wcount += 1
                wuk, wuk_r = s.wt[sl], [s.wt_r[sl]]
                emit(s.sq, lambda e, wuk=wuk: e.dma_start(out=wuk[:, 0:KVC, :],
                                                         in_=w_ukT[h * 128:(h + 1) * 128, :].rearrange("p (cc f) -> p cc f", f=128)), list(r_ukT), wuk_r)
                sl = s.wcount % len(s.wt)
                s.wcount += 1
                wuv, wuv_r = s.wt[sl], [s.wt_r[sl]]
                emit(s.sq, lambda e, wuv=wuv: e.dma_start(out=wuv[:, 0:KVC, :], in_=w_uv[:, h * 128:(h + 1) * 128].rearrange("(cc p) f -> p cc f", p=128)),
                     list(r_uv), wuv_r)
                ps6, pr6 = s.bank[6], [s.bank_r[6]]
                ps7, pr7 = s.bank[7], [s.bank_r[7]]
                for cc in range(KVC):
                    emit(s.pe, lambda e, cc=cc, wuk=wuk: e.matmul(ps6[:, cc * 128:(cc + 1) * 128], lhsT=wuk[:, cc, :], rhs=qh, start=True, stop=True),
                         wuk_r + views["qh"], pr6)
                emit(s.dve, lambda e: e.tensor_copy(out=qlat, in_=ps6[:, 0:KVC * 128]), pr6, views["qlat"])
                for j in range(NKC):
                    for cc in range(KVC):
                        emit(s.pe, lambda e, j=j, cc=cc: e.matmul(big[:, j * 128:(j + 1) * 128], lhsT=ckv[:, cc * S + j * 128:cc * S + (j + 1) * 128],
                                                                 rhs=qlat[:, cc * 128:(cc + 1) * 128], start=(cc == 0), stop=(cc == KVC - 1)),
                             views["ckv"] + views["qlat"], big_r)
                emit(s.act, lambda e, PT=PT: e.activation(out=PT[:, 0:NK], in_=big[:, 0:NK], func=AF.Exp, scale=scale), big_r, PT_r)
                emit(s.dve, lambda e, PT=PT: e.tensor_tensor(out=PT[:, 0:NK], in0=PT[:, 0:NK], in1=MT[:, 0:NK], op=ALU.mult), PT_r + MT_r, PT_r)
                emit(s.dve, lambda e, PT=PT, h=h: e.tensor_tensor(out=PT[:, NK - 256:NK], in0=PT[:, NK - 256:NK], in1=s.EB[:, h * 256:(h + 1) * 256], op=ALU.mult),
                     PT_r + [s.EB_r], PT_r)
                for j in range(NKC):
                    emit(s.pe, lambda e, j=j, PT=PT: e.matmul(ps7[:, 0:128], lhsT=s.onesb[:, :], rhs=PT[:, j * 128:(j + 1) * 128], start=(j == 0), stop=(j == NKC - 1)),
                         PT_r + [s.const_r], pr7)
                emit(s.dve, lambda e: e.reciprocal(out=rinv, in_=ps7[:, 0:128]), pr7, rinv_r)
                for cc in range(KVC):
                    for j in range(NKC):
                        emit(s.pe, lambda e, j=j, cc=cc, PT=PT: e.matmul(ps6[:, cc * 128:(cc + 1) * 128], lhsT=kvt[:, j * c.KVL + cc * 128:j * c.KVL + (cc + 1) * 128],
                                                                        rhs=PT[:, j * 128:(j + 1) * 128], start=(j == 0), stop=(j == NKC - 1)),
                             PT_r + views["kvt"], pr6)
                emit(s.act, lambda e: e.activation(out=olat, in_=ps6[:, 0:KVC * 128], func=AF.Identity), pr6, views["olat"])
                for cc in range(KVC):
                    emit(s.pe, lambda e, cc=cc, wuv=wuv: e.matmul(ps7[:, 128:256], lhsT=wuv[:, cc, :], rhs=olat[:, cc * 128:(cc + 1) * 128],
                                                                 start=(cc == 0), stop=(cc == KVC - 1)), wuv_r + views["olat"], pr7)
                emit(s.dve, lambda e, h=h: e.tensor_tensor(out=s.mid[:, h, HALO + t0:HALO + t0 + 128], in0=ps7[:, 128:256], in1=rinv, op=ALU.mult),
                     pr7 + rinv_r, [s.mid_r[h]])
        s.chk(10)
        if s.full:
            allt = []
            for v in views.values():
                allt += v[0].r + ([v[0].w] if v[0].w is not None else [])
            for r in s.hT_r:
                r.r = r.r + allt
        pre, epi3 = s.resid(gate)
        s.linear(w_out, r_out, 0, c.AW // 128, [(d * 128, 128) for d in range(DC)], s.midx, s.mid_r[0:NH], epi3, T, pre=pre)


def _rel_bucket(dist):
    import math
    dist = np.maximum(dist, 0)
    max_exact = 16
    large = max_exact + (np.log(np.maximum(dist, 1).astype(np.float32) / max_exact) / math.log(128 / max_exact)
                         * (32 - max_exact)).astype(np.int32)
    large = np.minimum(large, 31)
    return np.where(dist < max_exact, dist, large)


def _pvec(v, DC):
    v = np.asarray(v, np.float32)
    lead = int(np.prod(v.shape[:-1])) if v.ndim > 1 else 1
    return np.ascontiguousarray(v.reshape(lead, DC, 128).transpose(2, 0, 1).reshape(128, lead * DC))


def make_in_maps(cfg, inp):
    c = cfg
    D, DC, T = c.D, c.DC, c.T
    f = lambda a: np.asarray(a, np.float32)
    shared = {}
    shared["cT"] = np.ascontiguousarray(f(inp["c"]).reshape(4, DC, 128).transpose(2, 1, 0).reshape(128, DC * 4))
    shared["ada_t"] = _pvec(f(inp["ada_table"]), DC)
    shared["norm_mix"] = _pvec(f(inp["norm_mix"]), DC)
    shared["norm_ffn"] = _pvec(f(inp["norm_ffn"]), DC)
    shared["norm_final"] = _pvec(f(inp["norm_final"]), DC)
    shared["ident"] = np.eye(128, dtype=np.float32)
    ss, tt = np.meshgrid(np.arange(128), np.arange(128), indexing="ij")
    shared["tri"] = np.where(ss.T >= tt.T, 0.0, 0.0).astype(np.float32)
    tq, sk = np.meshgrid(np.arange(128), np.arange(128), indexing="ij")
    shared["tri"] = np.where(sk <= tq, 0.0, NEG).astype(np.float32)
    ohs = np.zeros((128, 2, 32, 128), np.float32)
    s_i, t_i = np.meshgrid(np.arange(128), np.arange(128), indexing="ij")
    for dl in range(2):
        delta = 1 - dl
        bk = _rel_bucket(delta * 128 + t_i - s_i)
        for b in range(32):
            ohs[:, dl, b, :] = (bk == b)
    shared["ohs"] = ohs.reshape(128, -1)
    shared["rbb"] = np.ascontiguousarray(np.broadcast_to(f(inp["rel_bias"]).reshape(1, 32 * c.NH), (128, 32 * c.NH)))
    shared["g_cq"] = _pvec(f(inp["att_g_cq"])[0], c.QL // 128)
    shared["g_ckv"] = _pvec(f(inp["att_g_ckv"])[0], c.KVL // 128)
    shared["sconv_cw"] = _pvec(f(inp["sconv_conv_w"])[0], DC)
    shared["conf_cw"] = _pvec(f(inp["conf_conv_w"])[0], DC)
    shared["conf_vec"] = _pvec(np.stack([f(inp["conf_conv_b"])[0], f(inp["conf_ln_g"])[0], f(inp["conf_ln_b"])[0]]), DC)
    shared["lru_cw"] = _pvec(f(inp["lru_conv_w"])[0], DC)
    shared["lru_vec"] = _pvec(np.stack([f(inp["lru_conv_b"])[0], f(inp["lru_b_a"])[0], f(inp["lru_b_x"])[0], f(inp["lru_lambda"])[0]]), DC)
    rt = f(inp["moe_router"])
    shared["router"] = np.ascontiguousarray(rt.reshape(rt.shape[0], DC, 128, 8).transpose(2, 0, 1, 3).reshape(128, rt.shape[0] * DC * 8))
    if shared["router"].shape[1] < 2 * DC * 8:
        shared["router"] = np.concatenate([shared["router"], np.zeros((128, 2 * DC * 8 - shared["router"].shape[1]), np.float32)], 1)

    wfull = {
        "att_w_in": f(inp["att_w_in"])[0],
        "att_w_qidx": f(inp["att_w_qidx"])[0],
        "att_w_uq": f(inp["att_w_uq"])[0],
        "att_w_ukT": np.ascontiguousarray(f(inp["att_w_uk"])[0].transpose(1, 2, 0)).reshape(c.AW, c.KVL),
        "att_w_uv": f(inp["att_w_uv"])[0].reshape(c.KVL, c.AW),
        "att_w_out": f(inp["att_w_out"])[0],
        "sconv_w_in": f(inp["sconv_w_in"])[0], "sconv_w_out": f(inp["sconv_w_out"])[0],
        "conf_w_in": f(inp["conf_w_in"])[0], "conf_w_out": f(inp["conf_w_out"])[0],
        "lru_w_in": f(inp["lru_w_in"])[0], "lru_w_out": f(inp["lru_w_out"])[0],
        "lru_w_a": f(inp["lru_w_a"])[0].reshape(D, c.LB), "lru_w_x": f(inp["lru_w_x"])[0].reshape(D, c.LB),
    }
    for l in range(2):
        wfull[f"ffn_w13_{l}"] = f(inp["ffn_w13"])[l]
        wfull[f"ffn_w2_{l}"] = f(inp["ffn_w2"])[l]
        wfull[f"moe_w13_{l}"] = f(inp["moe_w13"])[l].reshape(c.NE * D, 2 * c.DFE)
        wfull[f"moe_w2_{l}"] = f(inp["moe_w2"])[l].reshape(c.NE * c.DFE, D)
    ada_w = f(inp["ada_w"])
    ada_b = f(inp["ada_b"])
    x = f(inp["x"])
    NCOL = 6 * D // 8
    NCL = 6 * DC // 8
    maps = []
    for core in range(8):
        b, half = core // 2, core % 2
        m = dict(shared)
        m["xT"] = np.ascontiguousarray(x[b, half * T:(half + 1) * T, :].T)
        m["ada_w"] = np.ascontiguousarray(ada_w[:, core * NCOL:(core + 1) * NCOL])
        m["ada_b"] = np.ascontiguousarray(ada_b[core * NCOL:(core + 1) * NCOL].reshape(NCL, 128).T)
        bs = np.zeros((128, 4), np.float32)
        bs[:, b] = 1.0
        m["bsel"] = bs
        fl = np.zeros((128, 2), np.float32)
        fl[:, 0] = float(half)
        fl[:, 1] = 0.0 if half == 1 else NEG
        m["flag"] = fl
        for name, w in wfull.items():
            m[name] = w
        maps.append(m)
    return maps


_CACHE = {}


def run(cfg, inp, stop_after=None, only=None):
    key = (cfg.D, cfg.SEQ, stop_after, tuple(sorted(only)) if only else None)
    if key not in _CACHE:
        bld = B(cfg, stop_after, only)
        _CACHE[key] = (bld.build(), bld)
    nc = _CACHE[key][0]
    maps = make_in_maps(cfg, inp)
    res = run_bass_kernel_spmd(nc, maps, core_ids=list(range(8)))
    out = np.zeros((4, cfg.SEQ, cfg.D), np.float32)
    global LAST
    LAST = (res, _CACHE.get(key))
    for core in range(8):
        b, half = core // 2, core % 2
        out[b, half * cfg.T:(half + 1) * cfg.T, :] = np.asarray(res.results[core]["outT"]).T
    return out


def kernel(**inputs):
    return run(Cfg(), inputs)
```

```python
import types
import numpy as np
import ml_dtypes
from contextlib import ExitStack
import concourse.bass as bass
import concourse.mybir as mybir
from concourse.bass_utils import run_bass_kernel_spmd

F32 = mybir.dt.float32
BF16 = mybir.dt.bfloat16
AF = mybir.ActivationFunctionType
ALU = mybir.AluOpType
AX = mybir.AxisListType
NEG = -1.0e30
DEBUG = False
HALO = 32


class Cfg:
    def __init__(s, D=4096, SEQ=2048, NH=32, QL=1024, KVL=512, IH=64, TOPK=256, LH=16, DFF=8192, NE=8,
                 DFE=2048, DEPTH=4):
        s.D, s.SEQ, s.NH, s.QL, s.KVL, s.IH, s.LH, s.DFF, s.NE, s.DFE, s.DEPTH = D, SEQ, NH, QL, KVL, IH, LH, DFF, NE, DFE, DEPTH
        s.T = SEQ // 2
        s.S = SEQ
        s.KSEL = min(TOPK, SEQ // 4)
        s.DC = D // 128
        s.AW = NH * 128
        s.ATT_IN = QL + KVL + 128 + IH
        s.LB = D // LH
        s.TT = min(512, s.T)
        s.NT = s.T // s.TT
        s.TB = s.T // 128


class StopBuild(Exception):
    pass


class Res:
    def __init__(s, name):
        s.name, s.w, s.r = name, None, []


class Q:
    def __init__(s, nc, name, sems, is_dma):
        s.nc, s.name, s.sems, s.is_dma = nc, name, sems, is_dma
        s.n = 0
        s.seen = {}
        s.prog = []

    def wait_tok(s, tok):
        kind = tok[0]
        if kind == "c":
            _, F, n = tok
            if F is s and s.name == "pe":
                return
            if s.seen.get(F.name, 0) >= n:
                return
            s.seen[F.name] = n
            sem = F.sems[0]
            s.prog.append(lambda e, sem=sem, n=n: e.wait_ge(sem, n))
        else:
            _, sem, val, key = tok
            if s.seen.get(key, 0) >= val:
                return
            s.seen[key] = val
            s.prog.append(lambda e, sem=sem, val=val: e.wait_ge(sem, val))

    def issue(s, fn, own_sem=None):
        if own_sem is not None:
            s.prog.append(lambda e, fn=fn, sem=own_sem: fn(e).then_inc(sem))
            tok = ("d", own_sem, 1, id(own_sem))
            s.cc_toks = getattr(s, "cc_toks", []) + [tok]
            return tok
        if s.is_dma:
            K = len(s.sems)
            i = s.n
            s.n += 1
            slot = i % K
            sem = s.sems[slot]
            if i >= K:
                prev = 16 * (i // K)
                key = (s.name, slot)
                if s.seen.get(key, 0) < prev:
                    s.seen[key] = prev
                    s.prog.append(lambda e, sem=sem, prev=prev: e.wait_ge(sem, prev))
            s.prog.append(lambda e, fn=fn, sem=sem: fn(e).then_inc(sem, 16))
            return ("d", sem, 16 * (i // K + 1), (s.name, slot))
        s.n += 1
        sem = s.sems[0]
        s.prog.append(lambda e, fn=fn, sem=sem: fn(e).then_inc(sem, 1))
        return ("c", s, s.n)


def _freeze(fn):
    if fn.__closure__ is None:
        return fn
    cells = []
    for cell in fn.__closure__:
        try:
            cells.append(types.CellType(cell.cell_contents))
        except ValueError:
            cells.append(cell)
    return types.FunctionType(fn.__code__, fn.__globals__, fn.__name__, fn.__defaults__, tuple(cells))


def emit(q, fn, reads=(), writes=(), own_sem=None):
    fn = _freeze(fn)
    deps = []
    for t in reads:
        if t.w is not None:
            deps.append(t.w)
    for t in writes:
        if t.w is not None:
            deps.append(t.w)
        deps.extend(t.r)
    for d in deps:
        q.wait_tok(d)
    tok = q.issue(fn, own_sem)
    for t in reads:
        if tok[0] == "c":
            t.r = [x for x in t.r if not (x[0] == "c" and x[1] is tok[1])]
        t.r.append(tok)
    for t in writes:
        t.w = tok
        t.r = []
    return tok


class B:
    def __init__(s, cfg, stop_after=None, only=None):
        s.c = cfg
        s.stop_after = stop_after
        s.only = only
        s.debug = DEBUG
        s.nc = bass.Bass("TRN2", target_bir_lowering=False)
        s.es = ExitStack()
        s.din = {}
        s.nsem = 0

    def sem(s):
        s.nsem += 1
        return s.es.enter_context(s.nc.semaphore(f"s{s.nsem}"))

    def inp(s, name, shape, dt=F32):
        h = s.nc.dram_tensor(name, list(shape), dt, kind="ExternalInput").ap()
        s.din[name] = (tuple(shape), dt)
        return h

    def dram(s, name, shape, dt):
        return s.nc.dram_tensor(name, list(shape), dt).ap()

    def sb(s, name, shape, dt):
        return s.es.enter_context(s.nc.sbuf_tensor("sb_" + name, list(shape), dt))

    def weight(s, name, K, Fd):
        src = s.inp(name, [K, Fd])
        full = s.dram(name + "_f", [K, Fd], BF16)
        step = max(1, min(K, (1 << 20) // Fd))
        chunks = [(r0, min(K, r0 + step)) for r0 in range(0, K, step)]
        rls = [Res(name + "_f") for _ in chunks]
        s.wts[name] = (src, full, chunks, rls)
        return full, (chunks, rls)

    def gather(s, name):
        src, full, chunks, rls = s.wts[name]
        for (r0, r1), rl in zip(chunks, rls):
            emit(s.pq, lambda e: e.dma_start(out=full[r0:r1, :], in_=src[r0:r1, :]), [], [rl])

    def gather2(s, loc, rls, full, rf, rows, Fd, dt, name):
        quad = s.dram(name + "_q", [4 * rows, Fd], dt)
        rq = Res(name + "_q")
        emit(s.pq, lambda e: e.collective_compute("AllGather", ALU.bypass, replica_groups=[[0, 1, 2, 3], [4, 5, 6, 7]],
                                                  ins=[loc.opt()], outs=[quad.opt()]), rls, [rq], own_sem=s.sem())
        emit(s.pq, lambda e: e.collective_compute("AllGather", ALU.bypass, replica_groups=[[0, 4], [1, 5], [2, 6], [3, 7]],
                                                  ins=[quad.opt()], outs=[full.opt()]), [rq], [rf], own_sem=s.sem())

    def pair_exchange(s, loc, rl, pair, rp):
        emit(s.pq, lambda e: e.collective_compute("AllGather", ALU.bypass, replica_groups=[[0, 1], [2, 3], [4, 5], [6, 7]],
                                                  ins=[loc.opt()], outs=[pair.opt()]), [rl], [rp], own_sem=s.sem())

    def linear(s, Wd, Wres, k0, KC, cols, xfn, xres, epi, N, pre=None):
        PF = len(s.wt) - 1
        n = len(cols)
        slots = {}
        xr = list(xres)
        Wres = [rl for (r0, r1), rl in zip(Wres[0], Wres[1]) if r0 < k0 + KC * 128 and r1 > k0]

        def load(j):
            c0, w = cols[j]
            sl = s.wcount % len(s.wt)
            s.wcount += 1
            slots[j] = sl
            wt, wr = s.wt[sl], s.wt_r[sl]
            emit(s.sq, lambda e, wt=wt, c0=c0, w=w: e.dma_start(
                out=wt[:, 0:KC, 0:w], in_=Wd[k0:k0 + KC * 128, c0:c0 + w].rearrange("(kc p) f -> p kc f", p=128)),
                list(Wres), [wr])
            if pre is not None:
                pre(j)

        for j in range(min(PF, n)):
            load(j)
        for j in range(n):
            if j + PF < n:
                load(j + PF)
            c0, w = cols[j]
            sl = slots[j]
            wt, wr = s.wt[sl], s.wt_r[sl]
            if N > 512:
                pi = s.pcount % 3
                ps = s.psum[:, pi * 1024:(pi + 1) * 1024]
                pr = [s.bank_r[2 * pi], s.bank_r[2 * pi + 1]]
            else:
                pi = 4 + s.pcount % 2
                ps = s.bank[pi]
                pr = [s.bank_r[pi]]
            s.pcount += 1
            for n0 in range(0, N, 512):
                n1 = min(N, n0 + 512)
                for kc in range(KC):
                    rhs = xfn(kc, n0, n1)
                    emit(s.pe, lambda e, ps=ps, wt=wt, kc=kc, w=w, n0=n0, n1=n1, rhs=rhs: e.matmul(
                        ps[0:w, n0:n1], lhsT=wt[:, kc, 0:w], rhs=rhs, start=(kc == 0), stop=(kc == KC - 1)),
                        [wr] + xr, pr)
            epi(j, ps, pr, w)

    def next_xt(s):
        sl = s.xcount % len(s.xt)
        s.xcount += 1
        return s.xt[sl], s.xt_r[sl]

    def resid(s, gate):
        c = s.c
        slots = {}

        def pre(j):
            xt, xr = s.next_xt()
            slots[j] = (xt, xr)
            emit(s.sq, lambda e: e.dma_start(out=xt, in_=s.xs[j * 128:(j + 1) * 128, :]), [s.xs_r[j]], [xr])

        def epi(j, ps, pr, w):
            xt, xr = slots[j]
            emit(s.dve, lambda e: e.scalar_tensor_tensor(out=xt, in0=ps[:, 0:c.T], scalar=gate[:, j:j + 1], in1=xt,
                                                         op0=ALU.mult, op1=ALU.add), pr + [xr, s.mod_r], [xr])
            emit(s.sq, lambda e: e.dma_start(out=s.xs[j * 128:(j + 1) * 128, :], in_=xt), [xr], [s.xs_r[j]])

        return pre, epi

    def colsum_bc(s, acc, acc_r, out, out_r, scale, eps, power):
        c = s.c
        ps, pr = s.bank[6], [s.bank_r[6]]
        for n0 in range(0, c.T, 512):
            n1 = min(c.T, n0 + 512)
            emit(s.pe, lambda e, n0=n0, n1=n1: e.matmul(ps[:, 0:n1 - n0], lhsT=s.ones32[:, :], rhs=acc[:, n0:n1], start=True, stop=True),
                 acc_r + [s.const_r], pr)
            emit(s.dve, lambda e, n0=n0, n1=n1: e.tensor_scalar(out=out[:, n0:n1], in0=ps[:, 0:n1 - n0], scalar1=scale, scalar2=eps,
                                                                  op0=ALU.mult, op1=ALU.add), pr, out_r)
        if power is not None:
            s.rpow(out, out_r, power)

    def rpow(s, out, out_r, power):
        emit(s.act, lambda e: e.activation(out=out, in_=out, func=AF.Ln), out_r, out_r)
        emit(s.act, lambda e: e.activation(out=out, in_=out, func=AF.Exp, scale=power), out_r, out_r)

    def norm(s, gs, sh, out_fn=None, hook=None):
        c = s.c
        acc, acc_r = s.st[0], [s.st_r[0]]
        for dc in range(c.DC):
            xt, xr = s.next_xt()
            emit(s.sq, lambda e: e.dma_start(out=xt, in_=s.xs[dc * 128:(dc + 1) * 128, :]), [s.xs_r[dc]], [xr])
            if dc == 0:
                emit(s.act, lambda e: e.activation(out=acc, in_=xt, func=AF.Square), [xr], acc_r)
            else:
                tq, tr = s.tmp[dc % 2], s.tmp_r[dc % 2]
                emit(s.act, lambda e: e.activation(out=tq, in_=xt, func=AF.Square), [xr], [tr])
                emit(s.dve, lambda e: e.tensor_tensor(out=acc, in0=acc, in1=tq, op=ALU.add), [tr] + acc_r, acc_r)
        rstd, rstd_r = s.st[1], [s.st_r[1]]
        s.colsum_bc(acc, acc_r, rstd, rstd_r, 1.0 / c.D, 1e-6, -0.5)
        if "rstd" not in s.dbg_map:
            s.dbg("acc", acc, acc_r, c.T)
            s.dbg("rstd", rstd, rstd_r, c.T)
        for dc in range(c.DC):
            xt, xr = s.next_xt()
            emit(s.sq, lambda e: e.dma_start(out=xt, in_=s.xs[dc * 128:(dc + 1) * 128, :]), [s.xs_r[dc]], [xr])
            emit(s.dve, lambda e: e.tensor_tensor(out=xt, in0=xt, in1=rstd, op=ALU.mult), [xr] + rstd_r, [xr])
            bias = sh[:, dc:dc + 1] if sh is not None else 0.0
            if out_fn is not None:
                out_fn(dc, xt, xr, gs)
            elif hook is not None:
                tq, tr = s.tmp[dc % 2], s.tmp_r[dc % 2]
                emit(s.act, lambda e: e.activation(out=tq, in_=xt, func=AF.Identity, bias=bias, scale=gs[:, dc:dc + 1]),
                     [xr, s.mod_r], [tr])
                emit(s.dve, lambda e: e.tensor_copy(out=s.hT[:, dc, :], in_=tq), [tr], [s.hT_r[dc]])
                hook(dc, tq, tr)
            else:
                emit(s.act, lambda e: e.activation(out=s.hT[:, dc, :], in_=xt, func=AF.Identity, bias=bias, scale=gs[:, dc:dc + 1]),
                     [xr, s.mod_r], [s.hT_r[dc]])
                if dc == 0 and "h0" not in s.dbg_map:
                    s.dbg("h0", s.hT[:, 0, :], [s.hT_r[0]], c.T)

    def hx(s, kc, n0, n1):
        return s.hT[:, kc, n0:n1]

    def midx(s, kc, n0, n1):
        return s.mid[:, kc, HALO + n0:HALO + n1]

    def midc(s, dc):
        return s.mid[:, dc, HALO:HALO + s.c.T]

    def halo_exchange(s, tag):
        c = s.c
        loc = s.dram(f"halo_l{tag}", [c.D, HALO], BF16)
        pair = s.dram(f"halo_p{tag}", [2 * c.D, HALO], BF16)
        rl, rp = Res("hl"), Res("hp")
        emit(s.pq, lambda e: e.dma_start(out=loc.rearrange("(dc p) h -> p dc h", p=128), in_=s.mid[:, :, c.T:c.T + HALO]), s.mid_r, [rl])
        s.pair_exchange(loc, rl, pair, rp)
        emit(s.sq, lambda e: e.dma_start(out=s.mid[:, :, 0:HALO], in_=pair[0:c.D, :].rearrange("(dc p) h -> p dc h", p=128)), [rp], s.mid_r)
        emit(s.dve, lambda e: e.tensor_scalar(out=s.mid[:, :, 0:HALO], in0=s.mid[:, :, 0:HALO], scalar1=s.flag[:, 0:1], scalar2=None, op0=ALU.mult),
             s.mid_r + [s.const_r], s.mid_r)

    def dwconv(s, dc, cw, K, out, out_r, bias=None):
        c = s.c
        for k in range(K):
            off = HALO - (K - 1) + k
            src = s.mid[:, dc, off:off + c.T]
            wk = cw[:, k * c.DC + dc:k * c.DC + dc + 1]
            if k == 0:
                if bias is not None:
                    emit(s.dve, lambda e, src=src, wk=wk: e.tensor_scalar(out=out, in0=src, scalar1=wk, scalar2=bias[:, dc:dc + 1],
                                                                        op0=ALU.mult, op1=ALU.add), [s.mid_r[dc], s.cv_r], out_r)
                else:
                    emit(s.dve, lambda e, src=src, wk=wk: e.tensor_scalar(out=out, in0=src, scalar1=wk, scalar2=None, op0=ALU.mult),
                         [s.mid_r[dc], s.cv_r], out_r)
            else:
                emit(s.dve, lambda e, src=src, wk=wk: e.scalar_tensor_tensor(out=out, in0=src, scalar=wk, in1=out,
                                                                           op0=ALU.mult, op1=ALU.add), [s.mid_r[dc], s.cv_r] + out_r, out_r)

    def load_small(s, dst, src, res):
        emit(s.sq, lambda e: e.dma_start(out=dst, in_=src), [], [res])

    def build(s):
        c = s.c
        nc = s.nc
        es = s.es
        D, T, DC = c.D, c.T, c.DC
        s.pe = Q(nc, "pe", [s.sem()], False)
        s.act = Q(nc, "act", [s.sem()], False)
        s.dve = Q(nc, "dve", [s.sem()], False)
        s.sq = Q(nc, "sq", [s.sem() for _ in range(8)], True)
        s.pq = Q(nc, "pq", [s.sem() for _ in range(8)], True)
        s.wts = {}
        s.wcount = s.pcount = s.xcount = 0

        xT_in = s.inp("xT", [D, T])
        out_d = nc.dram_tensor("outT", [D, T], F32, kind="ExternalOutput").ap()
        s.dbg_d = nc.dram_tensor("dbg", [128, 8192], F32, kind="ExternalOutput").ap() if s.debug else None
        s.dbg_off = 0
        s.dbg_map = {}
        cT_in = s.inp("cT", [128, DC * 4])
        adaw_in = s.inp("ada_w", [D, 6 * D // 8])
        adab_in = s.inp("ada_b", [128, 6 * DC // 8])
        adat_in = s.inp("ada_t", [128, c.DEPTH * 6 * DC])
        nmix_in = s.inp("norm_mix", [128, c.DEPTH * DC])
        nffn_in = s.inp("norm_ffn", [128, c.DEPTH * DC])
        nfin_in = s.inp("norm_final", [128, DC])
        bsel_in = s.inp("bsel", [128, 4])
        flag_in = s.inp("flag", [128, 2])
        ident_in = s.inp("ident", [128, 128])
        tri_in = s.inp("tri", [128, 128])
        s.ohs_in = s.inp("ohs", [128, 2 * 32 * 128])
        s.rbb_in = s.inp("rbb", [128, 32 * c.NH])
        s.gcq_in = s.inp("g_cq", [128, c.QL // 128])
        s.gckv_in = s.inp("g_ckv", [128, c.KVL // 128])
        s.scw_in = s.inp("sconv_cw", [128, 3 * DC])
        s.cfw_in = s.inp("conf_cw", [128, 31 * DC])
        s.cfv_in = s.inp("conf_vec", [128, 3 * DC])
        s.lrw_in = s.inp("lru_cw", [128, 4 * DC])
        s.lrv_in = s.inp("lru_vec", [128, 4 * DC])
        s.rt_in = s.inp("router", [128, 2 * DC * 8])

        s.xs = s.dram("xs", [D, T], F32)
        s.xs_r = [Res(f"xs{i}") for i in range(DC)]

        s.full = (c.D == 4096)
        s.hT = s.sb("hT", [128, DC, T], BF16)
        s.hT_r = [Res(f"hT{i}") for i in range(DC)]
        s.mid = s.sb("mid", [128, DC, T + HALO], BF16)
        s.mid_r = [Res(f"mid{i}") for i in range(DC)]
        NW = 2 if s.full else 3
        s.wt = [s.sb(f"wt{i}", [128, DC, 128], BF16) for i in range(NW)]
        s.wt_r = [Res(f"wt{i}") for i in range(NW)]
        s.scr = s.sb("scr", [128, 7 * T], F32)
        s.xt = [s.scr[:, i * T:(i + 1) * T] for i in range(2)]
        s.xt_r = [Res(f"xt{i}") for i in range(2)]
        s.tmp = [s.scr[:, (2 + i) * T:(3 + i) * T] for i in range(2)]
        s.tmp_r = [Res(f"tmp{i}") for i in range(2)]
        s.st = [s.scr[:, (4 + i) * T:(5 + i) * T] for i in range(2)]
        s.st_r = [Res(f"st{i}") for i in range(2)]
        s.px = s.scr[:, 6 * T:7 * T]
        s.px_r = Res("px")
        s.xt = s.xt + [s.px]
        s.xt_r = s.xt_r + [s.px_r]
        psum = es.enter_context(nc.psum_tensor("psum", [128, 4096], F32))
        s.bank = [psum[:, i * 512:(i + 1) * 512] for i in range(8)]
        s.bank_r = [Res(f"bank{i}") for i in range(8)]
        s.psum = psum
        s.const_r = Res("const")
        s.ones32 = s.sb("ones32", [128, 128], F32)
        s.ident32 = s.sb("ident32", [128, 128], F32)
        s.identb = s.sb("identb", [128, 128], BF16)
        s.onesb = s.sb("onesb", [128, 128], BF16)
        s.tri = s.sb("tri", [128, 128], F32)
        s.flag = s.sb("flag", [128, 2], F32)
        bsel = s.sb("bsel", [128, 4], F32)
        modsel = s.sb("modsel", [128, 6 * DC], F32)
        s.modL = s.sb("modL", [128, c.DEPTH * 6 * DC], F32)
        nmix = s.sb("nmix", [128, c.DEPTH * DC], F32)
        nffn = s.sb("nffn", [128, c.DEPTH * DC], F32)
        s.nfin = s.sb("nfin", [128, DC], F32)
        s.rtf = s.sb("rtf", [128, DC * 8], F32)
        s.rtf_r = Res("rtf")
        s.gtok = s.sb("gtok", [128, c.TB, 8], F32)
        s.gtok_r = Res("gtok")
        s.sm = [s.sb(f"sm{i}", [128, 16], F32) for i in range(3)]
        s.sm_r = Res("sm")
        s.gb = s.sb("gb", [128, 128], F32)
        s.gb_r = Res("gb")
        s.EB = s.sb("EB", [128, c.NH * 256], BF16)
        s.EB_r = Res("EB")
        s.cvw = s.sb("cvw", [128, 31 * DC], F32)
        s.cvv = s.sb("cvv", [128, 4 * DC], F32)
        s.cv_r = Res("cv")
        s.vec = s.sb("vec", [128, 4 * DC], F32)
        s.vec_r = Res("vec")
        s.mod_r = Res("modL")
        small_r = Res("small")

        for dst, src in ((s.ident32[:, :], ident_in), (s.tri[:, :], tri_in), (s.flag[:, :], flag_in), (bsel[:, :], bsel_in),
                         (s.modL[:, :], adat_in), (nmix[:, :], nmix_in), (nffn[:, :], nffn_in), (s.nfin[:, :], nfin_in)):
            s.load_small(dst, src, small_r)
        emit(s.dve, lambda e: e.memset(s.ones32[:, :], 1.0), [], [s.const_r])
        emit(s.dve, lambda e: e.memset(s.onesb[:, :], 1.0), [s.const_r], [s.const_r])
        emit(s.dve, lambda e: e.tensor_copy(out=s.identb[:, :], in_=s.ident32[:, :]), [small_r, s.const_r], [s.const_r])

        for dc in range(DC):
            emit(s.sq, lambda e, dc=dc: e.dma_start(out=s.xs[dc * 128:(dc + 1) * 128, :], in_=xT_in[dc * 128:(dc + 1) * 128, :]), [], [s.xs_r[dc]])

        W = {}
        s.W = W
        decls = [("att_w_in", D, c.ATT_IN), ("att_w_qidx", c.QL, c.IH * 128), ("att_w_uq", c.QL, c.AW), ("att_w_ukT", c.AW, c.KVL),
                 ("att_w_uv", c.KVL, c.AW), ("att_w_out", c.AW, D), ("ffn_w13_0", D, 2 * c.DFF), ("ffn_w2_0", c.DFF, D),
                 ("sconv_w_in", D, 3 * D), ("sconv_w_out", D, D), ("moe_w13_0", c.NE * D, 2 * c.DFE), ("moe_w2_0", c.NE * c.DFE, D),
                 ("conf_w_in", D, 2 * D), ("conf_w_out", D, D), ("ffn_w13_1", D, 2 * c.DFF), ("ffn_w2_1", c.DFF, D),
                 ("lru_w_in", D, 2 * D), ("lru_w_a", D, c.LB), ("lru_w_x", D, c.LB), ("lru_w_out", D, D),
                 ("moe_w13_1", c.NE * D, 2 * c.DFE), ("moe_w2_1", c.NE * c.DFE, D)]
        for (name, K, Fd) in decls:
            W[name] = s.weight(name, K, Fd)
        order = [d[0] for d in decls]
        gi = [0]

        def gather_upto(name):
            while gi[0] < len(order) and gi[0] <= order.index(name):
                s.gather(order[gi[0]])
                gi[0] += 1

        s.cast_upto = gather_upto

        NCL = 6 * DC // 8
        cT = s.st[0]
        emit(s.sq, lambda e: e.dma_start(out=cT[:, 0:DC * 4], in_=cT_in), [], [s.st_r[0]])
        scT = s.sb("scT", [128, DC * 4], BF16)
        scT_r = Res("scT")
        emit(s.act, lambda e: e.activation(out=scT[:, :], in_=cT[:, 0:DC * 4], func=AF.Silu), [s.st_r[0]], [scT_r])
        adab = s.sb("adab", [128, NCL], F32)
        s.load_small(adab[:, :], adab_in, small_r)
        modp = s.sb("modp", [128, NCL * 4], F32)
        modp_r = Res("modp")
        for j in range(NCL):
            sl = s.wcount % len(s.wt)
            s.wcount += 1
            wt, wr = s.wt[sl], s.wt_r[sl]
            emit(s.pq, lambda e, wt=wt, j=j: e.dma_start(out=wt[:, 0:DC, :], in_=adaw_in[:, j * 128:(j + 1) * 128].rearrange("(kc p) f -> p kc f", p=128)),
                 [], [wr])
            ps, pr = s.bank[6 + j % 2], [s.bank_r[6 + j % 2]]
            for kc in range(DC):
                emit(s.pe, lambda e, ps=ps, wt=wt, kc=kc: e.matmul(ps[:, 0:4], lhsT=wt[:, kc, :], rhs=scT[:, kc * 4:(kc + 1) * 4],
                                                                   start=(kc == 0), stop=(kc == DC - 1)), [wr, scT_r], pr)
            emit(s.dve, lambda e, ps=ps, j=j: e.tensor_scalar(out=modp[:, j * 4:(j + 1) * 4], in0=ps[:, 0:4], scalar1=adab[:, j:j + 1], scalar2=None,
                                                              op0=ALU.add), pr + [small_r], [modp_r])
        modl_d = s.dram("modl", [NCL * 128, 4], F32)
        modf_d = s.dram("modf", [6 * D, 4], F32)
        rl, rf = Res("modl"), Res("modf")
        emit(s.pq, lambda e: e.dma_start(out=modl_d.rearrange("(j p) b -> p j b", p=128), in_=modp[:, :].rearrange("p (j b) -> p j b", b=4)), [modp_r], [rl])
        s.gather2(modl_d, [rl], modf_d, rf, NCL * 128, 4, F32, "modg")
        modall = s.st[1]
        m3 = modall[:, 0:6 * DC * 4].rearrange("p (j b) -> p j b", b=4)
        emit(s.sq, lambda e: e.dma_start(out=m3, in_=modf_d.rearrange("(j p) b -> p j b", p=128)), [rf], [s.st_r[1]])
        ms_r = Res("modsel")
        emit(s.dve, lambda e: e.tensor_scalar(out=modsel[:, :], in0=m3[:, :, 0], scalar1=bsel[:, 0:1], scalar2=None, op0=ALU.mult), [s.st_r[1], small_r], [ms_r])
        for b in range(1, 4):
            emit(s.dve, lambda e, b=b: e.scalar_tensor_tensor(out=modsel[:, :], in0=m3[:, :, b], scalar=bsel[:, b:b + 1], in1=modsel[:, :],
                                                              op0=ALU.mult, op1=ALU.add), [s.st_r[1], ms_r], [ms_r])
        mod_r = s.mod_r
        for i in range(c.DEPTH):
            o = i * 6 * DC
            emit(s.dve, lambda e, o=o: e.tensor_tensor(out=s.modL[:, o:o + 6 * DC], in0=s.modL[:, o:o + 6 * DC],
                                                       in1=modsel[:, :], op=ALU.add), [ms_r, small_r, mod_r], [mod_r])
            for (j, nw) in ((1, nmix), (4, nffn)):
                emit(s.dve, lambda e, o=o, j=j, nw=nw, i=i: e.scalar_tensor_tensor(
                    out=s.modL[:, o + j * DC:o + (j + 1) * DC], in0=s.modL[:, o + j * DC:o + (j + 1) * DC], scalar=1.0,
                    in1=nw[:, i * DC:(i + 1) * DC], op0=ALU.add, op1=ALU.mult), [mod_r, small_r], [mod_r])

        def M(i, j):
            o = i * 6 * DC + j * DC
            return s.modL[:, o:o + DC]

        gather_upto("att_w_out")

        s.dbg("modL", s.modL[:, :], [mod_r], c.DEPTH * 6 * DC)
        s.dbg("modsel", modsel[:, :], [ms_r], 6 * DC)
        s.dbg("modp", modp[:, :], [modp_r], NCL * 4)

        done = False
        try:
            s.layers(M, gather_upto)
        except StopBuild:
            done = True
        for i in range(0):
            if s.stop_after is not None and s.stop_after[0] == "pre":
                done = True
                break
            kind = i % 4
            if kind == 0:
                s.attention(M(i, 1), M(i, 0), M(i, 2))
                gather_upto("moe_w2_0")
            elif kind == 1:
                s.sconv(M(i, 1), M(i, 0), M(i, 2))
                gather_upto("ffn_w2_1")
            elif kind == 2:
                s.conformer(M(i, 1), M(i, 0), M(i, 2))
                gather_upto("moe_w2_1")
            else:
                s.rglru(M(i, 1), M(i, 0), M(i, 2))
            if s.stop_after == ("mix", i):
                done = True
                break
            if i % 2 == 0:
                s.norm(M(i, 4), M(i, 3))
                s.ffn(W[f"ffn_w13_{i // 2}"], W[f"ffn_w2_{i // 2}"], M(i, 5))
            else:
                s.moe(W[f"moe_w13_{i // 2}"], W[f"moe_w2_{i // 2}"], M(i, 4), M(i, 3), M(i, 5), i // 2)
            if s.stop_after == ("ffn", i):
                done = True
                break

        s.out_r = Res("out")

        def fin(dc, xt, xr, gs):
            emit(s.act, lambda e: e.activation(out=xt, in_=xt, func=AF.Identity, scale=gs[:, dc:dc + 1]), [xr, small_r], [xr])
            emit(s.sq, lambda e: e.dma_start(out=out_d[dc * 128:(dc + 1) * 128, :], in_=xt), [xr], [s.out_r])

        if not done:
            s.norm(s.nfin, None, out_fn=fin)
        else:
            for dc in range(DC):
                xt, xr = s.next_xt()
                emit(s.sq, lambda e: e.dma_start(out=xt, in_=s.xs[dc * 128:(dc + 1) * 128, :]), [s.xs_r[dc]], [xr])
                emit(s.pq, lambda e: e.dma_start(out=out_d[dc * 128:(dc + 1) * 128, :], in_=xt), [xr], [s.out_r])
        for tok in getattr(s.pq, "cc_toks", []):
            s.pq.wait_tok(tok)
        for q in (s.pe, s.act, s.dve):
            if q.n > 0:
                s.pq.wait_tok(("c", q, q.n))
        for q in (s.pq, s.sq):
            K = len(q.sems)
            for i in range(max(0, q.n - K), q.n):
                q.wait_tok(("d", q.sems[i % K], 16 * (i // K + 1), (q.name, i % K)))

        with nc.Block() as block:
            @block.tensor
            def _(e):
                for f in s.pe.prog:
                    f(e)

            @block.scalar
            def _(e):
                for f in s.act.prog:
                    f(e)

            @block.vector
            def _(e):
                for f in s.dve.prog:
                    f(e)

            @block.sync
            def _(e):
                for f in s.sq.prog:
                    f(e)

            @block.gpsimd
            def _(e):
                for f in s.pq.prog:
                    f(e)
        es.close()
        return nc

    def dbg(s, name, ap, res, n):
        if not s.debug or s.dbg_off + n > 8192:
            return
        o = s.dbg_off
        s.dbg_off += n
        s.dbg_map[name] = (o, n)
        emit(s.pq, lambda e: e.dma_start(out=s.dbg_d[:, o:o + n], in_=ap), res, [Res("dbg")])

    def chk(s, tag, kind="att"):
        if s.stop_after == (kind, tag):
            raise StopBuild()

    def layers(s, M, gather_upto):
        c = s.c
        W = s.W
        for i in range(c.DEPTH):
            if s.stop_after is not None and s.stop_after[0] == "pre":
                raise StopBuild()
            kind = i % 4
            if s.only is None or f"mix{i}" in s.only:
                gather_upto(["att_w_out", "sconv_w_out", "conf_w_out", "lru_w_out"][kind])
                if kind == 0:
                    s.attention(M(i, 1), M(i, 0), M(i, 2))
                elif kind == 1:
                    s.sconv(M(i, 1), M(i, 0), M(i, 2))
                elif kind == 2:
                    s.conformer(M(i, 1), M(i, 0), M(i, 2))
                else:
                    s.rglru(M(i, 1), M(i, 0), M(i, 2))
            if s.stop_after == ("mix", i):
                raise StopBuild()
            if s.only is None or f"ffn{i}" in s.only:
                gather_upto(["ffn_w2_0", "moe_w2_0", "ffn_w2_1", "moe_w2_1"][i])
                if i % 2 == 0:
                    s.norm(M(i, 4), M(i, 3))
                    s.ffn(W[f"ffn_w13_{i // 2}"], W[f"ffn_w2_{i // 2}"], M(i, 5))
                else:
                    s.moe(W[f"moe_w13_{i // 2}"], W[f"moe_w2_{i // 2}"], M(i, 4), M(i, 3), M(i, 5), i // 2)
            if s.stop_after == ("ffn", i):
                raise StopBuild()

    def swiglu_epi(s, gbc=None, gbc_r=None):
        c = s.c

        def epi(idx, ps, pr, w):
            jj, which = idx // 2, idx % 2
            tq, tr = s.tmp[jj % 2], s.tmp_r[jj % 2]
            if which == 0:
                emit(s.act, lambda e: e.activation(out=tq, in_=ps[:, 0:c.T], func=AF.Silu), pr, [tr])
                if gbc is not None:
                    emit(s.dve, lambda e: e.tensor_tensor(out=tq, in0=tq, in1=gbc, op=ALU.mult), [tr] + gbc_r, [tr])
            else:
                emit(s.dve, lambda e: e.tensor_tensor(out=s.midc(jj), in0=ps[:, 0:c.T], in1=tq, op=ALU.mult), pr + [tr], [s.mid_r[jj]])
        return epi

    def ffn(s, W13, W2, gate):
        c = s.c
        (w13, r13), (w2, r2) = W13, W2
        G = c.DC
        HC = c.DFF // 128
        for g in range(HC // G):
            cols = []
            for jj in range(G):
                j = g * G + jj
                cols.append((j * 128, 128))
                cols.append((c.DFF + j * 128, 128))
            s.linear(w13, r13, 0, c.DC, cols, s.hx, s.hT_r, s.swiglu_epi(), c.T)
            if "act0" not in s.dbg_map:
                s.dbg("act0", s.midc(0), [s.mid_r[0]], c.T)
            pre, epi2 = s.resid(gate)
            s.linear(w2, r2, g * G * 128, G, [(d * 128, 128) for d in range(c.DC)], s.midx, s.mid_r, epi2, c.T, pre=pre)

    def moe(s, W13, W2, gs, sh, gate, li):
        c = s.c
        (w13, r13), (w2, r2) = W13, W2
        DC, T = c.DC, c.T
        rtf = s.rtf
        emit(s.sq, lambda e: e.dma_start(out=rtf[:, :], in_=s.rt_in[:, li * DC * 8:(li + 1) * DC * 8]), [], [s.rtf_r])
        lgps = s.psum[:, 2048:3072]
        lgps_r = [s.bank_r[4], s.bank_r[5]]

        def hook(dc, tq, tr):
            for n0 in range(0, T, 512):
                n1 = min(T, n0 + 512)
                emit(s.pe, lambda e, n0=n0, n1=n1: e.matmul(lgps[0:8, n0:n1], lhsT=rtf[:, dc * 8:(dc + 1) * 8], rhs=tq[:, n0:n1],
                                                            start=(dc == 0), stop=(dc == DC - 1)), [tr, s.rtf_r], lgps_r)

        s.norm(gs, sh, hook=hook)
        s.chk(1, "moe")
        lgT = s.st[0]
        emit(s.dve, lambda e: e.tensor_copy(out=lgT[0:8, :], in_=lgps[0:8, 0:T]), lgps_r, [s.st_r[0]])
        gtok = s.gtok
        l8, m8, ex = s.sm[0], s.sm[1], s.sm[2]
        r8 = [s.sm_r]
        for tb in range(c.TB):
            ps, pr = s.bank[6], [s.bank_r[6]]
            emit(s.pe, lambda e, tb=tb: e.matmul(ps[:, 0:8], lhsT=lgT[0:8, tb * 128:(tb + 1) * 128], rhs=s.ident32[0:8, 0:8], start=True, stop=True),
                 [s.st_r[0], s.const_r], pr)
            emit(s.dve, lambda e: e.tensor_copy(out=l8[:, 0:8], in_=ps[:, 0:8]), pr, r8)
            emit(s.dve, lambda e: e.max(out=m8[:, 0:8], in_=l8[:, 0:8]), r8, r8)
            emit(s.dve, lambda e: e.tensor_scalar(out=ex[:, 8:9], in0=m8[:, 0:1], scalar1=-1.0, scalar2=None, op0=ALU.mult), r8, r8)
            emit(s.act, lambda e: e.activation(out=ex[:, 0:8], in_=l8[:, 0:8], func=AF.Exp, bias=ex[:, 8:9], scale=1.0), r8, r8)
            emit(s.dve, lambda e: e.scalar_tensor_tensor(out=ex[:, 0:8], in0=l8[:, 0:8], scalar=m8[:, 1:2], in1=ex[:, 0:8], op0=ALU.is_ge, op1=ALU.mult),
                 r8, r8)
            emit(s.dve, lambda e: e.tensor_reduce(out=ex[:, 9:10], in_=ex[:, 0:8], axis=AX.X, op=ALU.add), r8, r8)
            emit(s.dve, lambda e: e.reciprocal(out=ex[:, 9:10], in_=ex[:, 9:10]), r8, r8)
            emit(s.dve, lambda e, tb=tb: e.tensor_scalar(out=gtok[:, tb, :], in0=ex[:, 0:8], scalar1=ex[:, 9:10], scalar2=None, op0=ALU.mult),
                 r8, [s.gtok_r])
        s.chk(2, "moe")
        HCE = c.DFE // 128
        for ei in range(c.NE):
            gps = s.psum[:, 2048:3072]
            gpr = [s.bank_r[4], s.bank_r[5]]
            for tb in range(c.TB):
                emit(s.dve, lambda e, tb=tb: e.tensor_copy(out=s.gb[:, :], in_=gtok[:, tb, ei:ei + 1].to_broadcast([128, 128])), [s.gtok_r], [s.gb_r])
                emit(s.pe, lambda e, tb=tb: e.matmul(gps[:, tb * 128:(tb + 1) * 128], lhsT=s.gb[:, :], rhs=s.ident32[:, :], start=True, stop=True),
                     [s.gb_r, s.const_r], gpr)
            gbc, gbc_r = s.st[1], [s.st_r[1]]
            emit(s.act, lambda e: e.activation(out=gbc, in_=gps[:, 0:T], func=AF.Identity), gpr, gbc_r)
            s.chk(3, "moe")
            cols = []
            for jj in range(HCE):
                cols.append((jj * 128, 128))
                cols.append((c.DFE + jj * 128, 128))
            s.linear(w13, r13, ei * c.D, DC, cols, s.hx, s.hT_r, s.swiglu_epi(gbc, gbc_r), T)
            s.chk(4, "moe")
            pre, epi2 = s.resid(gate)
            s.linear(w2, r2, ei * c.DFE, HCE, [(d * 128, 128) for d in range(DC)], s.midx, s.mid_r[0:HCE], epi2, T, pre=pre)

    def sconv(s, gs, sh, gate):
        c = s.c
        D, DC, T = c.D, c.DC, c.T
        (w_in, r_in), (w_out, r_out) = s.W["sconv_w_in"], s.W["sconv_w_out"]
        s.load_small(s.cvw[:, 0:3 * DC], s.scw_in, s.cv_r)
        s.norm(gs, sh)
        cols = []
        for dc in range(DC):
            cols += [(D + dc * 128, 128), (2 * D + dc * 128, 128)]

        def epi1(idx, ps, pr, w):
            dc, which = idx // 2, idx % 2
            tq, tr = s.tmp[dc % 2], s.tmp_r[dc % 2]
            if which == 0:
                emit(s.act, lambda e: e.activation(out=tq, in_=ps[:, 0:T], func=AF.Identity), pr, [tr])
            else:
                emit(s.dve, lambda e: e.tensor_tensor(out=s.midc(dc), in0=ps[:, 0:T], in1=tq, op=ALU.mult), pr + [tr], [s.mid_r[dc]])

        s.linear(w_in, r_in, 0, DC, cols, s.hx, s.hT_r, epi1, T)
        s.halo_exchange("sc")
        if s.only is None:
            s.cast_upto("conf_w_out")

        def epi2(dc, ps, pr, w):
            tq, tr = s.tmp[dc % 2], [s.tmp_r[dc % 2]]
            s.dwconv(dc, s.cvw, 3, tq, tr)
            emit(s.dve, lambda e: e.tensor_tensor(out=s.midc(dc), in0=ps[:, 0:T], in1=tq, op=ALU.mult), pr + tr, [s.mid_r[dc]])

        s.linear(w_in, r_in, 0, DC, [(dc * 128, 128) for dc in range(DC)], s.hx, s.hT_r, epi2, T)
        pre, epi3 = s.resid(gate)
        s.linear(w_out, r_out, 0, DC, [(d * 128, 128) for d in range(DC)], s.midx, s.mid_r, epi3, T, pre=pre)

    def conformer(s, gs, sh, gate):
        c = s.c
        D, DC, T = c.D, c.DC, c.T
        (w_in, r_in), (w_out, r_out) = s.W["conf_w_in"], s.W["conf_w_out"]
        s.load_small(s.cvw[:, 0:31 * DC], s.cfw_in, s.cv_r)
        s.load_small(s.cvv[:, 0:3 * DC], s.cfv_in, s.cv_r)
        s.norm(gs, sh)
        cols = []
        for dc in range(DC):
            cols += [(D + dc * 128, 128), (dc * 128, 128)]

        def epi1(idx, ps, pr, w):
            dc, which = idx // 2, idx % 2
            tq, tr = s.tmp[dc % 2], s.tmp_r[dc % 2]
            if which == 0:
                emit(s.act, lambda e: e.activation(out=tq, in_=ps[:, 0:T], func=AF.Sigmoid), pr, [tr])
            else:
                emit(s.dve, lambda e: e.tensor_tensor(out=s.midc(dc), in0=ps[:, 0:T], in1=tq, op=ALU.mult), pr + [tr], [s.mid_r[dc]])

        s.linear(w_in, r_in, 0, DC, cols, s.hx, s.hT_r, epi1, T)
        s.halo_exchange("cf")
        if s.only is None:
            s.cast_upto("lru_w_out")
        acc1, acc2 = s.st[0], s.st[1]
        a1r, a2r = [s.st_r[0]], [s.st_r[1]]
        for dc in range(DC):
            tq, tr = s.tmp[dc % 2], [s.tmp_r[dc % 2]]
            s.dwconv(dc, s.cvw, 31, tq, tr, bias=s.cvv[:, 0:DC])
            sq, sqr = s.next_xt()
            emit(s.act, lambda e: e.activation(out=sq, in_=tq, func=AF.Square), tr, [sqr])
            emit(s.act, lambda e: e.activation(out=s.hT[:, dc, :], in_=tq, func=AF.Identity), tr, [s.hT_r[dc]])
            if dc == 0:
                emit(s.dve, lambda e: e.tensor_copy(out=acc1, in_=tq), tr, a1r)
                emit(s.dve, lambda e: e.tensor_copy(out=acc2, in_=sq), [sqr], a2r)
            else:
                emit(s.dve, lambda e: e.tensor_tensor(out=acc1, in0=acc1, in1=tq, op=ALU.add), tr + a1r, a1r)
                emit(s.dve, lambda e: e.tensor_tensor(out=acc2, in0=acc2, in1=sq, op=ALU.add), [sqr] + a2r, a2r)
        mean, mr = s.xt[0], [s.xt_r[0]]
        rstd, rr = s.xt[1], [s.xt_r[1]]
        s.colsum_bc(acc1, a1r, mean, mr, 1.0 / D, 0.0, None)
        s.colsum_bc(acc2, a2r, rstd, rr, 1.0 / D, 0.0, None)
        m2, m2r = s.px, [s.px_r]
        emit(s.dve, lambda e: e.tensor_tensor(out=m2, in0=mean, in1=mean, op=ALU.mult), mr, m2r)
        emit(s.dve, lambda e: e.tensor_tensor(out=rstd, in0=rstd, in1=m2, op=ALU.subtract), rr + m2r, rr)
        emit(s.dve, lambda e: e.tensor_scalar(out=rstd, in0=rstd, scalar1=1e-6, scalar2=None, op0=ALU.add), rr, rr)
        s.rpow(rstd, rr, -0.5)
        for dc in range(DC):
            tq, tr = s.tmp[dc % 2], [s.tmp_r[dc % 2]]
            emit(s.dve, lambda e: e.tensor_tensor(out=tq, in0=s.hT[:, dc, :], in1=mean, op=ALU.subtract), [s.hT_r[dc]] + mr, tr)
            emit(s.dve, lambda e: e.tensor_tensor(out=tq, in0=tq, in1=rstd, op=ALU.mult), tr + rr, tr)
            emit(s.act, lambda e: e.activation(out=s.midc(dc), in_=tq, func=AF.Silu, bias=s.cvv[:, 2 * DC + dc:2 * DC + dc + 1],
                                               scale=s.cvv[:, DC + dc:DC + dc + 1]), tr + [s.cv_r], [s.mid_r[dc]])
        pre, epi3 = s.resid(gate)
        s.linear(w_out, r_out, 0, DC, [(d * 128, 128) for d in range(DC)], s.midx, s.mid_r, epi3, T, pre=pre)

    def rglru(s, gs, sh, gate):
        c = s.c
        D, DC, T = c.D, c.DC, c.T
        CH = c.LB // 128
        (w_in, r_in), (w_out, r_out) = s.W["lru_w_in"], s.W["lru_w_out"]
        (w_a, rw_a), (w_x, rw_x) = s.W["lru_w_a"], s.W["lru_w_x"]
        s.load_small(s.cvw[:, 0:4 * DC], s.lrw_in, s.cv_r)
        s.load_small(s.cvv[:, 0:4 * DC], s.lrv_in, s.cv_r)
        vec, vr = s.vec, [s.vec_r]
        emit(s.act, lambda e: e.activation(out=vec[:, 0:DC], in_=s.cvv[:, 3 * DC:4 * DC], func=AF.Exp, scale=-1.0), [s.cv_r], vr)
        emit(s.dve, lambda e: e.tensor_scalar(out=vec[:, 0:DC], in0=vec[:, 0:DC], scalar1=1.0, scalar2=None, op0=ALU.add), vr, vr)
        emit(s.act, lambda e: e.activation(out=vec[:, 0:DC], in_=vec[:, 0:DC], func=AF.Ln), vr, vr)
        emit(s.dve, lambda e: e.tensor_scalar(out=vec[:, DC:2 * DC], in0=vec[:, 0:DC], scalar1=-16.0, scalar2=None, op0=ALU.mult), vr, vr)
        emit(s.dve, lambda e: e.tensor_scalar(out=vec[:, 0:DC], in0=vec[:, 0:DC], scalar1=-8.0, scalar2=None, op0=ALU.mult), vr, vr)
        s.chk(1, "lru")
        s.norm(gs, sh)

        def epi1(dc, ps, pr, w):
            emit(s.act, lambda e: e.activation(out=s.midc(dc), in_=ps[:, 0:T], func=AF.Identity), pr, [s.mid_r[dc]])

        s.linear(w_in, r_in, 0, DC, [(D + dc * 128, 128) for dc in range(DC)], s.hx, s.hT_r, epi1, T)
        s.chk(2, "lru")
        s.halo_exchange("lr")
        for dc in range(DC):
            tq, tr = s.tmp[dc % 2], [s.tmp_r[dc % 2]]
            s.dwconv(dc, s.cvw, 4, tq, tr, bias=s.cvv[:, 0:DC])
            emit(s.act, lambda e: e.activation(out=s.midc(dc), in_=tq, func=AF.Identity), tr, [s.mid_r[dc]])

        t_r, t_a2, t_a, t_u, t_g = s.xt[0], s.xt[1], s.tmp[0], s.tmp[1], s.st[0]
        r_r, r_a2, r_a, r_u, r_g = [s.xt_r[0]], [s.xt_r[1]], [s.tmp_r[0]], [s.tmp_r[1]], [s.st_r[0]]
        ob = s.st[1].bitcast(BF16)
        ob_r = [s.st_r[1]]
        assert CH <= 2

        def lru_pass(final_only):
            for hh in range(c.LH):
                for jc in range(CH):
                    j = hh * CH + jc
                    xfn = lambda kc, n0, n1, hh=hh: s.mid[:, hh * CH + kc, HALO + n0:HALO + n1]
                    xres = s.mid_r[hh * CH:(hh + 1) * CH]

                    def epi_r(_, ps, pr, w):
                        emit(s.act, lambda e: e.activation(out=t_r, in_=ps[:, 0:T], func=AF.Sigmoid, bias=s.cvv[:, DC + j:DC + j + 1], scale=1.0),
                             pr + [s.cv_r], r_r)
                        emit(s.act, lambda e: e.activation(out=t_a, in_=t_r, func=AF.Exp, scale=vec[:, j:j + 1]), r_r + vr, r_a)
                        emit(s.act, lambda e: e.activation(out=t_a2, in_=t_r, func=AF.Exp, scale=vec[:, DC + j:DC + j + 1]), r_r + vr, r_a2)
                        emit(s.dve, lambda e: e.tensor_scalar(out=t_a2, in0=t_a2, scalar1=-1.0, scalar2=1.0, op0=ALU.mult, op1=ALU.add), r_a2, r_a2)
                        emit(s.act, lambda e: e.activation(out=t_a2, in_=t_a2, func=AF.Sqrt), r_a2, r_a2)

                    def epi_i(_, ps, pr, w):
                        emit(s.act, lambda e: e.activation(out=t_u, in_=ps[:, 0:T], func=AF.Sigmoid, bias=s.cvv[:, 2 * DC + j:2 * DC + j + 1], scale=1.0),
                             pr + [s.cv_r], r_u)
                        emit(s.dve, lambda e: e.tensor_tensor(out=t_u, in0=t_u, in1=s.midc(j), op=ALU.mult), r_u + [s.mid_r[j]], r_u)
                        emit(s.dve, lambda e: e.tensor_tensor(out=t_u, in0=t_u, in1=t_a2, op=ALU.mult), r_u + r_a2, r_u)
                        init = 0.0 if final_only else vec[:, 3 * DC + j:3 * DC + j + 1]
                        emit(s.dve, lambda e: e.tensor_tensor_scan(out=t_r, data0=t_a, data1=t_u, initial=init, op0=ALU.mult, op1=ALU.add),
                             r_a + r_u + vr + r_r, r_r)
                        if final_only:
                            emit(s.dve, lambda e: e.tensor_copy(out=vec[:, 2 * DC + j:2 * DC + j + 1], in_=t_r[:, T - 1:T]), r_r + vr, vr)

                    s.linear(w_a, rw_a, hh * c.LB, CH, [(jc * 128, 128)], xfn, xres, epi_r, T)
                    s.linear(w_x, rw_x, hh * c.LB, CH, [(jc * 128, 128)], xfn, xres, epi_i, T)
                    if not final_only:
                        def epi_g(_, ps, pr, w):
                            emit(s.act, lambda e: e.activation(out=t_g, in_=ps[:, 0:T], func=AF.Square), pr, r_g)
                            emit(s.dve, lambda e: e.tensor_scalar(out=t_g, in0=t_g, scalar1=0.044715, scalar2=1.0, op0=ALU.mult, op1=ALU.add), r_g, r_g)
                            emit(s.dve, lambda e: e.tensor_tensor(out=t_g, in0=t_g, in1=ps[:, 0:T], op=ALU.mult), r_g + pr, r_g)
                            emit(s.act, lambda e: e.activation(out=t_g, in_=t_g, func=AF.Sigmoid, scale=1.5957691216057308), r_g, r_g)
                            emit(s.dve, lambda e: e.tensor_tensor(out=t_g, in0=t_g, in1=ps[:, 0:T], op=ALU.mult), r_g + pr, r_g)
                            emit(s.dve, lambda e: e.tensor_tensor(out=ob[:, jc * T:(jc + 1) * T], in0=t_g, in1=t_r, op=ALU.mult), r_g + r_r, ob_r)
                        s.linear(w_in, r_in, 0, DC, [(j * 128, 128)], s.hx, s.hT_r, epi_g, T)
                if not final_only:
                    for jc in range(CH):
                        j = hh * CH + jc
                        emit(s.act, lambda e, j=j, jc=jc: e.activation(out=s.midc(j), in_=ob[:, jc * T:(jc + 1) * T], func=AF.Identity), ob_r, [s.mid_r[j]])

        s.chk(3, "lru")
        lru_pass(True)
        s.chk(4, "lru")
        loc = s.dram("lru_l", [128, DC], F32)
        pair = s.dram("lru_p", [256, DC], F32)
        rl, rp = Res("ll"), Res("lp")
        emit(s.pq, lambda e: e.dma_start(out=loc, in_=vec[:, 2 * DC:3 * DC]), vr, [rl])
        s.pair_exchange(loc, rl, pair, rp)
        if s.only is None:
            s.cast_upto("moe_w2_1")
        emit(s.sq, lambda e: e.dma_start(out=vec[:, 3 * DC:4 * DC], in_=pair[0:128, :]), [rp], vr)
        emit(s.dve, lambda e: e.tensor_scalar(out=vec[:, 3 * DC:4 * DC], in0=vec[:, 3 * DC:4 * DC], scalar1=s.flag[:, 0:1], scalar2=None, op0=ALU.mult),
             vr + [s.const_r], vr)
        s.chk(5, "lru")
        lru_pass(False)
        s.chk(6, "lru")
        pre, epi3 = s.resid(gate)
        s.linear(w_out, r_out, 0, DC, [(d * 128, 128) for d in range(DC)], s.midx, s.mid_r, epi3, T, pre=pre)

    def attention(s, gs, sh, gate):
        c = s.c
        D, DC, T, S, TB = c.D, c.DC, c.T, c.S, c.TB
        QC, KVC, NH, IH = c.QL // 128, c.KVL // 128, c.NH, c.IH
        SB = S // 128
        W = s.W
        (w_in, r_in), (w_qidx, r_qidx), (w_uq, r_uq) = W["att_w_in"], W["att_w_qidx"], W["att_w_uq"]
        (w_ukT, r_ukT), (w_uv, r_uv), (w_out, r_out) = W["att_w_ukT"], W["att_w_uv"], W["att_w_out"]
        s.load_small(s.cvv[:, 0:QC], s.gcq_in, s.cv_r)
        s.load_small(s.cvv[:, QC:QC + KVC], s.gckv_in, s.cv_r)

        if s.full:
            midflat = s.mid[:, :, :].rearrange("p a b -> p (a b)")
            ohs = midflat[:, 0:2 * 32 * 128]
            ohs_r = s.mid_r
            rbb = s.st[0]
            rbb_r = [s.st_r[0]]
        else:
            ohs = s.sb("ohs_sb", [128, 2 * 32 * 128], BF16)[:, :]
            ohs_r = [Res("ohs")]
            rbb = s.sb("rbb_sb", [128, 32 * NH], F32)[:, :]
            rbb_r = [Res("rbb")]
        emit(s.pq, lambda e: e.dma_start(out=ohs, in_=s.ohs_in), [], ohs_r)
        emit(s.sq, lambda e: e.dma_start(out=rbb[:, 0:32 * NH], in_=s.rbb_in), [], rbb_r)
        assert NH <= 32
        negb = s.gb[:, 0:NH]
        nb_r = [s.gb_r]
        emit(s.dve, lambda e: e.tensor_scalar(out=negb, in0=rbb[:, 31 * NH:32 * NH], scalar1=-1.0, scalar2=None, op0=ALU.mult), rbb_r, nb_r)
        eacc = s.tmp[0][:, 0:128]
        ea_r = [s.tmp_r[0]]
        for h in range(NH):
            for dl in range(2):
                for b in range(32):
                    src = ohs[:, (dl * 32 + b) * 128:(dl * 32 + b + 1) * 128]
                    sc = rbb[:, b * NH + h:b * NH + h + 1]
                    if b == 0:
                        emit(s.dve, lambda e, src=src, sc=sc: e.tensor_scalar(out=eacc, in0=src, scalar1=sc, scalar2=None, op0=ALU.mult),
                             ohs_r + rbb_r, ea_r)
                    else:
                        emit(s.dve, lambda e, src=src, sc=sc: e.scalar_tensor_tensor(out=eacc, in0=src, scalar=sc, in1=eacc, op0=ALU.mult, op1=ALU.add),
                             ohs_r + rbb_r + ea_r, ea_r)
                o = (h * 2 + dl) * 128
                emit(s.act, lambda e, o=o, h=h: e.activation(out=s.EB[:, o:o + 128], in_=eacc, func=AF.Exp, bias=negb[:, h:h + 1], scale=1.0),
                     ea_r + nb_r, [s.EB_r])

        s.chk(1)
        s.norm(gs, sh)
        s.chk(2)
        accq, aq_r = s.st[0], [s.st_r[0]]
        acck, ak_r = s.st[1], [s.st_r[1]]

        def epi_in(j, ps, pr, w):
            if j < QC + KVC:
                acc, ar = (accq, aq_r) if j < QC else (acck, ak_r)
                first = (j == 0) or (j == QC)
                if first:
                    emit(s.act, lambda e: e.activation(out=acc, in_=ps[:, 0:T], func=AF.Square), pr, ar)
                else:
                    tq, tr = s.tmp[j % 2], [s.tmp_r[j % 2]]
                    emit(s.act, lambda e: e.activation(out=tq, in_=ps[:, 0:T], func=AF.Square), pr, tr)
                    emit(s.dve, lambda e: e.tensor_tensor(out=acc, in0=acc, in1=tq, op=ALU.add), tr + ar, ar)
            emit(s.act, lambda e: e.activation(out=s.midc(j), in_=ps[:, 0:T], func=AF.Identity), pr, [s.mid_r[j]])

        s.linear(w_in, r_in, 0, DC, [(j * 128, 128) for j in range(QC + KVC + 1)], s.hx, s.hT_r, epi_in, T)

        s.chk(3)
        if s.full:
            hflat = s.hT[:, :, :].rearrange("p a b -> p (a b)")
        else:
            hflat = s.sb("att_scratch", [128, QC * T + KVC * S + SB * c.KVL + S + 16 * 128 + TB * IH * 2 + KVC * 256 + 128 + 256 + 64], BF16)[:, :]
        off = [0]

        def carve(n):
            a = hflat[:, off[0]:off[0] + n]
            off[0] += n
            return a

        assert TB * IH <= 31 * DC
        wtok = s.cvw[:, 0:TB * IH]
        wtok_r = [s.cv_r]
        sl = s.wcount % len(s.wt)
        s.wcount += 1
        wt, wr = s.wt[sl], s.wt_r[sl]
        wcol = c.QL + c.KVL + 128
        emit(s.sq, lambda e: e.dma_start(out=wt[:, 0:DC, 0:IH], in_=w_in[:, wcol:wcol + IH].rearrange("(kc p) f -> p kc f", p=128)), list(r_in[1]), [wr])
        for tb in range(TB):
            ps, pr = s.bank[7], [s.bank_r[7]]
            for kc in range(DC):
                emit(s.pe, lambda e, tb=tb, kc=kc: e.matmul(ps[:, 0:IH], lhsT=s.hT[:, kc, tb * 128:(tb + 1) * 128], rhs=wt[:, kc, 0:IH],
                                                            start=(kc == 0), stop=(kc == DC - 1)), [wr, s.hT_r[kc]], pr)
            emit(s.dve, lambda e, tb=tb: e.tensor_copy(out=wtok[:, tb * IH:(tb + 1) * IH], in_=ps[:, 0:IH]), pr, wtok_r)

        cqn = carve(QC * T)
        ckv = carve(KVC * S)
        kvt = carve(SB * c.KVL)
        kidx = carve(S)
        GI = min(16, IH)
        qig = carve(GI * 128)
        qh = carve(128)
        qlat = carve(KVC * 128)
        olat = carve(KVC * 128)
        views = {n: [Res(n)] for n in ("cqn", "ckv", "kvt", "kidx", "qig", "qh", "qlat", "olat")}
        if s.full:
            base = []
            for r in s.hT_r:
                base += r.r + ([r.w] if r.w is not None else [])
            for v in views.values():
                v[0].r = list(base)
        rinv = carve(256).bitcast(F32)
        rinv_r = [Res("rinv")]
        views["rinv"] = rinv_r
        if s.full:
            rinv_r[0].r = list(base)

        rq, rq_r = s.xt[0], [s.xt_r[0]]
        rk, rk_r = s.xt[1], [s.xt_r[1]]
        s.colsum_bc(accq, aq_r, rq, rq_r, 1.0 / c.QL, 1e-6, -0.5)
        s.colsum_bc(acck, ak_r, rk, rk_r, 1.0 / c.KVL, 1e-6, -0.5)
        for j in range(QC + KVC):
            tq, tr = s.tmp[j % 2], [s.tmp_r[j % 2]]
            rs, rs_r = (rq, rq_r) if j < QC else (rk, rk_r)
            emit(s.dve, lambda e: e.tensor_tensor(out=tq, in0=s.midc(j), in1=rs, op=ALU.mult), [s.mid_r[j]] + rs_r, tr)
            if j < QC:
                dst, dr = cqn[:, j * T:(j + 1) * T], views["cqn"]
            else:
                cc = j - QC
                dst, dr = ckv[:, cc * S + T:cc * S + 2 * T], views["ckv"]
            emit(s.act, lambda e, dst=dst: e.activation(out=dst, in_=tq, func=AF.Identity, scale=s.cvv[:, j:j + 1]), tr + [s.cv_r], dr)
        emit(s.dve, lambda e: e.tensor_copy(out=kidx[:, T:2 * T], in_=s.midc(QC + KVC)), [s.mid_r[QC + KVC]], views["kidx"])

        s.chk(4)
        NR = (KVC + 1) * 128
        loc = s.dram("kv_l", [NR, T], BF16)
        pair = s.dram("kv_p", [2 * NR, T], BF16)
        rl, rp = Res("kvl"), Res("kvp")
        ckv3 = ckv.rearrange("p (a b) -> p a b", b=S)
        emit(s.pq, lambda e: e.dma_start(out=loc[0:KVC * 128, :].rearrange("(cc p) t -> p cc t", p=128), in_=ckv3[:, :, T:2 * T]), views["ckv"], [rl])
        emit(s.pq, lambda e: e.dma_start(out=loc[KVC * 128:NR, :], in_=kidx[:, T:2 * T]), views["kidx"], [rl])
        s.pair_exchange(loc, rl, pair, rp)
        if s.only is None:
            s.cast_upto("sconv_w_out")
        emit(s.sq, lambda e: e.dma_start(out=ckv3[:, :, 0:T], in_=pair[0:KVC * 128, :].rearrange("(cc p) t -> p cc t", p=128)), [rp], views["ckv"])
        emit(s.sq, lambda e: e.dma_start(out=kidx[:, 0:T], in_=pair[KVC * 128:NR, :]), [rp], views["kidx"])

        s.chk(5)
        for sb_ in range(SB):
            ps, pr = s.bank[6 + sb_ % 2], [s.bank_r[6 + sb_ % 2]]
            for cc in range(KVC):
                emit(s.pe, lambda e, ps=ps, cc=cc, sb_=sb_: e.matmul(ps[:, cc * 128:(cc + 1) * 128], lhsT=ckv[:, cc * S + sb_ * 128:cc * S + (sb_ + 1) * 128],
                                                                  rhs=s.identb[:, :], start=True, stop=True), views["ckv"] + [s.const_r], pr)
            emit(s.act, lambda e, ps=ps, sb_=sb_: e.activation(out=kvt[:, sb_ * c.KVL:(sb_ + 1) * c.KVL], in_=ps[:, 0:c.KVL], func=AF.Identity), pr, views["kvt"])

        s.chk(6)
        big = s.psum[:, 0:2048]
        big_r = s.bank_r[0:4]
        Sc = s.scr[:, 0:S]
        Sc_r = [s.xt_r[0], s.xt_r[1]]
        Sw = s.scr[:, 2 * T:2 * T + S]
        Sw_r = [s.tmp_r[0], s.tmp_r[1]]
        Mb = s.scr[:, 2 * T:3 * T].bitcast(BF16)
        PTs = [s.st[0].bitcast(BF16), s.px.bitcast(BF16)]
        PT_rs = [[s.st_r[0]], [s.px_r]]
        MT = s.st[1].bitcast(BF16)
        MT_r = [s.st_r[1]]
        m8, thr = s.sm[0], s.sm[1]
        sm_r = [s.sm_r]
        scale = 128 ** -0.5

        for qb in range(TB):
            NKC = TB + qb + 1
            NK = NKC * 128
            t0 = qb * 128
            xq = lambda kc, n0, n1: cqn[:, kc * T + t0 + n0:kc * T + t0 + n1]
            first = True
            for g0 in range(0, IH, GI):
                def epi_qi(jl, ps, pr, w):
                    emit(s.act, lambda e: e.activation(out=qig[:, jl * 128:(jl + 1) * 128], in_=ps[:, 0:128], func=AF.Identity), pr, views["qig"])
                s.linear(w_qidx, r_qidx, 0, QC, [((g0 + jl) * 128, 128) for jl in range(GI)], xq, views["cqn"], epi_qi, 128)
                for jl in range(GI):
                    hh = g0 + jl
                    for n0 in range(0, NK, 512):
                        n1 = min(NK, n0 + 512)
                        emit(s.pe, lambda e, jl=jl, n0=n0, n1=n1: e.matmul(big[:, n0:n1], lhsT=qig[:, jl * 128:(jl + 1) * 128], rhs=kidx[:, n0:n1],
                                                                          start=True, stop=True), views["qig"] + views["kidx"], big_r)
                    emit(s.act, lambda e: e.activation(out=Sw[:, 0:NK], in_=big[:, 0:NK], func=AF.Relu), big_r, Sw_r)
                    wsc = wtok[:, qb * IH + hh:qb * IH + hh + 1]
                    if first:
                        emit(s.dve, lambda e, wsc=wsc: e.tensor_scalar(out=Sc[:, 0:NK], in0=Sw[:, 0:NK], scalar1=wsc, scalar2=None, op0=ALU.mult),
                             Sw_r + wtok_r, Sc_r)
                        first = False
                    else:
                        emit(s.dve, lambda e, wsc=wsc: e.scalar_tensor_tensor(out=Sc[:, 0:NK], in0=Sw[:, 0:NK], scalar=wsc, in1=Sc[:, 0:NK],
                                                                             op0=ALU.mult, op1=ALU.add), Sw_r + wtok_r + Sc_r, Sc_r)
            s.chk(7)
            emit(s.dve, lambda e: e.tensor_scalar(out=Sc[:, 0:T], in0=Sc[:, 0:T], scalar1=s.flag[:, 1:2], scalar2=None, op0=ALU.add), Sc_r + [s.const_r], Sc_r)
            emit(s.dve, lambda e: e.tensor_tensor(out=Sc[:, NK - 128:NK], in0=Sc[:, NK - 128:NK], in1=s.tri[:, :], op=ALU.add), Sc_r + [s.const_r], Sc_r)
            rounds = c.KSEL // 8
            for r in range(rounds):
                srcv = Sc if r == 0 else Sw
                src_r = Sc_r if r == 0 else Sw_r
                emit(s.dve, lambda e, srcv=srcv: e.max(out=m8[:, 0:8], in_=srcv[:, 0:NK]), src_r + sm_r, sm_r)
                if r < rounds - 1:
                    emit(s.dve, lambda e, srcv=srcv: e.match_replace(out=Sw[:, 0:NK], in_to_replace=m8[:, 0:8], in_values=srcv[:, 0:NK], imm_value=NEG),
                         src_r + sm_r + Sw_r, Sw_r)
            emit(s.dve, lambda e: e.tensor_scalar(out=thr[:, 0:1], in0=m8[:, 7:8], scalar1=-1.0e29, scalar2=None, op0=ALU.max), sm_r, sm_r)
            emit(s.dve, lambda e: e.tensor_scalar(out=Mb[:, 0:NK], in0=Sc[:, 0:NK], scalar1=thr[:, 0:1], scalar2=None, op0=ALU.is_ge), Sc_r + sm_r + Sw_r, Sw_r)
            s.chk(8)
            for j0 in range(0, NKC, 4):
                j1 = min(NKC, j0 + 4)
                ps, pr = s.bank[7], [s.bank_r[7]]
                for j in range(j0, j1):
                    emit(s.pe, lambda e, j=j, j0=j0: e.matmul(ps[:, (j - j0) * 128:(j - j0 + 1) * 128], lhsT=Mb[:, j * 128:(j + 1) * 128], rhs=s.identb[:, :],
                                                             start=True, stop=True), Sw_r + [s.const_r], pr)
                emit(s.act, lambda e, j0=j0, j1=j1: e.activation(out=MT[:, j0 * 128:j1 * 128], in_=ps[:, 0:(j1 - j0) * 128], func=AF.Identity), pr, MT_r)
            s.chk(9)
            for h in range(NH):
                PT, PT_r = PTs[h % 2], PT_rs[h % 2]

                def epi_q(_, ps, pr, w):
                    emit(s.act, lambda e: e.activation(out=qh, in_=ps[:, 0:128], func=AF.Identity), pr, views["qh"])
                s.linear(w_uq, r_uq, 0, QC, [(h * 128, 128)], xq, views["cqn"], epi_q, 128)
                sl = s.wcount % len(s.wt)
                s.wcount += 1
                wuk, wuk_r = s.wt[sl], [s.wt_r[sl]]
                emit(s.sq, lambda e, wuk=wuk: e.dma_start(out=wuk[:, 0:KVC, :],
                                                         in_=w_ukT[h * 128:(h + 1) * 128, :].rearrange("p (cc f) -> p cc f", f=128)), list(r_ukT[1]), wuk_r)
                sl = s.wcount % len(s.wt)
                s.wcount += 1
                wuv, wuv_r = s.wt[sl], [s.wt_r[sl]]
                emit(s.sq, lambda e, wuv=wuv: e.dma_start(out=wuv[:, 0:KVC, :], in_=w_uv[:, h * 128:(h + 1) * 128].rearrange("(cc p) f -> p cc f", p=128)),
                     list(r_uv[1]), wuv_r)
                ps6, pr6 = s.bank[6], [s.bank_r[6]]
                ps7, pr7 = s.bank[7], [s.bank_r[7]]
                for cc in range(KVC):
                    emit(s.pe, lambda e, cc=cc, wuk=wuk: e.matmul(ps6[:, cc * 128:(cc + 1) * 128], lhsT=wuk[:, cc, :], rhs=qh, start=True, stop=True),
                         wuk_r + views["qh"], pr6)
                emit(s.dve, lambda e: e.tensor_copy(out=qlat, in_=ps6[:, 0:KVC * 128]), pr6, views["qlat"])
                for j in range(NKC):
                    for cc in range(KVC):
                        emit(s.pe, lambda e, j=j, cc=cc: e.matmul(big[:, j * 128:(j + 1) * 128], lhsT=ckv[:, cc * S + j * 128:cc * S + (j + 1) * 128],
                                                                 rhs=qlat[:, cc * 128:(cc + 1) * 128], start=(cc == 0), stop=(cc == KVC - 1)),
                             views["ckv"] + views["qlat"], big_r)
                emit(s.act, lambda e, PT=PT: e.activation(out=PT[:, 0:NK], in_=big[:, 0:NK], func=AF.Exp, scale=scale), big_r, PT_r)
                emit(s.dve, lambda e, PT=PT: e.tensor_tensor(out=PT[:, 0:NK], in0=PT[:, 0:NK], in1=MT[:, 0:NK], op=ALU.mult), PT_r + MT_r, PT_r)
                emit(s.dve, lambda e, PT=PT, h=h: e.tensor_tensor(out=PT[:, NK - 256:NK], in0=PT[:, NK - 256:NK], in1=s.EB[:, h * 256:(h + 1) * 256], op=ALU.mult),
                     PT_r + [s.EB_r], PT_r)
                for j in range(NKC):
                    emit(s.pe, lambda e, j=j, PT=PT: e.matmul(ps7[:, 0:128], lhsT=s.onesb[:, :], rhs=PT[:, j * 128:(j + 1) * 128], start=(j == 0), stop=(j == NKC - 1)),
                         PT_r + [s.const_r], pr7)
                emit(s.dve, lambda e: e.reciprocal(out=rinv, in_=ps7[:, 0:128]), pr7, rinv_r)
                for cc in range(KVC):
                    for j in range(NKC):
                        emit(s.pe, lambda e, j=j, cc=cc, PT=PT: e.matmul(ps6[:, cc * 128:(cc + 1) * 128], lhsT=kvt[:, j * c.KVL + cc * 128:j * c.KVL + (cc + 1) * 128],
                                                                        rhs=PT[:, j * 128:(j + 1) * 128], start=(j == 0), stop=(j == NKC - 1)),
                             PT_r + views["kvt"], pr6)
                emit(s.act, lambda e: e.activation(out=olat, in_=ps6[:, 0:KVC * 128], func=AF.Identity), pr6, views["olat"])
                for cc in range(KVC):
                    emit(s.pe, lambda e, cc=cc, wuv=wuv: e.matmul(ps7[:, 128:256], lhsT=wuv[:, cc, :], rhs=olat[:, cc * 128:(cc + 1) * 128],
                                                                 start=(cc == 0), stop=(cc == KVC - 1)), wuv_r + views["olat"], pr7)
                emit(s.dve, lambda e, h=h: e.tensor_tensor(out=s.mid[:, h, HALO + t0:HALO + t0 + 128], in0=ps7[:, 128:256], in1=rinv, op=ALU.mult),
                     pr7 + rinv_r, [s.mid_r[h]])
        s.chk(10)
        if s.full:
            allt = []
            for v in views.values():
                allt += v[0].r + ([v[0].w] if v[0].w is not None else [])
            for r in s.hT_r:
                r.r = r.r + allt
        pre, epi3 = s.resid(gate)
        s.linear(w_out, r_out, 0, c.AW // 128, [(d * 128, 128) for d in range(DC)], s.midx, s.mid_r[0:NH], epi3, T, pre=pre)


def _rel_bucket(dist):
    import math
    dist = np.maximum(dist, 0)
    max_exact = 16
    large = max_exact + (np.log(np.maximum(dist, 1).astype(np.float32) / max_exact) / math.log(128 / max_exact)
                         * (32 - max_exact)).astype(np.int32)
    large = np.minimum(large, 31)
    return np.where(dist < max_exact, dist, large)


def _pvec(v, DC):
    v = np.asarray(v, np.float32)
    lead = int(np.prod(v.shape[:-1])) if v.ndim > 1 else 1
    return np.ascontiguousarray(v.reshape(lead, DC, 128).transpose(2, 0, 1).reshape(128, lead * DC))


def make_in_maps(cfg, inp):
    c = cfg
    D, DC, T = c.D, c.DC, c.T
    f = lambda a: np.asarray(a, np.float32)
    shared = {}
    shared["cT"] = np.ascontiguousarray(f(inp["c"]).reshape(4, DC, 128).transpose(2, 1, 0).reshape(128, DC * 4))
    shared["ada_t"] = _pvec(f(inp["ada_table"]), DC)
    shared["norm_mix"] = _pvec(f(inp["norm_mix"]), DC)
    shared["norm_ffn"] = _pvec(f(inp["norm_ffn"]), DC)
    shared["norm_final"] = _pvec(f(inp["norm_final"]), DC)
    shared["ident"] = np.eye(128, dtype=np.float32)
    ss, tt = np.meshgrid(np.arange(128), np.arange(128), indexing="ij")
    shared["tri"] = np.where(ss.T >= tt.T, 0.0, 0.0).astype(np.float32)
    tq, sk = np.meshgrid(np.arange(128), np.arange(128), indexing="ij")
    shared["tri"] = np.where(sk <= tq, 0.0, NEG).astype(np.float32)
    ohs = np.zeros((128, 2, 32, 128), np.float32)
    s_i, t_i = np.meshgrid(np.arange(128), np.arange(128), indexing="ij")
    for dl in range(2):
        delta = 1 - dl
        bk = _rel_bucket(delta * 128 + t_i - s_i)
        for b in range(32):
            ohs[:, dl, b, :] = (bk == b)
    shared["ohs"] = ohs.reshape(128, -1)
    shared["rbb"] = np.ascontiguousarray(np.broadcast_to(f(inp["rel_bias"]).reshape(1, 32 * c.NH), (128, 32 * c.NH)))
    shared["g_cq"] = _pvec(f(inp["att_g_cq"])[0], c.QL // 128)
    shared["g_ckv"] = _pvec(f(inp["att_g_ckv"])[0], c.KVL // 128)
    shared["sconv_cw"] = _pvec(f(inp["sconv_conv_w"])[0], DC)
    shared["conf_cw"] = _pvec(f(inp["conf_conv_w"])[0], DC)
    shared["conf_vec"] = _pvec(np.stack([f(inp["conf_conv_b"])[0], f(inp["conf_ln_g"])[0], f(inp["conf_ln_b"])[0]]), DC)
    shared["lru_cw"] = _pvec(f(inp["lru_conv_w"])[0], DC)
    shared["lru_vec"] = _pvec(np.stack([f(inp["lru_conv_b"])[0], f(inp["lru_b_a"])[0], f(inp["lru_b_x"])[0], f(inp["lru_lambda"])[0]]), DC)
    rt = f(inp["moe_router"])
    shared["router"] = np.ascontiguousarray(rt.reshape(rt.shape[0], DC, 128, 8).transpose(2, 0, 1, 3).reshape(128, rt.shape[0] * DC * 8))
    if shared["router"].shape[1] < 2 * DC * 8:
        shared["router"] = np.concatenate([shared["router"], np.zeros((128, 2 * DC * 8 - shared["router"].shape[1]), np.float32)], 1)

    wfull = {
        "att_w_in": f(inp["att_w_in"])[0],
        "att_w_qidx": f(inp["att_w_qidx"])[0],
        "att_w_uq": f(inp["att_w_uq"])[0],
        "att_w_ukT": np.ascontiguousarray(f(inp["att_w_uk"])[0].transpose(1, 2, 0)).reshape(c.AW, c.KVL),
        "att_w_uv": f(inp["att_w_uv"])[0].reshape(c.KVL, c.AW),
        "att_w_out": f(inp["att_w_out"])[0],
        "sconv_w_in": f(inp["sconv_w_in"])[0], "sconv_w_out": f(inp["sconv_w_out"])[0],
        "conf_w_in": f(inp["conf_w_in"])[0], "conf_w_out": f(inp["conf_w_out"])[0],
        "lru_w_in": f(inp["lru_w_in"])[0], "lru_w_out": f(inp["lru_w_out"])[0],
        "lru_w_a": f(inp["lru_w_a"])[0].reshape(D, c.LB), "lru_w_x": f(inp["lru_w_x"])[0].reshape(D, c.LB),
    }
    for l in range(2):
        wfull[f"ffn_w13_{l}"] = f(inp["ffn_w13"])[l]
        wfull[f"ffn_w2_{l}"] = f(inp["ffn_w2"])[l]
        wfull[f"moe_w13_{l}"] = f(inp["moe_w13"])[l].reshape(c.NE * D, 2 * c.DFE)
        wfull[f"moe_w2_{l}"] = f(inp["moe_w2"])[l].reshape(c.NE * c.DFE, D)
    ada_w = f(inp["ada_w"])
    ada_b = f(inp["ada_b"])
    x = f(inp["x"])
    NCOL = 6 * D // 8
    NCL = 6 * DC // 8
    maps = []
    for core in range(8):
        b, half = core // 2, core % 2
        m = dict(shared)
        m["xT"] = np.ascontiguousarray(x[b, half * T:(half + 1) * T, :].T)
        m["ada_w"] = np.ascontiguousarray(ada_w[:, core * NCOL:(core + 1) * NCOL])
        m["ada_b"] = np.ascontiguousarray(ada_b[core * NCOL:(core + 1) * NCOL].reshape(NCL, 128).T)
        bs = np.zeros((128, 4), np.float32)
        bs[:, b] = 1.0
        m["bsel"] = bs
        fl = np.zeros((128, 2), np.float32)
        fl[:, 0] = float(half)
        fl[:, 1] = 0.0 if half == 1 else NEG
        m["flag"] = fl
        for name, w in wfull.items():
            m[name] = w
        maps.append(m)
    return maps


_CACHE = {}


def run(cfg, inp, stop_after=None, only=None):
    key = (cfg.D, cfg.SEQ, stop_after, tuple(sorted(only)) if only else None)
    if key not in _CACHE:
        bld = B(cfg, stop_after, only)
        _CACHE[key] = (bld.build(), bld)
    nc = _CACHE[key][0]
    maps = make_in_maps(cfg, inp)
    res = run_bass_kernel_spmd(nc, maps, core_ids=list(range(8)))
    out = np.zeros((4, cfg.SEQ, cfg.D), np.float32)
    global LAST
    LAST = (res, _CACHE.get(key))
    for core in range(8):
        b, half = core // 2, core % 2
        out[b, half * cfg.T:(half + 1) * cfg.T, :] = np.asarray(res.results[core]["outT"]).T
    return out


def kernel(**inputs):
    return run(Cfg(), inputs)
```

```python
import types
import numpy as np
import ml_dtypes
from contextlib import ExitStack
import concourse.bass as bass
import concourse.mybir as mybir
from concourse.bass_utils import run_bass_kernel_spmd

F32 = mybir.dt.float32
BF16 = mybir.dt.bfloat16
AF = mybir.ActivationFunctionType
ALU = mybir.AluOpType
AX = mybir.AxisListType
NEG = -1.0e30
DEBUG = False
HALO = 32


class Cfg:
    def __init__(s, D=4096, SEQ=2048, NH=32, QL=1024, KVL=512, IH=64, TOPK=256, LH=16, DFF=8192, NE=8,
                 DFE=2048, DEPTH=4):
        s.D, s.SEQ, s.NH, s.QL, s.KVL, s.IH, s.LH, s.DFF, s.NE, s.DFE, s.DEPTH = D, SEQ, NH, QL, KVL, IH, LH, DFF, NE, DFE, DEPTH
        s.T = SEQ // 2
        s.S = SEQ
        s.KSEL = min(TOPK, SEQ // 4)
        s.DC = D // 128
        s.AW = NH * 128
        s.ATT_IN = QL + KVL + 128 + IH
        s.LB = D // LH
        s.TT = min(512, s.T)
        s.NT = s.T // s.TT
        s.TB = s.T // 128


class StopBuild(Exception):
    pass


class Res:
    def __init__(s, name):
        s.name, s.w, s.r = name, None, []


class Q:
    def __init__(s, nc, name, sems, is_dma):
        s.nc, s.name, s.sems, s.is_dma = nc, name, sems, is_dma
        s.n = 0
        s.seen = {}
        s.prog = []

    def wait_tok(s, tok):
        kind = tok[0]
        if kind == "c":
            _, F, n = tok
            if F is s and s.name == "pe":
                return
            if s.seen.get(F.name, 0) >= n:
                return
            s.seen[F.name] = n
            sem = F.sems[0]
            s.prog.append(lambda e, sem=sem, n=n: e.wait_ge(sem, n))
        else:
            _, sem, val, key = tok
            if s.seen.get(key, 0) >= val:
                return
            s.seen[key] = val
            s.prog.append(lambda e, sem=sem, val=val: e.wait_ge(sem, val))

    def issue(s, fn, own_sem=None):
        if own_sem is not None:
            s.prog.append(lambda e, fn=fn, sem=own_sem: fn(e).then_inc(sem))
            tok = ("d", own_sem, 1, id(own_sem))
            s.cc_toks = getattr(s, "cc_toks", []) + [tok]
            return tok
        if s.is_dma:
            K = len(s.sems)
            i = s.n
            s.n += 1
            slot = i % K
            sem = s.sems[slot]
            if i >= K:
                prev = 16 * (i // K)
                key = (s.name, slot)
                if s.seen.get(key, 0) < prev:
                    s.seen[key] = prev
                    s.prog.append(lambda e, sem=sem, prev=prev: e.wait_ge(sem, prev))
            s.prog.append(lambda e, fn=fn, sem=sem: fn(e).then_inc(sem, 16))
            return ("d", sem, 16 * (i // K + 1), (s.name, slot))
        s.n += 1
        sem = s.sems[0]
        s.prog.append(lambda e, fn=fn, sem=sem: fn(e).then_inc(sem, 1))
        return ("c", s, s.n)


def _freeze(fn):
    if fn.__closure__ is None:
        return fn
    cells = []
    for cell in fn.__closure__:
        try:
            cells.append(types.CellType(cell.cell_contents))
        except ValueError:
            cells.append(cell)
    return types.FunctionType(fn.__code__, fn.__globals__, fn.__name__, fn.__defaults__, tuple(cells))


def emit(q, fn, reads=(), writes=(), own_sem=None):
    fn = _freeze(fn)
    deps = []
    for t in reads:
        if t.w is not None:
            deps.append(t.w)
    for t in writes:
        if t.w is not None:
            deps.append(t.w)
        deps.extend(t.r)
    for d in deps:
        q.wait_tok(d)
    tok = q.issue(fn, own_sem)
    for t in reads:
        if tok[0] == "c":
            t.r = [x for x in t.r if not (x[0] == "c" and x[1] is tok[1])]
        t.r.append(tok)
    for t in writes:
        t.w = tok
        t.r = []
    return tok


class B:
    def __init__(s, cfg, stop_after=None, only=None):
        s.c = cfg
        s.stop_after = stop_after
        s.only = only
        s.debug = DEBUG
        s.nc = bass.Bass("TRN2", target_bir_lowering=False)
        s.es = ExitStack()
        s.din = {}
        s.nsem = 0

    def sem(s):
        s.nsem += 1
        return s.es.enter_context(s.nc.semaphore(f"s{s.nsem}"))

    def inp(s, name, shape, dt=F32):
        h = s.nc.dram_tensor(name, list(shape), dt, kind="ExternalInput").ap()
        s.din[name] = (tuple(shape), dt)
        return h

    def dram(s, name, shape, dt):
        return s.nc.dram_tensor(name, list(shape), dt).ap()

    def sb(s, name, shape, dt):
        return s.es.enter_context(s.nc.sbuf_tensor("sb_" + name, list(shape), dt))

    def weight(s, name, K, Fd):
        Fp = ((Fd + 127) // 128) * 128
        src = s.inp(name, [Fp, K])
        full = s.dram(name + "_f", [Fp, K], BF16)
        step = max(128, min(Fp, ((1 << 20) // K) // 128 * 128))
        chunks = [(r0, min(Fp, r0 + step)) for r0 in range(0, Fp, step)]
        rls = [Res(name + "_f") for _ in chunks]
        s.wts[name] = (src, full, chunks, rls)
        return full, (chunks, rls)

    def gather(s, name):
        src, full, chunks, rls = s.wts[name]
        for (r0, r1), rl in zip(chunks, rls):
            emit(s.pq, lambda e: e.dma_start(out=full[r0:r1, :], in_=src[r0:r1, :]), [], [rl])

    def gather2(s, loc, rls, full, rf, rows, Fd, dt, name):
        quad = s.dram(name + "_q", [4 * rows, Fd], dt)
        rq = Res(name + "_q")
        emit(s.pq, lambda e: e.collective_compute("AllGather", ALU.bypass, replica_groups=[[0, 1, 2, 3], [4, 5, 6, 7]],
                                                  ins=[loc.opt()], outs=[quad.opt()]), rls, [rq], own_sem=s.sem())
        emit(s.pq, lambda e: e.collective_compute("AllGather", ALU.bypass, replica_groups=[[0, 4], [1, 5], [2, 6], [3, 7]],
                                                  ins=[quad.opt()], outs=[full.opt()]), [rq], [rf], own_sem=s.sem())

    def pair_exchange(s, loc, rl, pair, rp):
        emit(s.pq, lambda e: e.collective_compute("AllGather", ALU.bypass, replica_groups=[[0, 1], [2, 3], [4, 5], [6, 7]],
                                                  ins=[loc.opt()], outs=[pair.opt()]), [rl], [rp], own_sem=s.sem())

    def linear(s, Wd, Wres, k0, KC, cols, xfn, xres, epi, N, pre=None):
        PF = len(s.wt) - 1
        n = len(cols)
        slots = {}
        xr = list(xres)
        Wch = Wres

        def load(j):
            c0, w = cols[j]
            sl = s.wcount % len(s.wt)
            s.wcount += 1
            slots[j] = sl
            wt, wr = s.wt[sl], s.wt_r[sl]
            assert c0 % 128 == 0
            wdep = [rl for (r0, r1), rl in zip(Wch[0], Wch[1]) if r0 < c0 + 128 and r1 > c0]
            emit(s.sq, lambda e, wt=wt, c0=c0, w=w: e.dma_start(
                out=wt[:, 0:KC, 0:w], in_=Wd[c0:c0 + 128, k0:k0 + KC * 128].rearrange("p (kc f) -> p kc f", f=128)[:, :, 0:w]),
                wdep, [wr])
            if pre is not None:
                pre(j)

        for j in range(min(PF, n)):
            load(j)
        for j in range(n):
            if j + PF < n:
                load(j + PF)
            c0, w = cols[j]
            sl = slots[j]
            wt, wr = s.wt[sl], s.wt_r[sl]
            if N > 512:
                pi = s.pcount % 3
                ps = s.psum[:, pi * 1024:(pi + 1) * 1024]
                pr = [s.bank_r[2 * pi], s.bank_r[2 * pi + 1]]
            else:
                pi = 4 + s.pcount % 2
                ps = s.bank[pi]
                pr = [s.bank_r[pi]]
            s.pcount += 1
            for n0 in range(0, N, 512):
                n1 = min(N, n0 + 512)
                for kc in range(KC):
                    rhs = xfn(kc, n0, n1)
                    emit(s.pe, lambda e, ps=ps, wt=wt, kc=kc, w=w, n0=n0, n1=n1, rhs=rhs: e.matmul(
                        ps[0:w, n0:n1], lhsT=wt[:, kc, 0:w], rhs=rhs, start=(kc == 0), stop=(kc == KC - 1)),
                        [wr] + xr, pr)
            epi(j, ps, pr, w)

    def next_xt(s):
        sl = s.xcount % len(s.xt)
        s.xcount += 1
        return s.xt[sl], s.xt_r[sl]

    def resid(s, gate):
        c = s.c
        slots = {}

        def pre(j):
            xt, xr = s.next_xt()
            slots[j] = (xt, xr)
            emit(s.sq, lambda e: e.dma_start(out=xt, in_=s.xs[j * 128:(j + 1) * 128, :]), [s.xs_r[j]], [xr])

        def epi(j, ps, pr, w):
            xt, xr = slots[j]
            emit(s.dve, lambda e: e.scalar_tensor_tensor(out=xt, in0=ps[:, 0:c.T], scalar=gate[:, j:j + 1], in1=xt,
                                                         op0=ALU.mult, op1=ALU.add), pr + [xr, s.mod_r], [xr])
            emit(s.sq, lambda e: e.dma_start(out=s.xs[j * 128:(j + 1) * 128, :], in_=xt), [xr], [s.xs_r[j]])

        return pre, epi

    def colsum_bc(s, acc, acc_r, out, out_r, scale, eps, power):
        c = s.c
        ps, pr = s.bank[6], [s.bank_r[6]]
        for n0 in range(0, c.T, 512):
            n1 = min(c.T, n0 + 512)
            emit(s.pe, lambda e, n0=n0, n1=n1: e.matmul(ps[:, 0:n1 - n0], lhsT=s.ones32[:, :], rhs=acc[:, n0:n1], start=True, stop=True),
                 acc_r + [s.const_r], pr)
            emit(s.dve, lambda e, n0=n0, n1=n1: e.tensor_scalar(out=out[:, n0:n1], in0=ps[:, 0:n1 - n0], scalar1=scale, scalar2=eps,
                                                                  op0=ALU.mult, op1=ALU.add), pr, out_r)
        if power is not None:
            s.rpow(out, out_r, power)

    def rpow(s, out, out_r, power):
        emit(s.act, lambda e: e.activation(out=out, in_=out, func=AF.Ln), out_r, out_r)
        emit(s.act, lambda e: e.activation(out=out, in_=out, func=AF.Exp, scale=power), out_r, out_r)

    def norm(s, gs, sh, out_fn=None, hook=None):
        c = s.c
        acc, acc_r = s.st[0], [s.st_r[0]]
        for dc in range(c.DC):
            xt, xr = s.next_xt()
            emit(s.sq, lambda e: e.dma_start(out=xt, in_=s.xs[dc * 128:(dc + 1) * 128, :]), [s.xs_r[dc]], [xr])
            if dc == 0:
                emit(s.act, lambda e: e.activation(out=acc, in_=xt, func=AF.Square), [xr], acc_r)
            else:
                tq, tr = s.tmp[dc % 2], s.tmp_r[dc % 2]
                emit(s.act, lambda e: e.activation(out=tq, in_=xt, func=AF.Square), [xr], [tr])
                emit(s.dve, lambda e: e.tensor_tensor(out=acc, in0=acc, in1=tq, op=ALU.add), [tr] + acc_r, acc_r)
        rstd, rstd_r = s.st[1], [s.st_r[1]]
        s.colsum_bc(acc, acc_r, rstd, rstd_r, 1.0 / c.D, 1e-6, -0.5)
        if "rstd" not in s.dbg_map:
            s.dbg("acc", acc, acc_r, c.T)
            s.dbg("rstd", rstd, rstd_r, c.T)
        for dc in range(c.DC):
            xt, xr = s.next_xt()
            emit(s.sq, lambda e: e.dma_start(out=xt, in_=s.xs[dc * 128:(dc + 1) * 128, :]), [s.xs_r[dc]], [xr])
            emit(s.dve, lambda e: e.tensor_tensor(out=xt, in0=xt, in1=rstd, op=ALU.mult), [xr] + rstd_r, [xr])
            bias = sh[:, dc:dc + 1] if sh is not None else 0.0
            if out_fn is not None:
                out_fn(dc, xt, xr, gs)
            elif hook is not None:
                tq, tr = s.tmp[dc % 2], s.tmp_r[dc % 2]
                emit(s.act, lambda e: e.activation(out=tq, in_=xt, func=AF.Identity, bias=bias, scale=gs[:, dc:dc + 1]),
                     [xr, s.mod_r], [tr])
                emit(s.dve, lambda e: e.tensor_copy(out=s.hT[:, dc, :], in_=tq), [tr], [s.hT_r[dc]])
                hook(dc, tq, tr)
            else:
                emit(s.act, lambda e: e.activation(out=s.hT[:, dc, :], in_=xt, func=AF.Identity, bias=bias, scale=gs[:, dc:dc + 1]),
                     [xr, s.mod_r], [s.hT_r[dc]])
                if dc == 0 and "h0" not in s.dbg_map:
                    s.dbg("h0", s.hT[:, 0, :], [s.hT_r[0]], c.T)

    def hx(s, kc, n0, n1):
        return s.hT[:, kc, n0:n1]

    def midx(s, kc, n0, n1):
        return s.mid[:, kc, HALO + n0:HALO + n1]

    def midc(s, dc):
        return s.mid[:, dc, HALO:HALO + s.c.T]

    def halo_exchange(s, tag):
        c = s.c
        loc = s.dram(f"halo_l{tag}", [c.D, HALO], BF16)
        pair = s.dram(f"halo_p{tag}", [2 * c.D, HALO], BF16)
        rl, rp = Res("hl"), Res("hp")
        emit(s.pq, lambda e: e.dma_start(out=loc.rearrange("(dc p) h -> p dc h", p=128), in_=s.mid[:, :, c.T:c.T + HALO]), s.mid_r, [rl])
        s.pair_exchange(loc, rl, pair, rp)
        emit(s.sq, lambda e: e.dma_start(out=s.mid[:, :, 0:HALO], in_=pair[0:c.D, :].rearrange("(dc p) h -> p dc h", p=128)), [rp], s.mid_r)
        emit(s.dve, lambda e: e.tensor_scalar(out=s.mid[:, :, 0:HALO], in0=s.mid[:, :, 0:HALO], scalar1=s.flag[:, 0:1], scalar2=None, op0=ALU.mult),
             s.mid_r + [s.const_r], s.mid_r)

    def dwconv(s, dc, cw, K, out, out_r, bias=None):
        c = s.c
        for k in range(K):
            off = HALO - (K - 1) + k
            src = s.mid[:, dc, off:off + c.T]
            wk = cw[:, k * c.DC + dc:k * c.DC + dc + 1]
            if k == 0:
                if bias is not None:
                    emit(s.dve, lambda e, src=src, wk=wk: e.tensor_scalar(out=out, in0=src, scalar1=wk, scalar2=bias[:, dc:dc + 1],
                                                                        op0=ALU.mult, op1=ALU.add), [s.mid_r[dc], s.cv_r], out_r)
                else:
                    emit(s.dve, lambda e, src=src, wk=wk: e.tensor_scalar(out=out, in0=src, scalar1=wk, scalar2=None, op0=ALU.mult),
                         [s.mid_r[dc], s.cv_r], out_r)
            else:
                emit(s.dve, lambda e, src=src, wk=wk: e.scalar_tensor_tensor(out=out, in0=src, scalar=wk, in1=out,
                                                                           op0=ALU.mult, op1=ALU.add), [s.mid_r[dc], s.cv_r] + out_r, out_r)

    def load_small(s, dst, src, res):
        emit(s.sq, lambda e: e.dma_start(out=dst, in_=src), [], [res])

    def build(s):
        c = s.c
        nc = s.nc
        es = s.es
        D, T, DC = c.D, c.T, c.DC
        s.pe = Q(nc, "pe", [s.sem()], False)
        s.act = Q(nc, "act", [s.sem()], False)
        s.dve = Q(nc, "dve", [s.sem()], False)
        s.sq = Q(nc, "sq", [s.sem() for _ in range(8)], True)
        s.pq = Q(nc, "pq", [s.sem() for _ in range(8)], True)
        s.wts = {}
        s.wcount = s.pcount = s.xcount = 0

        xT_in = s.inp("xT", [D, T])
        out_d = nc.dram_tensor("outT", [D, T], F32, kind="ExternalOutput").ap()
        s.dbg_d = nc.dram_tensor("dbg", [128, 8192], F32, kind="ExternalOutput").ap() if s.debug else None
        s.dbg_off = 0
        s.dbg_map = {}
        cT_in = s.inp("cT", [128, DC * 4])
        adaw_in = s.inp("ada_w", [D, 6 * D // 8])
        adab_in = s.inp("ada_b", [128, 6 * DC // 8])
        adat_in = s.inp("ada_t", [128, c.DEPTH * 6 * DC])
        nmix_in = s.inp("norm_mix", [128, c.DEPTH * DC])
        nffn_in = s.inp("norm_ffn", [128, c.DEPTH * DC])
        nfin_in = s.inp("norm_final", [128, DC])
        bsel_in = s.inp("bsel", [128, 4])
        flag_in = s.inp("flag", [128, 2])
        ident_in = s.inp("ident", [128, 128])
        tri_in = s.inp("tri", [128, 128])
        s.ohs_in = s.inp("ohs", [128, 2 * 32 * 128])
        s.rbb_in = s.inp("rbb", [128, 32 * c.NH])
        s.gcq_in = s.inp("g_cq", [128, c.QL // 128])
        s.gckv_in = s.inp("g_ckv", [128, c.KVL // 128])
        s.scw_in = s.inp("sconv_cw", [128, 3 * DC])
        s.cfw_in = s.inp("conf_cw", [128, 31 * DC])
        s.cfv_in = s.inp("conf_vec", [128, 3 * DC])
        s.lrw_in = s.inp("lru_cw", [128, 4 * DC])
        s.lrv_in = s.inp("lru_vec", [128, 4 * DC])
        s.rt_in = s.inp("router", [128, 2 * DC * 8])

        s.xs = s.dram("xs", [D, T], F32)
        s.xs_r = [Res(f"xs{i}") for i in range(DC)]

        s.full = (c.D == 4096)
        s.hT = s.sb("hT", [128, DC, T], BF16)
        s.hT_r = [Res(f"hT{i}") for i in range(DC)]
        s.mid = s.sb("mid", [128, DC, T + HALO], BF16)
        s.mid_r = [Res(f"mid{i}") for i in range(DC)]
        NW = 2 if s.full else 3
        s.wt = [s.sb(f"wt{i}", [128, DC, 128], BF16) for i in range(NW)]
        s.wt_r = [Res(f"wt{i}") for i in range(NW)]
        s.scr = s.sb("scr", [128, 7 * T], F32)
        s.xt = [s.scr[:, i * T:(i + 1) * T] for i in range(2)]
        s.xt_r = [Res(f"xt{i}") for i in range(2)]
        s.tmp = [s.scr[:, (2 + i) * T:(3 + i) * T] for i in range(2)]
        s.tmp_r = [Res(f"tmp{i}") for i in range(2)]
        s.st = [s.scr[:, (4 + i) * T:(5 + i) * T] for i in range(2)]
        s.st_r = [Res(f"st{i}") for i in range(2)]
        s.px = s.scr[:, 6 * T:7 * T]
        s.px_r = Res("px")
        s.xt = s.xt + [s.px]
        s.xt_r = s.xt_r + [s.px_r]
        psum = es.enter_context(nc.psum_tensor("psum", [128, 4096], F32))
        s.bank = [psum[:, i * 512:(i + 1) * 512] for i in range(8)]
        s.bank_r = [Res(f"bank{i}") for i in range(8)]
        s.psum = psum
        s.const_r = Res("const")
        s.ones32 = s.sb("ones32", [128, 128], F32)
        s.ident32 = s.sb("ident32", [128, 128], F32)
        s.identb = s.sb("identb", [128, 128], BF16)
        s.onesb = s.sb("onesb", [128, 128], BF16)
        s.tri = s.sb("tri", [128, 128], F32)
        s.flag = s.sb("flag", [128, 2], F32)
        bsel = s.sb("bsel", [128, 4], F32)
        modsel = s.sb("modsel", [128, 6 * DC], F32)
        s.modL = s.sb("modL", [128, c.DEPTH * 6 * DC], F32)
        nmix = s.sb("nmix", [128, c.DEPTH * DC], F32)
        nffn = s.sb("nffn", [128, c.DEPTH * DC], F32)
        s.nfin = s.sb("nfin", [128, DC], F32)
        s.rtf = s.sb("rtf", [128, DC * 8], F32)
        s.rtf_r = Res("rtf")
        s.gtok = s.sb("gtok", [128, c.TB, 8], F32)
        s.gtok_r = Res("gtok")
        s.sm = [s.sb(f"sm{i}", [128, 16], F32) for i in range(3)]
        s.sm_r = Res("sm")
        s.gb = s.sb("gb", [128, 128], F32)
        s.gb_r = Res("gb")
        s.EB = s.sb("EB", [128, c.NH * 256], BF16)
        s.EB_r = Res("EB")
        s.cvw = s.sb("cvw", [128, 31 * DC], F32)
        s.cvv = s.sb("cvv", [128, 4 * DC], F32)
        s.cv_r = Res("cv")
        s.vec = s.sb("vec", [128, 4 * DC], F32)
        s.vec_r = Res("vec")
        s.mod_r = Res("modL")
        small_r = Res("small")

        for dst, src in ((s.ident32[:, :], ident_in), (s.tri[:, :], tri_in), (s.flag[:, :], flag_in), (bsel[:, :], bsel_in),
                         (s.modL[:, :], adat_in), (nmix[:, :], nmix_in), (nffn[:, :], nffn_in), (s.nfin[:, :], nfin_in)):
            s.load_small(dst, src, small_r)
        emit(s.dve, lambda e: e.memset(s.ones32[:, :], 1.0), [], [s.const_r])
        emit(s.dve, lambda e: e.memset(s.onesb[:, :], 1.0), [s.const_r], [s.const_r])
        emit(s.dve, lambda e: e.tensor_copy(out=s.identb[:, :], in_=s.ident32[:, :]), [small_r, s.const_r], [s.const_r])

        for dc in range(DC):
            emit(s.sq, lambda e, dc=dc: e.dma_start(out=s.xs[dc * 128:(dc + 1) * 128, :], in_=xT_in[dc * 128:(dc + 1) * 128, :]), [], [s.xs_r[dc]])

        W = {}
        s.W = W
        decls = [("att_w_in", D, c.ATT_IN), ("att_w_qidx", c.QL, c.IH * 128), ("att_w_uq", c.QL, c.AW), ("att_w_ukT", c.AW, c.KVL),
                 ("att_w_uv", c.KVL, c.AW), ("att_w_out", c.AW, D), ("ffn_w13_0", D, 2 * c.DFF), ("ffn_w2_0", c.DFF, D),
                 ("sconv_w_in", D, 3 * D), ("sconv_w_out", D, D), ("moe_w13_0", D, c.NE * 2 * c.DFE), ("moe_w2_0", c.DFE, c.NE * D),
                 ("conf_w_in", D, 2 * D), ("conf_w_out", D, D), ("ffn_w13_1", D, 2 * c.DFF), ("ffn_w2_1", c.DFF, D),
                 ("lru_w_in", D, 2 * D), ("lru_w_a", D, c.LB), ("lru_w_x", D, c.LB), ("lru_w_out", D, D),
                 ("moe_w13_1", D, c.NE * 2 * c.DFE), ("moe_w2_1", c.DFE, c.NE * D)]
        for (name, K, Fd) in decls:
            W[name] = s.weight(name, K, Fd)
        order = [d[0] for d in decls]
        gi = [0]

        def gather_upto(name):
            while gi[0] < len(order) and gi[0] <= order.index(name):
                s.gather(order[gi[0]])
                gi[0] += 1

        s.cast_upto = gather_upto

        NCL = 6 * DC // 8
        cT = s.st[0]
        emit(s.sq, lambda e: e.dma_start(out=cT[:, 0:DC * 4], in_=cT_in), [], [s.st_r[0]])
        scT = s.sb("scT", [128, DC * 4], BF16)
        scT_r = Res("scT")
        emit(s.act, lambda e: e.activation(out=scT[:, :], in_=cT[:, 0:DC * 4], func=AF.Silu), [s.st_r[0]], [scT_r])
        adab = s.sb("adab", [128, NCL], F32)
        s.load_small(adab[:, :], adab_in, small_r)
        modp = s.sb("modp", [128, NCL * 4], F32)
        modp_r = Res("modp")
        for j in range(NCL):
            sl = s.wcount % len(s.wt)
            s.wcount += 1
            wt, wr = s.wt[sl], s.wt_r[sl]
            emit(s.pq, lambda e, wt=wt, j=j: e.dma_start(out=wt[:, 0:DC, :], in_=adaw_in[:, j * 128:(j + 1) * 128].rearrange("(kc p) f -> p kc f", p=128)),
                 [], [wr])
            ps, pr = s.bank[6 + j % 2], [s.bank_r[6 + j % 2]]
            for kc in range(DC):
                emit(s.pe, lambda e, ps=ps, wt=wt, kc=kc: e.matmul(ps[:, 0:4], lhsT=wt[:, kc, :], rhs=scT[:, kc * 4:(kc + 1) * 4],
                                                                   start=(kc == 0), stop=(kc == DC - 1)), [wr, scT_r], pr)
            emit(s.dve, lambda e, ps=ps, j=j: e.tensor_scalar(out=modp[:, j * 4:(j + 1) * 4], in0=ps[:, 0:4], scalar1=adab[:, j:j + 1], scalar2=None,
                                                              op0=ALU.add), pr + [small_r], [modp_r])
        modl_d = s.dram("modl", [NCL * 128, 4], F32)
        modf_d = s.dram("modf", [6 * D, 4], F32)
        rl, rf = Res("modl"), Res("modf")
        emit(s.pq, lambda e: e.dma_start(out=modl_d.rearrange("(j p) b -> p j b", p=128), in_=modp[:, :].rearrange("p (j b) -> p j b", b=4)), [modp_r], [rl])
        s.gather2(modl_d, [rl], modf_d, rf, NCL * 128, 4, F32, "modg")
        modall = s.st[1]
        m3 = modall[:, 0:6 * DC * 4].rearrange("p (j b) -> p j b", b=4)
        emit(s.sq, lambda e: e.dma_start(out=m3, in_=modf_d.rearrange("(j p) b -> p j b", p=128)), [rf], [s.st_r[1]])
        ms_r = Res("modsel")
        emit(s.dve, lambda e: e.tensor_scalar(out=modsel[:, :], in0=m3[:, :, 0], scalar1=bsel[:, 0:1], scalar2=None, op0=ALU.mult), [s.st_r[1], small_r], [ms_r])
        for b in range(1, 4):
            emit(s.dve, lambda e, b=b: e.scalar_tensor_tensor(out=modsel[:, :], in0=m3[:, :, b], scalar=bsel[:, b:b + 1], in1=modsel[:, :],
                                                              op0=ALU.mult, op1=ALU.add), [s.st_r[1], ms_r], [ms_r])
        mod_r = s.mod_r
        for i in range(c.DEPTH):
            o = i * 6 * DC
            emit(s.dve, lambda e, o=o: e.tensor_tensor(out=s.modL[:, o:o + 6 * DC], in0=s.modL[:, o:o + 6 * DC],
                                                       in1=modsel[:, :], op=ALU.add), [ms_r, small_r, mod_r], [mod_r])
            for (j, nw) in ((1, nmix), (4, nffn)):
                emit(s.dve, lambda e, o=o, j=j, nw=nw, i=i: e.scalar_tensor_tensor(
                    out=s.modL[:, o + j * DC:o + (j + 1) * DC], in0=s.modL[:, o + j * DC:o + (j + 1) * DC], scalar=1.0,
                    in1=nw[:, i * DC:(i + 1) * DC], op0=ALU.add, op1=ALU.mult), [mod_r, small_r], [mod_r])

        def M(i, j):
            o = i * 6 * DC + j * DC
            return s.modL[:, o:o + DC]

        gather_upto("att_w_out")

        s.dbg("modL", s.modL[:, :], [mod_r], c.DEPTH * 6 * DC)
        s.dbg("modsel", modsel[:, :], [ms_r], 6 * DC)
        s.dbg("modp", modp[:, :], [modp_r], NCL * 4)

        done = False
        try:
            s.layers(M, gather_upto)
        except StopBuild:
            done = True
        for i in range(0):
            if s.stop_after is not None and s.stop_after[0] == "pre":
                done = True
                break
            kind = i % 4
            if kind == 0:
                s.attention(M(i, 1), M(i, 0), M(i, 2))
                gather_upto("moe_w2_0")
            elif kind == 1:
                s.sconv(M(i, 1), M(i, 0), M(i, 2))
                gather_upto("ffn_w2_1")
            elif kind == 2:
                s.conformer(M(i, 1), M(i, 0), M(i, 2))
                gather_upto("moe_w2_1")
            else:
                s.rglru(M(i, 1), M(i, 0), M(i, 2))
            if s.stop_after == ("mix", i):
                done = True
                break
            if i % 2 == 0:
                s.norm(M(i, 4), M(i, 3))
                s.ffn(W[f"ffn_w13_{i // 2}"], W[f"ffn_w2_{i // 2}"], M(i, 5))
            else:
                s.moe(W[f"moe_w13_{i // 2}"], W[f"moe_w2_{i // 2}"], M(i, 4), M(i, 3), M(i, 5), i // 2)
            if s.stop_after == ("ffn", i):
                done = True
                break

        s.out_r = Res("out")

        def fin(dc, xt, xr, gs):
            emit(s.act, lambda e: e.activation(out=xt, in_=xt, func=AF.Identity, scale=gs[:, dc:dc + 1]), [xr, small_r], [xr])
            emit(s.sq, lambda e: e.dma_start(out=out_d[dc * 128:(dc + 1) * 128, :], in_=xt), [xr], [s.out_r])

        if not done:
            s.norm(s.nfin, None, out_fn=fin)
        else:
            for dc in range(DC):
                xt, xr = s.next_xt()
                emit(s.sq, lambda e: e.dma_start(out=xt, in_=s.xs[dc * 128:(dc + 1) * 128, :]), [s.xs_r[dc]], [xr])
                emit(s.pq, lambda e: e.dma_start(out=out_d[dc * 128:(dc + 1) * 128, :], in_=xt), [xr], [s.out_r])
        for tok in getattr(s.pq, "cc_toks", []):
            s.pq.wait_tok(tok)
        for q in (s.pe, s.act, s.dve):
            if q.n > 0:
                s.pq.wait_tok(("c", q, q.n))
        for q in (s.pq, s.sq):
            K = len(q.sems)
            for i in range(max(0, q.n - K), q.n):
                q.wait_tok(("d", q.sems[i % K], 16 * (i // K + 1), (q.name, i % K)))

        with nc.Block() as block:
            @block.tensor
            def _(e):
                for f in s.pe.prog:
                    f(e)

            @block.scalar
            def _(e):
                for f in s.act.prog:
                    f(e)

            @block.vector
            def _(e):
                for f in s.dve.prog:
                    f(e)

            @block.sync
            def _(e):
                for f in s.sq.prog:
                    f(e)

            @block.gpsimd
            def _(e):
                for f in s.pq.prog:
                    f(e)
        es.close()
        return nc

    def dbg(s, name, ap, res, n):
        if not s.debug or s.dbg_off + n > 8192:
            return
        o = s.dbg_off
        s.dbg_off += n
        s.dbg_map[name] = (o, n)
        emit(s.pq, lambda e: e.dma_start(out=s.dbg_d[:, o:o + n], in_=ap), res, [Res("dbg")])

    def chk(s, tag, kind="att"):
        if s.stop_after == (kind, tag):
            raise StopBuild()

    def layers(s, M, gather_upto):
        c = s.c
        W = s.W
        for i in range(c.DEPTH):
            if s.stop_after is not None and s.stop_after[0] == "pre":
                raise StopBuild()
            kind = i % 4
            if s.only is None or f"mix{i}" in s.only:
                gather_upto(["att_w_out", "sconv_w_out", "conf_w_out", "lru_w_out"][kind])
                if kind == 0:
                    s.attention(M(i, 1), M(i, 0), M(i, 2))
                elif kind == 1:
                    s.sconv(M(i, 1), M(i, 0), M(i, 2))
                elif kind == 2:
                    s.conformer(M(i, 1), M(i, 0), M(i, 2))
                else:
                    s.rglru(M(i, 1), M(i, 0), M(i, 2))
            if s.stop_after == ("mix", i):
                raise StopBuild()
            if s.only is None or f"ffn{i}" in s.only:
                gather_upto(["ffn_w2_0", "moe_w2_0", "ffn_w2_1", "moe_w2_1"][i])
                if i % 2 == 0:
                    s.norm(M(i, 4), M(i, 3))
                    s.ffn(W[f"ffn_w13_{i // 2}"], W[f"ffn_w2_{i // 2}"], M(i, 5))
                else:
                    s.moe(W[f"moe_w13_{i // 2}"], W[f"moe_w2_{i // 2}"], M(i, 4), M(i, 3), M(i, 5), i // 2)
            if s.stop_after == ("ffn", i):
                raise StopBuild()

    def swiglu_epi(s, gbc=None, gbc_r=None):
        c = s.c

        def epi(idx, ps, pr, w):
            jj, which = idx // 2, idx % 2
            tq, tr = s.tmp[jj % 2], s.tmp_r[jj % 2]
            if which == 0:
                emit(s.act, lambda e: e.activation(out=tq, in_=ps[:, 0:c.T], func=AF.Silu), pr, [tr])
                if gbc is not None:
                    emit(s.dve, lambda e: e.tensor_tensor(out=tq, in0=tq, in1=gbc, op=ALU.mult), [tr] + gbc_r, [tr])
            else:
                emit(s.dve, lambda e: e.tensor_tensor(out=s.midc(jj), in0=ps[:, 0:c.T], in1=tq, op=ALU.mult), pr + [tr], [s.mid_r[jj]])
        return epi

    def ffn(s, W13, W2, gate):
        c = s.c
        (w13, r13), (w2, r2) = W13, W2
        G = c.DC
        HC = c.DFF // 128
        for g in range(HC // G):
            cols = []
            for jj in range(G):
                j = g * G + jj
                cols.append((j * 128, 128))
                cols.append((c.DFF + j * 128, 128))
            s.linear(w13, r13, 0, c.DC, cols, s.hx, s.hT_r, s.swiglu_epi(), c.T)
            if "act0" not in s.dbg_map:
                s.dbg("act0", s.midc(0), [s.mid_r[0]], c.T)
            pre, epi2 = s.resid(gate)
            s.linear(w2, r2, g * G * 128, G, [(d * 128, 128) for d in range(c.DC)], s.midx, s.mid_r, epi2, c.T, pre=pre)

    def moe(s, W13, W2, gs, sh, gate, li):
        c = s.c
        (w13, r13), (w2, r2) = W13, W2
        DC, T = c.DC, c.T
        rtf = s.rtf
        emit(s.sq, lambda e: e.dma_start(out=rtf[:, :], in_=s.rt_in[:, li * DC * 8:(li + 1) * DC * 8]), [], [s.rtf_r])
        lgps = s.psum[:, 2048:3072]
        lgps_r = [s.bank_r[4], s.bank_r[5]]

        def hook(dc, tq, tr):
            for n0 in range(0, T, 512):
                n1 = min(T, n0 + 512)
                emit(s.pe, lambda e, n0=n0, n1=n1: e.matmul(lgps[0:8, n0:n1], lhsT=rtf[:, dc * 8:(dc + 1) * 8], rhs=tq[:, n0:n1],
                                                            start=(dc == 0), stop=(dc == DC - 1)), [tr, s.rtf_r], lgps_r)

        s.norm(gs, sh, hook=hook)
        s.chk(1, "moe")
        lgT = s.st[0]
        emit(s.dve, lambda e: e.tensor_copy(out=lgT[0:8, :], in_=lgps[0:8, 0:T]), lgps_r, [s.st_r[0]])
        gtok = s.gtok
        l8, m8, ex = s.sm[0], s.sm[1], s.sm[2]
        r8 = [s.sm_r]
        for tb in range(c.TB):
            ps, pr = s.bank[6], [s.bank_r[6]]
            emit(s.pe, lambda e, tb=tb: e.matmul(ps[:, 0:8], lhsT=lgT[0:8, tb * 128:(tb + 1) * 128], rhs=s.ident32[0:8, 0:8], start=True, stop=True),
                 [s.st_r[0], s.const_r], pr)
            emit(s.dve, lambda e: e.tensor_copy(out=l8[:, 0:8], in_=ps[:, 0:8]), pr, r8)
            emit(s.dve, lambda e: e.max(out=m8[:, 0:8], in_=l8[:, 0:8]), r8, r8)
            emit(s.dve, lambda e: e.tensor_scalar(out=ex[:, 8:9], in0=m8[:, 0:1], scalar1=-1.0, scalar2=None, op0=ALU.mult), r8, r8)
            emit(s.act, lambda e: e.activation(out=ex[:, 0:8], in_=l8[:, 0:8], func=AF.Exp, bias=ex[:, 8:9], scale=1.0), r8, r8)
            emit(s.dve, lambda e: e.scalar_tensor_tensor(out=ex[:, 0:8], in0=l8[:, 0:8], scalar=m8[:, 1:2], in1=ex[:, 0:8], op0=ALU.is_ge, op1=ALU.mult),
                 r8, r8)
            emit(s.dve, lambda e: e.tensor_reduce(out=ex[:, 9:10], in_=ex[:, 0:8], axis=AX.X, op=ALU.add), r8, r8)
            emit(s.dve, lambda e: e.reciprocal(out=ex[:, 9:10], in_=ex[:, 9:10]), r8, r8)
            emit(s.dve, lambda e, tb=tb: e.tensor_scalar(out=gtok[:, tb, :], in0=ex[:, 0:8], scalar1=ex[:, 9:10], scalar2=None, op0=ALU.mult),
                 r8, [s.gtok_r])
        s.chk(2, "moe")
        HCE = c.DFE // 128
        for ei in range(c.NE):
            gps = s.psum[:, 2048:3072]
            gpr = [s.bank_r[4], s.bank_r[5]]
            for tb in range(c.TB):
                emit(s.dve, lambda e, tb=tb: e.tensor_copy(out=s.gb[:, :], in_=gtok[:, tb, ei:ei + 1].to_broadcast([128, 128])), [s.gtok_r], [s.gb_r])
                emit(s.pe, lambda e, tb=tb: e.matmul(gps[:, tb * 128:(tb + 1) * 128], lhsT=s.gb[:, :], rhs=s.ident32[:, :], start=True, stop=True),
                     [s.gb_r, s.const_r], gpr)
            gbc, gbc_r = s.st[1], [s.st_r[1]]
            emit(s.act, lambda e: e.activation(out=gbc, in_=gps[:, 0:T], func=AF.Identity), gpr, gbc_r)
            s.chk(3, "moe")
            cols = []
            for jj in range(HCE):
                cols.append((ei * 2 * c.DFE + jj * 128, 128))
                cols.append((ei * 2 * c.DFE + c.DFE + jj * 128, 128))
            s.linear(w13, r13, 0, DC, cols, s.hx, s.hT_r, s.swiglu_epi(gbc, gbc_r), T)
            s.chk(4, "moe")
            pre, epi2 = s.resid(gate)
            s.linear(w2, r2, 0, HCE, [(ei * c.D + d * 128, 128) for d in range(DC)], s.midx, s.mid_r[0:HCE], epi2, T, pre=pre)

    def sconv(s, gs, sh, gate):
        c = s.c
        D, DC, T = c.D, c.DC, c.T
        (w_in, r_in), (w_out, r_out) = s.W["sconv_w_in"], s.W["sconv_w_out"]
        s.load_small(s.cvw[:, 0:3 * DC], s.scw_in, s.cv_r)
        s.norm(gs, sh)
        cols = []
        for dc in range(DC):
            cols += [(D + dc * 128, 128), (2 * D + dc * 128, 128)]

        def epi1(idx, ps, pr, w):
            dc, which = idx // 2, idx % 2
            tq, tr = s.tmp[dc % 2], s.tmp_r[dc % 2]
            if which == 0:
                emit(s.act, lambda e: e.activation(out=tq, in_=ps[:, 0:T], func=AF.Identity), pr, [tr])
            else:
                emit(s.dve, lambda e: e.tensor_tensor(out=s.midc(dc), in0=ps[:, 0:T], in1=tq, op=ALU.mult), pr + [tr], [s.mid_r[dc]])

        s.linear(w_in, r_in, 0, DC, cols, s.hx, s.hT_r, epi1, T)
        s.halo_exchange("sc")
        if s.only is None:
            s.cast_upto("conf_w_out")

        def epi2(dc, ps, pr, w):
            tq, tr = s.tmp[dc % 2], [s.tmp_r[dc % 2]]
            s.dwconv(dc, s.cvw, 3, tq, tr)
            emit(s.dve, lambda e: e.tensor_tensor(out=s.midc(dc), in0=ps[:, 0:T], in1=tq, op=ALU.mult), pr + tr, [s.mid_r[dc]])

        s.linear(w_in, r_in, 0, DC, [(dc * 128, 128) for dc in range(DC)], s.hx, s.hT_r, epi2, T)
        pre, epi3 = s.resid(gate)
        s.linear(w_out, r_out, 0, DC, [(d * 128, 128) for d in range(DC)], s.midx, s.mid_r, epi3, T, pre=pre)

    def conformer(s, gs, sh, gate):
        c = s.c
        D, DC, T = c.D, c.DC, c.T
        (w_in, r_in), (w_out, r_out) = s.W["conf_w_in"], s.W["conf_w_out"]
        s.load_small(s.cvw[:, 0:31 * DC], s.cfw_in, s.cv_r)
        s.load_small(s.cvv[:, 0:3 * DC], s.cfv_in, s.cv_r)
        s.norm(gs, sh)
        cols = []
        for dc in range(DC):
            cols += [(D + dc * 128, 128), (dc * 128, 128)]

        def epi1(idx, ps, pr, w):
            dc, which = idx // 2, idx % 2
            tq, tr = s.tmp[dc % 2], s.tmp_r[dc % 2]
            if which == 0:
                emit(s.act, lambda e: e.activation(out=tq, in_=ps[:, 0:T], func=AF.Sigmoid), pr, [tr])
            else:
                emit(s.dve, lambda e: e.tensor_tensor(out=s.midc(dc), in0=ps[:, 0:T], in1=tq, op=ALU.mult), pr + [tr], [s.mid_r[dc]])

        s.linear(w_in, r_in, 0, DC, cols, s.hx, s.hT_r, epi1, T)
        s.halo_exchange("cf")
        if s.only is None:
            s.cast_upto("lru_w_out")
        acc1, acc2 = s.st[0], s.st[1]
        a1r, a2r = [s.st_r[0]], [s.st_r[1]]
        for dc in range(DC):
            tq, tr = s.tmp[dc % 2], [s.tmp_r[dc % 2]]
            s.dwconv(dc, s.cvw, 31, tq, tr, bias=s.cvv[:, 0:DC])
            sq, sqr = s.next_xt()
            emit(s.act, lambda e: e.activation(out=sq, in_=tq, func=AF.Square), tr, [sqr])
            emit(s.act, lambda e: e.activation(out=s.hT[:, dc, :], in_=tq, func=AF.Identity), tr, [s.hT_r[dc]])
            if dc == 0:
                emit(s.dve, lambda e: e.tensor_copy(out=acc1, in_=tq), tr, a1r)
                emit(s.dve, lambda e: e.tensor_copy(out=acc2, in_=sq), [sqr], a2r)
            else:
                emit(s.dve, lambda e: e.tensor_tensor(out=acc1, in0=acc1, in1=tq, op=ALU.add), tr + a1r, a1r)
                emit(s.dve, lambda e: e.tensor_tensor(out=acc2, in0=acc2, in1=sq, op=ALU.add), [sqr] + a2r, a2r)
        mean, mr = s.xt[0], [s.xt_r[0]]
        rstd, rr = s.xt[1], [s.xt_r[1]]
        s.colsum_bc(acc1, a1r, mean, mr, 1.0 / D, 0.0, None)
        s.colsum_bc(acc2, a2r, rstd, rr, 1.0 / D, 0.0, None)
        m2, m2r = s.px, [s.px_r]
        emit(s.dve, lambda e: e.tensor_tensor(out=m2, in0=mean, in1=mean, op=ALU.mult), mr, m2r)
        emit(s.dve, lambda e: e.tensor_tensor(out=rstd, in0=rstd, in1=m2, op=ALU.subtract), rr + m2r, rr)
        emit(s.dve, lambda e: e.tensor_scalar(out=rstd, in0=rstd, scalar1=1e-6, scalar2=None, op0=ALU.add), rr, rr)
        s.rpow(rstd, rr, -0.5)
        for dc in range(DC):
            tq, tr = s.tmp[dc % 2], [s.tmp_r[dc % 2]]
            emit(s.dve, lambda e: e.tensor_tensor(out=tq, in0=s.hT[:, dc, :], in1=mean, op=ALU.subtract), [s.hT_r[dc]] + mr, tr)
            emit(s.dve, lambda e: e.tensor_tensor(out=tq, in0=tq, in1=rstd, op=ALU.mult), tr + rr, tr)
            emit(s.act, lambda e: e.activation(out=s.midc(dc), in_=tq, func=AF.Silu, bias=s.cvv[:, 2 * DC + dc:2 * DC + dc + 1],
                                               scale=s.cvv[:, DC + dc:DC + dc + 1]), tr + [s.cv_r], [s.mid_r[dc]])
        pre, epi3 = s.resid(gate)
        s.linear(w_out, r_out, 0, DC, [(d * 128, 128) for d in range(DC)], s.midx, s.mid_r, epi3, T, pre=pre)

    def rglru(s, gs, sh, gate):
        c = s.c
        D, DC, T = c.D, c.DC, c.T
        CH = c.LB // 128
        (w_in, r_in), (w_out, r_out) = s.W["lru_w_in"], s.W["lru_w_out"]
        (w_a, rw_a), (w_x, rw_x) = s.W["lru_w_a"], s.W["lru_w_x"]
        s.load_small(s.cvw[:, 0:4 * DC], s.lrw_in, s.cv_r)
        s.load_small(s.cvv[:, 0:4 * DC], s.lrv_in, s.cv_r)
        vec, vr = s.vec, [s.vec_r]
        emit(s.act, lambda e: e.activation(out=vec[:, 0:DC], in_=s.cvv[:, 3 * DC:4 * DC], func=AF.Exp, scale=-1.0), [s.cv_r], vr)
        emit(s.dve, lambda e: e.tensor_scalar(out=vec[:, 0:DC], in0=vec[:, 0:DC], scalar1=1.0, scalar2=None, op0=ALU.add), vr, vr)
        emit(s.act, lambda e: e.activation(out=vec[:, 0:DC], in_=vec[:, 0:DC], func=AF.Ln), vr, vr)
        emit(s.dve, lambda e: e.tensor_scalar(out=vec[:, DC:2 * DC], in0=vec[:, 0:DC], scalar1=-16.0, scalar2=None, op0=ALU.mult), vr, vr)
        emit(s.dve, lambda e: e.tensor_scalar(out=vec[:, 0:DC], in0=vec[:, 0:DC], scalar1=-8.0, scalar2=None, op0=ALU.mult), vr, vr)
        s.chk(1, "lru")
        s.norm(gs, sh)

        def epi1(dc, ps, pr, w):
            emit(s.act, lambda e: e.activation(out=s.midc(dc), in_=ps[:, 0:T], func=AF.Identity), pr, [s.mid_r[dc]])

        s.linear(w_in, r_in, 0, DC, [(D + dc * 128, 128) for dc in range(DC)], s.hx, s.hT_r, epi1, T)
        s.chk(2, "lru")
        s.halo_exchange("lr")
        for dc in range(DC):
            tq, tr = s.tmp[dc % 2], [s.tmp_r[dc % 2]]
            s.dwconv(dc, s.cvw, 4, tq, tr, bias=s.cvv[:, 0:DC])
            emit(s.act, lambda e: e.activation(out=s.midc(dc), in_=tq, func=AF.Identity), tr, [s.mid_r[dc]])

        t_r, t_a2, t_a, t_u, t_g = s.xt[0], s.xt[1], s.tmp[0], s.tmp[1], s.st[0]
        r_r, r_a2, r_a, r_u, r_g = [s.xt_r[0]], [s.xt_r[1]], [s.tmp_r[0]], [s.tmp_r[1]], [s.st_r[0]]
        ob = s.st[1].bitcast(BF16)
        ob_r = [s.st_r[1]]
        assert CH <= 2

        def lru_pass(final_only):
            for hh in range(c.LH):
                for jc in range(CH):
                    j = hh * CH + jc
                    xfn = lambda kc, n0, n1, hh=hh: s.mid[:, hh * CH + kc, HALO + n0:HALO + n1]
                    xres = s.mid_r[hh * CH:(hh + 1) * CH]

                    def epi_r(_, ps, pr, w):
                        emit(s.act, lambda e: e.activation(out=t_r, in_=ps[:, 0:T], func=AF.Sigmoid, bias=s.cvv[:, DC + j:DC + j + 1], scale=1.0),
                             pr + [s.cv_r], r_r)
                        emit(s.act, lambda e: e.activation(out=t_a, in_=t_r, func=AF.Exp, scale=vec[:, j:j + 1]), r_r + vr, r_a)
                        emit(s.act, lambda e: e.activation(out=t_a2, in_=t_r, func=AF.Exp, scale=vec[:, DC + j:DC + j + 1]), r_r + vr, r_a2)
                        emit(s.dve, lambda e: e.tensor_scalar(out=t_a2, in0=t_a2, scalar1=-1.0, scalar2=1.0, op0=ALU.mult, op1=ALU.add), r_a2, r_a2)
                        emit(s.act, lambda e: e.activation(out=t_a2, in_=t_a2, func=AF.Sqrt), r_a2, r_a2)

                    def epi_i(_, ps, pr, w):
                        emit(s.act, lambda e: e.activation(out=t_u, in_=ps[:, 0:T], func=AF.Sigmoid, bias=s.cvv[:, 2 * DC + j:2 * DC + j + 1], scale=1.0),
                             pr + [s.cv_r], r_u)
                        emit(s.dve, lambda e: e.tensor_tensor(out=t_u, in0=t_u, in1=s.midc(j), op=ALU.mult), r_u + [s.mid_r[j]], r_u)
                        emit(s.dve, lambda e: e.tensor_tensor(out=t_u, in0=t_u, in1=t_a2, op=ALU.mult), r_u + r_a2, r_u)
                        init = 0.0 if final_only else vec[:, 3 * DC + j:3 * DC + j + 1]
                        emit(s.dve, lambda e: e.tensor_tensor_scan(out=t_r, data0=t_a, data1=t_u, initial=init, op0=ALU.mult, op1=ALU.add),
                             r_a + r_u + vr + r_r, r_r)
                        if final_only:
                            emit(s.dve, lambda e: e.tensor_copy(out=vec[:, 2 * DC + j:2 * DC + j + 1], in_=t_r[:, T - 1:T]), r_r + vr, vr)

                    s.linear(w_a, rw_a, hh * c.LB, CH, [(jc * 128, 128)], xfn, xres, epi_r, T)
                    s.linear(w_x, rw_x, hh * c.LB, CH, [(jc * 128, 128)], xfn, xres, epi_i, T)
                    if not final_only:
                        def epi_g(_, ps, pr, w):
                            emit(s.act, lambda e: e.activation(out=t_g, in_=ps[:, 0:T], func=AF.Square), pr, r_g)
                            emit(s.dve, lambda e: e.tensor_scalar(out=t_g, in0=t_g, scalar1=0.044715, scalar2=1.0, op0=ALU.mult, op1=ALU.add), r_g, r_g)
                            emit(s.dve, lambda e: e.tensor_tensor(out=t_g, in0=t_g, in1=ps[:, 0:T], op=ALU.mult), r_g + pr, r_g)
                            emit(s.act, lambda e: e.activation(out=t_g, in_=t_g, func=AF.Sigmoid, scale=1.5957691216057308), r_g, r_g)
                            emit(s.dve, lambda e: e.tensor_tensor(out=t_g, in0=t_g, in1=ps[:, 0:T], op=ALU.mult), r_g + pr, r_g)
                            emit(s.dve, lambda e: e.tensor_tensor(out=ob[:, jc * T:(jc + 1) * T], in0=t_g, in1=t_r, op=ALU.mult), r_g + r_r, ob_r)
                        s.linear(w_in, r_in, 0, DC, [(j * 128, 128)], s.hx, s.hT_r, epi_g, T)
                if not final_only:
                    for jc in range(CH):
                        j = hh * CH + jc
                        emit(s.act, lambda e, j=j, jc=jc: e.activation(out=s.midc(j), in_=ob[:, jc * T:(jc + 1) * T], func=AF.Identity), ob_r, [s.mid_r[j]])

        s.chk(3, "lru")
        lru_pass(True)
        s.chk(4, "lru")
        loc = s.dram("lru_l", [128, DC], F32)
        pair = s.dram("lru_p", [256, DC], F32)
        rl, rp = Res("ll"), Res("lp")
        emit(s.pq, lambda e: e.dma_start(out=loc, in_=vec[:, 2 * DC:3 * DC]), vr, [rl])
        s.pair_exchange(loc, rl, pair, rp)
        if s.only is None:
            s.cast_upto("moe_w2_1")
        emit(s.sq, lambda e: e.dma_start(out=vec[:, 3 * DC:4 * DC], in_=pair[0:128, :]), [rp], vr)
        emit(s.dve, lambda e: e.tensor_scalar(out=vec[:, 3 * DC:4 * DC], in0=vec[:, 3 * DC:4 * DC], scalar1=s.flag[:, 0:1], scalar2=None, op0=ALU.mult),
             vr + [s.const_r], vr)
        s.chk(5, "lru")
        lru_pass(False)
        s.chk(6, "lru")
        pre, epi3 = s.resid(gate)
        s.linear(w_out, r_out, 0, DC, [(d * 128, 128) for d in range(DC)], s.midx, s.mid_r, epi3, T, pre=pre)

    def attention(s, gs, sh, gate):
        c = s.c
        D, DC, T, S, TB = c.D, c.DC, c.T, c.S, c.TB
        QC, KVC, NH, IH = c.QL // 128, c.KVL // 128, c.NH, c.IH
        SB = S // 128
        W = s.W
        (w_in, r_in), (w_qidx, r_qidx), (w_uq, r_uq) = W["att_w_in"], W["att_w_qidx"], W["att_w_uq"]
        (w_ukT, r_ukT), (w_uv, r_uv), (w_out, r_out) = W["att_w_ukT"], W["att_w_uv"], W["att_w_out"]
        s.load_small(s.cvv[:, 0:QC], s.gcq_in, s.cv_r)
        s.load_small(s.cvv[:, QC:QC + KVC], s.gckv_in, s.cv_r)

        if s.full:
            midflat = s.mid[:, :, :].rearrange("p a b -> p (a b)")
            ohs = midflat[:, 0:2 * 32 * 128]
            ohs_r = s.mid_r
            rbb = s.st[0]
            rbb_r = [s.st_r[0]]
        else:
            ohs = s.sb("ohs_sb", [128, 2 * 32 * 128], BF16)[:, :]
            ohs_r = [Res("ohs")]
            rbb = s.sb("rbb_sb", [128, 32 * NH], F32)[:, :]
            rbb_r = [Res("rbb")]
        emit(s.pq, lambda e: e.dma_start(out=ohs, in_=s.ohs_in), [], ohs_r)
        emit(s.sq, lambda e: e.dma_start(out=rbb[:, 0:32 * NH], in_=s.rbb_in), [], rbb_r)
        assert NH <= 32
        negb = s.gb[:, 0:NH]
        nb_r = [s.gb_r]
        emit(s.dve, lambda e: e.tensor_scalar(out=negb, in0=rbb[:, 31 * NH:32 * NH], scalar1=-1.0, scalar2=None, op0=ALU.mult), rbb_r, nb_r)
        eacc = s.tmp[0][:, 0:128]
        ea_r = [s.tmp_r[0]]
        for h in range(NH):
            for dl in range(2):
                for b in range(32):
                    src = ohs[:, (dl * 32 + b) * 128:(dl * 32 + b + 1) * 128]
                    sc = rbb[:, b * NH + h:b * NH + h + 1]
                    if b == 0:
                        emit(s.dve, lambda e, src=src, sc=sc: e.tensor_scalar(out=eacc, in0=src, scalar1=sc, scalar2=None, op0=ALU.mult),
                             ohs_r + rbb_r, ea_r)
                    else:
                        emit(s.dve, lambda e, src=src, sc=sc: e.scalar_tensor_tensor(out=eacc, in0=src, scalar=sc, in1=eacc, op0=ALU.mult, op1=ALU.add),
                             ohs_r + rbb_r + ea_r, ea_r)
                o = (h * 2 + dl) * 128
                emit(s.act, lambda e, o=o, h=h: e.activation(out=s.EB[:, o:o + 128], in_=eacc, func=AF.Exp, bias=negb[:, h:h + 1], scale=1.0),
                     ea_r + nb_r, [s.EB_r])

        s.chk(1)
        s.norm(gs, sh)
        s.chk(2)
        accq, aq_r = s.st[0], [s.st_r[0]]
        acck, ak_r = s.st[1], [s.st_r[1]]

        def epi_in(j, ps, pr, w):
            if j < QC + KVC:
                acc, ar = (accq, aq_r) if j < QC else (acck, ak_r)
                first = (j == 0) or (j == QC)
                if first:
                    emit(s.act, lambda e: e.activation(out=acc, in_=ps[:, 0:T], func=AF.Square), pr, ar)
                else:
                    tq, tr = s.tmp[j % 2], [s.tmp_r[j % 2]]
                    emit(s.act, lambda e: e.activation(out=tq, in_=ps[:, 0:T], func=AF.Square), pr, tr)
                    emit(s.dve, lambda e: e.tensor_tensor(out=acc, in0=acc, in1=tq, op=ALU.add), tr + ar, ar)
            emit(s.act, lambda e: e.activation(out=s.midc(j), in_=ps[:, 0:T], func=AF.Identity), pr, [s.mid_r[j]])

        s.linear(w_in, r_in, 0, DC, [(j * 128, 128) for j in range(QC + KVC + 1)], s.hx, s.hT_r, epi_in, T)

        s.chk(3)
        if s.full:
            hflat = s.hT[:, :, :].rearrange("p a b -> p (a b)")
        else:
            hflat = s.sb("att_scratch", [128, QC * T + KVC * S + SB * c.KVL + S + 16 * 128 + TB * IH * 2 + KVC * 256 + 128 + 256 + 64], BF16)[:, :]
        off = [0]

        def carve(n):
            a = hflat[:, off[0]:off[0] + n]
            off[0] += n
            return a

        assert TB * IH <= 31 * DC
        wtok = s.cvw[:, 0:TB * IH]
        wtok_r = [s.cv_r]
        sl = s.wcount % len(s.wt)
        s.wcount += 1
        wt, wr = s.wt[sl], s.wt_r[sl]
        wcol = c.QL + c.KVL + 128
        emit(s.sq, lambda e: e.dma_start(out=wt[:, 0:DC, 0:IH], in_=w_in[wcol:wcol + 128, :].rearrange("p (kc f) -> p kc f", f=128)[:, :, 0:IH]), list(r_in[1]), [wr])
        for tb in range(TB):
            ps, pr = s.bank[7], [s.bank_r[7]]
            for kc in range(DC):
                emit(s.pe, lambda e, tb=tb, kc=kc: e.matmul(ps[:, 0:IH], lhsT=s.hT[:, kc, tb * 128:(tb + 1) * 128], rhs=wt[:, kc, 0:IH],
                                                            start=(kc == 0), stop=(kc == DC - 1)), [wr, s.hT_r[kc]], pr)
            emit(s.dve, lambda e, tb=tb: e.tensor_copy(out=wtok[:, tb * IH:(tb + 1) * IH], in_=ps[:, 0:IH]), pr, wtok_r)

        cqn = carve(QC * T)
        ckv = carve(KVC * S)
        kvt = carve(SB * c.KVL)
        kidx = carve(S)
        GI = min(16, IH)
        qig = carve(GI * 128)
        qh = carve(128)
        qlat = carve(KVC * 128)
        olat = carve(KVC * 128)
        views = {n: [Res(n)] for n in ("cqn", "ckv", "kvt", "kidx", "qig", "qh", "qlat", "olat")}
        if s.full:
            base = []
            for r in s.hT_r:
                base += r.r + ([r.w] if r.w is not None else [])
            for v in views.values():
                v[0].r = list(base)
        rinv = carve(256).bitcast(F32)
        rinv_r = [Res("rinv")]
        views["rinv"] = rinv_r
        if s.full:
            rinv_r[0].r = list(base)

        rq, rq_r = s.xt[0], [s.xt_r[0]]
        rk, rk_r = s.xt[1], [s.xt_r[1]]
        s.colsum_bc(accq, aq_r, rq, rq_r, 1.0 / c.QL, 1e-6, -0.5)
        s.colsum_bc(acck, ak_r, rk, rk_r, 1.0 / c.KVL, 1e-6, -0.5)
        for j in range(QC + KVC):
            tq, tr = s.tmp[j % 2], [s.tmp_r[j % 2]]
            rs, rs_r = (rq, rq_r) if j < QC else (rk, rk_r)
            emit(s.dve, lambda e: e.tensor_tensor(out=tq, in0=s.midc(j), in1=rs, op=ALU.mult), [s.mid_r[j]] + rs_r, tr)
            if j < QC:
                dst, dr = cqn[:, j * T:(j + 1) * T], views["cqn"]
            else:
                cc = j - QC
                dst, dr = ckv[:, cc * S + T:cc * S + 2 * T], views["ckv"]
            emit(s.act, lambda e, dst=dst: e.activation(out=dst, in_=tq, func=AF.Identity, scale=s.cvv[:, j:j + 1]), tr + [s.cv_r], dr)
        emit(s.dve, lambda e: e.tensor_copy(out=kidx[:, T:2 * T], in_=s.midc(QC + KVC)), [s.mid_r[QC + KVC]], views["kidx"])

        s.chk(4)
        NR = (KVC + 1) * 128
        loc = s.dram("kv_l", [NR, T], BF16)
        pair = s.dram("kv_p", [2 * NR, T], BF16)
        rl, rp = Res("kvl"), Res("kvp")
        ckv3 = ckv.rearrange("p (a b) -> p a b", b=S)
        emit(s.pq, lambda e: e.dma_start(out=loc[0:KVC * 128, :].rearrange("(cc p) t -> p cc t", p=128), in_=ckv3[:, :, T:2 * T]), views["ckv"], [rl])
        emit(s.pq, lambda e: e.dma_start(out=loc[KVC * 128:NR, :], in_=kidx[:, T:2 * T]), views["kidx"], [rl])
        s.pair_exchange(loc, rl, pair, rp)
        if s.only is None:
            s.cast_upto("sconv_w_out")
        emit(s.sq, lambda e: e.dma_start(out=ckv3[:, :, 0:T], in_=pair[0:KVC * 128, :].rearrange("(cc p) t -> p cc t", p=128)), [rp], views["ckv"])
        emit(s.sq, lambda e: e.dma_start(out=kidx[:, 0:T], in_=pair[KVC * 128:NR, :]), [rp], views["kidx"])

        s.chk(5)
        for sb_ in range(SB):
            ps, pr = s.bank[6 + sb_ % 2], [s.bank_r[6 + sb_ % 2]]
            for cc in range(KVC):
                emit(s.pe, lambda e, ps=ps, cc=cc, sb_=sb_: e.matmul(ps[:, cc * 128:(cc + 1) * 128], lhsT=ckv[:, cc * S + sb_ * 128:cc * S + (sb_ + 1) * 128],
                                                                  rhs=s.identb[:, :], start=True, stop=True), views["ckv"] + [s.const_r], pr)
            emit(s.act, lambda e, ps=ps, sb_=sb_: e.activation(out=kvt[:, sb_ * c.KVL:(sb_ + 1) * c.KVL], in_=ps[:, 0:c.KVL], func=AF.Identity), pr, views["kvt"])

        s.chk(6)
        big = s.psum[:, 0:2048]
        big_r = s.bank_r[0:4]
        Sc = s.scr[:, 0:S]
        Sc_r = [s.xt_r[0], s.xt_r[1]]
        Sw = s.scr[:, 2 * T:2 * T + S]
        Sw_r = [s.tmp_r[0], s.tmp_r[1]]
        Mb = s.scr[:, 2 * T:3 * T].bitcast(BF16)
        PTs = [s.st[0].bitcast(BF16), s.px.bitcast(BF16)]
        PT_rs = [[s.st_r[0]], [s.px_r]]
        MT = s.st[1].bitcast(BF16)
        MT_r = [s.st_r[1]]
        m8, thr = s.sm[0], s.sm[1]
        sm_r = [s.sm_r]
        scale = 128 ** -0.5

        for qb in range(TB):
            NKC = TB + qb + 1
            NK = NKC * 128
            t0 = qb * 128
            xq = lambda kc, n0, n1: cqn[:, kc * T + t0 + n0:kc * T + t0 + n1]
            first = True
            for g0 in range(0, IH, GI):
                def epi_qi(jl, ps, pr, w):
                    emit(s.act, lambda e: e.activation(out=qig[:, jl * 128:(jl + 1) * 128], in_=ps[:, 0:128], func=AF.Identity), pr, views["qig"])
                s.linear(w_qidx, r_qidx, 0, QC, [((g0 + jl) * 128, 128) for jl in range(GI)], xq, views["cqn"], epi_qi, 128)
                for jl in range(GI):
                    hh = g0 + jl
                    for n0 in range(0, NK, 512):
                        n1 = min(NK, n0 + 512)
                        emit(s.pe, lambda e, jl=jl, n0=n0, n1=n1: e.matmul(big[:, n0:n1], lhsT=qig[:, jl * 128:(jl + 1) * 128], rhs=kidx[:, n0:n1],
                                                                          start=True, stop=True), views["qig"] + views["kidx"], big_r)
                    emit(s.act, lambda e: e.activation(out=Sw[:, 0:NK], in_=big[:, 0:NK], func=AF.Relu), big_r, Sw_r)
                    wsc = wtok[:, qb * IH + hh:qb * IH + hh + 1]
                    if first:
                        emit(s.dve, lambda e, wsc=wsc: e.tensor_scalar(out=Sc[:, 0:NK], in0=Sw[:, 0:NK], scalar1=wsc, scalar2=None, op0=ALU.mult),
                             Sw_r + wtok_r, Sc_r)
                        first = False
                    else:
                        emit(s.dve, lambda e, wsc=wsc: e.scalar_tensor_tensor(out=Sc[:, 0:NK], in0=Sw[:, 0:NK], scalar=wsc, in1=Sc[:, 0:NK],
                                                                             op0=ALU.mult, op1=ALU.add), Sw_r + wtok_r + Sc_r, Sc_r)
            s.chk(7)
            emit(s.dve, lambda e: e.tensor_scalar(out=Sc[:, 0:T], in0=Sc[:, 0:T], scalar1=s.flag[:, 1:2], scalar2=None, op0=ALU.add), Sc_r + [s.const_r], Sc_r)
            emit(s.dve, lambda e: e.tensor_tensor(out=Sc[:, NK - 128:NK], in0=Sc[:, NK - 128:NK], in1=s.tri[:, :], op=ALU.add), Sc_r + [s.const_r], Sc_r)
            rounds = c.KSEL // 8
            for r in range(rounds):
                srcv = Sc if r == 0 else Sw
                src_r = Sc_r if r == 0 else Sw_r
                emit(s.dve, lambda e, srcv=srcv: e.max(out=m8[:, 0:8], in_=srcv[:, 0:NK]), src_r + sm_r, sm_r)
                if r < rounds - 1:
                    emit(s.dve, lambda e, srcv=srcv: e.match_replace(out=Sw[:, 0:NK], in_to_replace=m8[:, 0:8], in_values=srcv[:, 0:NK], imm_value=NEG),
                         src_r + sm_r + Sw_r, Sw_r)
            emit(s.dve, lambda e: e.tensor_scalar(out=thr[:, 0:1], in0=m8[:, 7:8], scalar1=-1.0e29, scalar2=None, op0=ALU.max), sm_r, sm_r)
            emit(s.dve, lambda e: e.tensor_scalar(out=Mb[:, 0:NK], in0=Sc[:, 0:NK], scalar1=thr[:, 0:1], scalar2=None, op0=ALU.is_ge), Sc_r + sm_r + Sw_r, Sw_r)
            s.chk(8)
            for j0 in range(0, NKC, 4):
                j1 = min(NKC, j0 + 4)
                ps, pr = s.bank[7], [s.bank_r[7]]
                for j in range(j0, j1):
                    emit(s.pe, lambda e, j=j, j0=j0: e.matmul(ps[:, (j - j0) * 128:(j - j0 + 1) * 128], lhsT=Mb[:, j * 128:(j + 1) * 128], rhs=s.identb[:, :],
                                                             start=True, stop=True), Sw_r + [s.const_r], pr)
                emit(s.act, lambda e, j0=j0, j1=j1: e.activation(out=MT[:, j0 * 128:j1 * 128], in_=ps[:, 0:(j1 - j0) * 128], func=AF.Identity), pr, MT_r)
            s.chk(9)
            for h in range(NH):
                PT, PT_r = PTs[h % 2], PT_rs[h % 2]

                def epi_q(_, ps, pr, w):
                    emit(s.act, lambda e: e.activation(out=qh, in_=ps[:, 0:128], func=AF.Identity), pr, views["qh"])
                s.linear(w_uq, r_uq, 0, QC, [(h * 128, 128)], xq, views["cqn"], epi_q, 128)
                sl = s.wcount % len(s.wt)
                s.wcount += 1
                wuk, wuk_r = s.wt[sl], [s.wt_r[sl]]
                emit(s.sq, lambda e, wuk=wuk: e.dma_start(out=wuk[:, 0:KVC, :],
                                                         in_=w_ukT[:, h * 128:(h + 1) * 128].rearrange("(cc p) f -> p cc f", p=128)), list(r_ukT[1]), wuk_r)
                sl = s.wcount % len(s.wt)
                s.wcount += 1
                wuv, wuv_r = s.wt[sl], [s.wt_r[sl]]
                emit(s.sq, lambda e, wuv=wuv: e.dma_start(out=wuv[:, 0:KVC, :], in_=w_uv[h * 128:(h + 1) * 128, :].rearrange("p (kc f) -> p kc f", f=128)),
                     list(r_uv[1]), wuv_r)
                ps6, pr6 = s.bank[6], [s.bank_r[6]]
                ps7, pr7 = s.bank[7], [s.bank_r[7]]
                for cc in range(KVC):
                    emit(s.pe, lambda e, cc=cc, wuk=wuk: e.matmul(ps6[:, cc * 128:(cc + 1) * 128], lhsT=wuk[:, cc, :], rhs=qh, start=True, stop=True),
                         wuk_r + views["qh"], pr6)
                emit(s.dve, lambda e: e.tensor_copy(out=qlat, in_=ps6[:, 0:KVC * 128]), pr6, views["qlat"])
                for j in range(NKC):
                    for cc in range(KVC):
                        emit(s.pe, lambda e, j=j, cc=cc: e.matmul(big[:, j * 128:(j + 1) * 128], lhsT=ckv[:, cc * S + j * 128:cc * S + (j + 1) * 128],
                                                                 rhs=qlat[:, cc * 128:(cc + 1) * 128], start=(cc == 0), stop=(cc == KVC - 1)),
                             views["ckv"] + views["qlat"], big_r)
                emit(s.act, lambda e, PT=PT: e.activation(out=PT[:, 0:NK], in_=big[:, 0:NK], func=AF.Exp, scale=scale), big_r, PT_r)
                emit(s.dve, lambda e, PT=PT: e.tensor_tensor(out=PT[:, 0:NK], in0=PT[:, 0:NK], in1=MT[:, 0:NK], op=ALU.mult), PT_r + MT_r, PT_r)
                emit(s.dve, lambda e, PT=PT, h=h: e.tensor_tensor(out=PT[:, NK - 256:NK], in0=PT[:, NK - 256:NK], in1=s.EB[:, h * 256:(h + 1) * 256], op=ALU.mult),
                     PT_r + [s.EB_r], PT_r)
                for j in range(NKC):
                    emit(s.pe, lambda e, j=j, PT=PT: e.matmul(ps7[:, 0:128], lhsT=s.onesb[:, :], rhs=PT[:, j * 128:(j + 1) * 128], start=(j == 0), stop=(j == NKC - 1)),
                         PT_r + [s.const_r], pr7)
                emit(s.dve, lambda e: e.reciprocal(out=rinv, in_=ps7[:, 0:128]), pr7, rinv_r)
                for cc in range(KVC):
                    for j in range(NKC):
                        emit(s.pe, lambda e, j=j, cc=cc, PT=PT: e.matmul(ps6[:, cc * 128:(cc + 1) * 128], lhsT=kvt[:, j * c.KVL + cc * 128:j * c.KVL + (cc + 1) * 128],
                                                                        rhs=PT[:, j * 128:(j + 1) * 128], start=(j == 0), stop=(j == NKC - 1)),
                             PT_r + views["kvt"], pr6)
                emit(s.act, lambda e: e.activation(out=olat, in_=ps6[:, 0:KVC * 128], func=AF.Identity), pr6, views["olat"])
                for cc in range(KVC):
                    emit(s.pe, lambda e, cc=cc, wuv=wuv: e.matmul(ps7[:, 128:256], lhsT=wuv[:, cc, :], rhs=olat[:, cc * 128:(cc + 1) * 128],
                                                                 start=(cc == 0), stop=(cc == KVC - 1)), wuv_r + views["olat"], pr7)
                emit(s.dve, lambda e, h=h: e.tensor_tensor(out=s.mid[:, h, HALO + t0:HALO + t0 + 128], in0=ps7[:, 128:256], in1=rinv, op=ALU.mult),
                     pr7 + rinv_r, [s.mid_r[h]])
        s.chk(10)
        if s.full:
            allt = []
            for v in views.values():
                allt += v[0].r + ([v[0].w] if v[0].w is not None else [])
            for r in s.hT_r:
                r.r = r.r + allt
        pre, epi3 = s.resid(gate)
        s.linear(w_out, r_out, 0, c.AW // 128, [(d * 128, 128) for d in range(DC)], s.midx, s.mid_r[0:NH], epi3, T, pre=pre)


def _rel_bucket(dist):
    import math
    dist = np.maximum(dist, 0)
    max_exact = 16
    large = max_exact + (np.log(np.maximum(dist, 1).astype(np.float32) / max_exact) / math.log(128 / max_exact)
                         * (32 - max_exact)).astype(np.int32)
    large = np.minimum(large, 31)
    return np.where(dist < max_exact, dist, large)


def _pvec(v, DC):
    v = np.asarray(v, np.float32)
    lead = int(np.prod(v.shape[:-1])) if v.ndim > 1 else 1
    return np.ascontiguousarray(v.reshape(lead, DC, 128).transpose(2, 0, 1).reshape(128, lead * DC))


def make_in_maps(cfg, inp):
    c = cfg
    D, DC, T = c.D, c.DC, c.T
    f = lambda a: np.asarray(a, np.float32)
    shared = {}
    shared["cT"] = np.ascontiguousarray(f(inp["c"]).reshape(4, DC, 128).transpose(2, 1, 0).reshape(128, DC * 4))
    shared["ada_t"] = _pvec(f(inp["ada_table"]), DC)
    shared["norm_mix"] = _pvec(f(inp["norm_mix"]), DC)
    shared["norm_ffn"] = _pvec(f(inp["norm_ffn"]), DC)
    shared["norm_final"] = _pvec(f(inp["norm_final"]), DC)
    shared["ident"] = np.eye(128, dtype=np.float32)
    ss, tt = np.meshgrid(np.arange(128), np.arange(128), indexing="ij")
    shared["tri"] = np.where(ss.T >= tt.T, 0.0, 0.0).astype(np.float32)
    tq, sk = np.meshgrid(np.arange(128), np.arange(128), indexing="ij")
    shared["tri"] = np.where(sk <= tq, 0.0, NEG).astype(np.float32)
    ohs = np.zeros((128, 2, 32, 128), np.float32)
    s_i, t_i = np.meshgrid(np.arange(128), np.arange(128), indexing="ij")
    for dl in range(2):
        delta = 1 - dl
        bk = _rel_bucket(delta * 128 + t_i - s_i)
        for b in range(32):
            ohs[:, dl, b, :] = (bk == b)
    shared["ohs"] = ohs.reshape(128, -1)
    shared["rbb"] = np.ascontiguousarray(np.broadcast_to(f(inp["rel_bias"]).reshape(1, 32 * c.NH), (128, 32 * c.NH)))
    shared["g_cq"] = _pvec(f(inp["att_g_cq"])[0], c.QL // 128)
    shared["g_ckv"] = _pvec(f(inp["att_g_ckv"])[0], c.KVL // 128)
    shared["sconv_cw"] = _pvec(f(inp["sconv_conv_w"])[0], DC)
    shared["conf_cw"] = _pvec(f(inp["conf_conv_w"])[0], DC)
    shared["conf_vec"] = _pvec(np.stack([f(inp["conf_conv_b"])[0], f(inp["conf_ln_g"])[0], f(inp["conf_ln_b"])[0]]), DC)
    shared["lru_cw"] = _pvec(f(inp["lru_conv_w"])[0], DC)
    shared["lru_vec"] = _pvec(np.stack([f(inp["lru_conv_b"])[0], f(inp["lru_b_a"])[0], f(inp["lru_b_x"])[0], f(inp["lru_lambda"])[0]]), DC)
    rt = f(inp["moe_router"])
    shared["router"] = np.ascontiguousarray(rt.reshape(rt.shape[0], DC, 128, 8).transpose(2, 0, 1, 3).reshape(128, rt.shape[0] * DC * 8))
    if shared["router"].shape[1] < 2 * DC * 8:
        shared["router"] = np.concatenate([shared["router"], np.zeros((128, 2 * DC * 8 - shared["router"].shape[1]), np.float32)], 1)

    wfull = {
        "att_w_in": f(inp["att_w_in"])[0],
        "att_w_qidx": f(inp["att_w_qidx"])[0],
        "att_w_uq": f(inp["att_w_uq"])[0],
        "att_w_ukT": np.ascontiguousarray(f(inp["att_w_uk"])[0].transpose(1, 2, 0)).reshape(c.AW, c.KVL),
        "att_w_uv": f(inp["att_w_uv"])[0].reshape(c.KVL, c.AW),
        "att_w_out": f(inp["att_w_out"])[0],
        "sconv_w_in": f(inp["sconv_w_in"])[0], "sconv_w_out": f(inp["sconv_w_out"])[0],
        "conf_w_in": f(inp["conf_w_in"])[0], "conf_w_out": f(inp["conf_w_out"])[0],
        "lru_w_in": f(inp["lru_w_in"])[0], "lru_w_out": f(inp["lru_w_out"])[0],
        "lru_w_a": f(inp["lru_w_a"])[0].reshape(D, c.LB), "lru_w_x": f(inp["lru_w_x"])[0].reshape(D, c.LB),
    }
    for l in range(2):
        wfull[f"ffn_w13_{l}"] = f(inp["ffn_w13"])[l]
        wfull[f"ffn_w2_{l}"] = f(inp["ffn_w2"])[l]
        wfull[f"moe_w13_{l}"] = np.ascontiguousarray(f(inp["moe_w13"])[l].transpose(1, 0, 2)).reshape(D, c.NE * 2 * c.DFE)
        wfull[f"moe_w2_{l}"] = np.ascontiguousarray(f(inp["moe_w2"])[l].transpose(1, 0, 2)).reshape(c.DFE, c.NE * D)
    for name in list(wfull.keys()):
        w = wfull[name]
        K, Fd = w.shape
        Fp = ((Fd + 127) // 128) * 128
        if Fp != Fd:
            w = np.concatenate([w, np.zeros((K, Fp - Fd), np.float32)], axis=1)
        wfull[name] = np.ascontiguousarray(w.reshape(K // 128, 128, Fp // 128, 128).transpose(2, 1, 0, 3)).reshape(Fp, K)
    ada_w = f(inp["ada_w"])
    ada_b = f(inp["ada_b"])
    x = f(inp["x"])
    NCOL = 6 * D // 8
    NCL = 6 * DC // 8
    maps = []
    for core in range(8):
        b, half = core // 2, core % 2
        m = dict(shared)
        m["xT"] = np.ascontiguousarray(x[b, half * T:(half + 1) * T, :].T)
        m["ada_w"] = np.ascontiguousarray(ada_w[:, core * NCOL:(core + 1) * NCOL])
        m["ada_b"] = np.ascontiguousarray(ada_b[core * NCOL:(core + 1) * NCOL].reshape(NCL, 128).T)
        bs = np.zeros((128, 4), np.float32)
        bs[:, b] = 1.0
        m["bsel"] = bs
        fl = np.zeros((128, 2), np.float32)
        fl[:, 0] = float(half)
        fl[:, 1] = 0.0 if half == 1 else NEG
        m["flag"] = fl
        for name, w in wfull.items():
            m[name] = w
        maps.append(m)
    return maps


_CACHE = {}


def run(cfg, inp, stop_after=None, only=None):
    key = (cfg.D, cfg.SEQ, stop_after, tuple(sorted(only)) if only else None)
    if key not in _CACHE:
        bld = B(cfg, stop_after, only)
        _CACHE[key] = (bld.build(), bld)
    nc = _CACHE[key][0]
    maps = make_in_maps(cfg, inp)
    res = run_bass_kernel_spmd(nc, maps, core_ids=list(range(8)))
    out = np.zeros((4, cfg.SEQ, cfg.D), np.float32)
    global LAST
    LAST = (res, _CACHE.get(key))
    for core in range(8):
        b, half = core // 2, core % 2
        out[b, half * cfg.T:(half + 1) * cfg.T, :] = np.asarray(res.results[core]["outT"]).T
    return out


def kernel(**inputs):
    return run(Cfg(), inputs)
```

```python
import types
import numpy as np
import ml_dtypes
from contextlib import ExitStack
import concourse.bass as bass
import concourse.mybir as mybir
from concourse.bass_utils import run_bass_kernel_spmd

F32 = mybir.dt.float32
BF16 = mybir.dt.bfloat16
AF = mybir.ActivationFunctionType
ALU = mybir.AluOpType
AX = mybir.AxisListType
NEG = -1.0e30
DEBUG = False
HALO = 32


class Cfg:
    def __init__(s, D=4096, SEQ=2048, NH=32, QL=1024, KVL=512, IH=64, TOPK=256, LH=16, DFF=8192, NE=8,
                 DFE=2048, DEPTH=4):
        s.D, s.SEQ, s.NH, s.QL, s.KVL, s.IH, s.LH, s.DFF, s.NE, s.DFE, s.DEPTH = D, SEQ, NH, QL, KVL, IH, LH, DFF, NE, DFE, DEPTH
        s.T = SEQ // 2
        s.S = SEQ
        s.KSEL = min(TOPK, SEQ // 4)
        s.DC = D // 128
        s.AW = NH * 128
        s.ATT_IN = QL + KVL + 128 + IH
        s.LB = D // LH
        s.TT = min(512, s.T)
        s.NT = s.T // s.TT
        s.TB = s.T // 128


class StopBuild(Exception):
    pass


class Res:
    def __init__(s, name):
        s.name, s.w, s.r = name, None, []


class Q:
    def __init__(s, nc, name, sems, is_dma):
        s.nc, s.name, s.sems, s.is_dma = nc, name, sems, is_dma
        s.n = 0
        s.seen = {}
        s.prog = []

    def wait_tok(s, tok):
        kind = tok[0]
        if kind == "c":
            _, F, n = tok
            if F is s and s.name == "pe":
                return
            if s.seen.get(F.name, 0) >= n:
                return
            s.seen[F.name] = n
            sem = F.sems[0]
            s.prog.append(lambda e, sem=sem, n=n: e.wait_ge(sem, n))
        else:
            _, sem, val, key = tok
            if s.seen.get(key, 0) >= val:
                return
            s.seen[key] = val
            s.prog.append(lambda e, sem=sem, val=val: e.wait_ge(sem, val))

    def issue(s, fn, own_sem=None):
        if own_sem is not None:
            s.prog.append(lambda e, fn=fn, sem=own_sem: fn(e).then_inc(sem))
            tok = ("d", own_sem, 1, id(own_sem))
            s.cc_toks = getattr(s, "cc_toks", []) + [tok]
            return tok
        if s.is_dma:
            K = len(s.sems)
            i = s.n
            s.n += 1
            slot = i % K
            sem = s.sems[slot]
            if i >= K:
                prev = 16 * (i // K)
                key = (s.name, slot)
                if s.seen.get(key, 0) < prev:
                    s.seen[key] = prev
                    s.prog.append(lambda e, sem=sem, prev=prev: e.wait_ge(sem, prev))
            s.prog.append(lambda e, fn=fn, sem=sem: fn(e).then_inc(sem, 16))
            return ("d", sem, 16 * (i // K + 1), (s.name, slot))
        s.n += 1
        sem = s.sems[0]
        s.prog.append(lambda e, fn=fn, sem=sem: fn(e).then_inc(sem, 1))
        return ("c", s, s.n)


def _freeze(fn):
    if fn.__closure__ is None:
        return fn
    cells = []
    for cell in fn.__closure__:
        try:
            cells.append(types.CellType(cell.cell_contents))
        except ValueError:
            cells.append(cell)
    return types.FunctionType(fn.__code__, fn.__globals__, fn.__name__, fn.__defaults__, tuple(cells))


def emit(q, fn, reads=(), writes=(), own_sem=None):
    fn = _freeze(fn)
    deps = []
    for t in reads:
        if t.w is not None:
            deps.append(t.w)
    for t in writes:
        if t.w is not None:
            deps.append(t.w)
        deps.extend(t.r)
    for d in deps:
        q.wait_tok(d)
    tok = q.issue(fn, own_sem)
    for t in reads:
        if tok[0] == "c":
            t.r = [x for x in t.r if not (x[0] == "c" and x[1] is tok[1])]
        t.r.append(tok)
    for t in writes:
        t.w = tok
        t.r = []
    return tok


class B:
    def __init__(s, cfg, stop_after=None, only=None):
        s.c = cfg
        s.stop_after = stop_after
        s.only = only
        s.debug = DEBUG
        s.nc = bass.Bass("TRN2", target_bir_lowering=False)
        s.es = ExitStack()
        s.din = {}
        s.nsem = 0

    def sem(s):
        s.nsem += 1
        return s.es.enter_context(s.nc.semaphore(f"s{s.nsem}"))

    def inp(s, name, shape, dt=F32):
        h = s.nc.dram_tensor(name, list(shape), dt, kind="ExternalInput").ap()
        s.din[name] = (tuple(shape), dt)
        return h

    def dram(s, name, shape, dt):
        return s.nc.dram_tensor(name, list(shape), dt).ap()

    def sb(s, name, shape, dt):
        return s.es.enter_context(s.nc.sbuf_tensor("sb_" + name, list(shape), dt))

    def weight(s, name, K, Fd):
        Fp = ((Fd + 127) // 128) * 128
        src = s.inp(name, [Fp, K])
        full = s.dram(name + "_f", [Fp, K], BF16)
        step = max(128, min(Fp, ((1 << 20) // K) // 128 * 128))
        chunks = [(r0, min(Fp, r0 + step)) for r0 in range(0, Fp, step)]
        rls = [Res(name + "_f") for _ in chunks]
        s.wts[name] = (src, full, chunks, rls)
        return full, (chunks, rls)

    def gather(s, name):
        src, full, chunks, rls = s.wts[name]
        for (r0, r1), rl in zip(chunks, rls):
            emit(s.pq, lambda e: e.dma_start(out=full[r0:r1, :], in_=src[r0:r1, :]), [], [rl])

    def gather2(s, loc, rls, full, rf, rows, Fd, dt, name):
        quad = s.dram(name + "_q", [4 * rows, Fd], dt)
        rq = Res(name + "_q")
        emit(s.pq, lambda e: e.collective_compute("AllGather", ALU.bypass, replica_groups=[[0, 1, 2, 3], [4, 5, 6, 7]],
                                                  ins=[loc.opt()], outs=[quad.opt()]), rls, [rq], own_sem=s.sem())
        emit(s.pq, lambda e: e.collective_compute("AllGather", ALU.bypass, replica_groups=[[0, 4], [1, 5], [2, 6], [3, 7]],
                                                  ins=[quad.opt()], outs=[full.opt()]), [rq], [rf], own_sem=s.sem())

    def pair_exchange(s, loc, rl, pair, rp):
        emit(s.pq, lambda e: e.collective_compute("AllGather", ALU.bypass, replica_groups=[[0, 1], [2, 3], [4, 5], [6, 7]],
                                                  ins=[loc.opt()], outs=[pair.opt()]), [rl], [rp], own_sem=s.sem())

    def linear(s, Wd, Wres, k0, KC, cols, xfn, xres, epi, N, pre=None):
        PF = len(s.wt) - 1
        n = len(cols)
        slots = {}
        xr = list(xres)
        Wch = Wres

        def load(j):
            c0, w = cols[j]
            sl = s.wcount % len(s.wt)
            s.wcount += 1
            slots[j] = sl
            wt, wr = s.wt[sl], s.wt_r[sl]
            assert c0 % 128 == 0
            wdep = [rl for (r0, r1), rl in zip(Wch[0], Wch[1]) if r0 < c0 + 128 and r1 > c0]
            emit(s.sq, lambda e, wt=wt, c0=c0, w=w: e.dma_start(
                out=wt[:, 0:KC, 0:w], in_=Wd[c0:c0 + 128, k0:k0 + KC * 128].rearrange("p (kc f) -> p kc f", f=128)[:, :, 0:w]),
                wdep, [wr])
            if pre is not None:
                pre(j)

        for j in range(min(PF, n)):
            load(j)
        for j in range(n):
            if j + PF < n:
                load(j + PF)
            c0, w = cols[j]
            sl = slots[j]
            wt, wr = s.wt[sl], s.wt_r[sl]
            if N > 512:
                pi = s.pcount % 3
                ps = s.psum[:, pi * 1024:(pi + 1) * 1024]
                pr = [s.bank_r[2 * pi], s.bank_r[2 * pi + 1]]
            else:
                pi = 4 + s.pcount % 2
                ps = s.bank[pi]
                pr = [s.bank_r[pi]]
            s.pcount += 1
            for n0 in range(0, N, 512):
                n1 = min(N, n0 + 512)
                for kc in range(KC):
                    rhs = xfn(kc, n0, n1)
                    emit(s.pe, lambda e, ps=ps, wt=wt, kc=kc, w=w, n0=n0, n1=n1, rhs=rhs: e.matmul(
                        ps[0:w, n0:n1], lhsT=wt[:, kc, 0:w], rhs=rhs, start=(kc == 0), stop=(kc == KC - 1)),
                        [wr] + xr, pr)
            epi(j, ps, pr, w)

    def next_xt(s):
        sl = s.xcount % len(s.xt)
        s.xcount += 1
        return s.xt[sl], s.xt_r[sl]

    def resid(s, gate):
        c = s.c
        slots = {}

        def pre(j):
            xt, xr = s.next_xt()
            slots[j] = (xt, xr)
            emit(s.sq, lambda e: e.dma_start(out=xt, in_=s.xs[j * 128:(j + 1) * 128, :]), [s.xs_r[j]], [xr])

        def epi(j, ps, pr, w):
            xt, xr = slots[j]
            emit(s.dve, lambda e: e.scalar_tensor_tensor(out=xt, in0=ps[:, 0:c.T], scalar=gate[:, j:j + 1], in1=xt,
                                                         op0=ALU.mult, op1=ALU.add), pr + [xr, s.mod_r], [xr])
            emit(s.sq, lambda e: e.dma_start(out=s.xs[j * 128:(j + 1) * 128, :], in_=xt), [xr], [s.xs_r[j]])

        return pre, epi

    def colsum_bc(s, acc, acc_r, out, out_r, scale, eps, power):
        c = s.c
        ps, pr = s.bank[6], [s.bank_r[6]]
        for n0 in range(0, c.T, 512):
            n1 = min(c.T, n0 + 512)
            emit(s.pe, lambda e, n0=n0, n1=n1: e.matmul(ps[:, 0:n1 - n0], lhsT=s.ones32[:, :], rhs=acc[:, n0:n1], start=True, stop=True),
                 acc_r + [s.const_r], pr)
            emit(s.dve, lambda e, n0=n0, n1=n1: e.tensor_scalar(out=out[:, n0:n1], in0=ps[:, 0:n1 - n0], scalar1=scale, scalar2=eps,
                                                                  op0=ALU.mult, op1=ALU.add), pr, out_r)
        if power is not None:
            s.rpow(out, out_r, power)

    def rpow(s, out, out_r, power):
        emit(s.act, lambda e: e.activation(out=out, in_=out, func=AF.Ln), out_r, out_r)
        emit(s.act, lambda e: e.activation(out=out, in_=out, func=AF.Exp, scale=power), out_r, out_r)

    def norm(s, gs, sh, out_fn=None, hook=None):
        c = s.c
        acc, acc_r = s.st[0], [s.st_r[0]]
        for dc in range(c.DC):
            xt, xr = s.next_xt()
            emit(s.sq, lambda e: e.dma_start(out=xt, in_=s.xs[dc * 128:(dc + 1) * 128, :]), [s.xs_r[dc]], [xr])
            if dc == 0:
                emit(s.act, lambda e: e.activation(out=acc, in_=xt, func=AF.Square), [xr], acc_r)
            else:
                tq, tr = s.tmp[dc % 2], s.tmp_r[dc % 2]
                emit(s.act, lambda e: e.activation(out=tq, in_=xt, func=AF.Square), [xr], [tr])
                emit(s.dve, lambda e: e.tensor_tensor(out=acc, in0=acc, in1=tq, op=ALU.add), [tr] + acc_r, acc_r)
        rstd, rstd_r = s.st[1], [s.st_r[1]]
        s.colsum_bc(acc, acc_r, rstd, rstd_r, 1.0 / c.D, 1e-6, -0.5)
        if "rstd" not in s.dbg_map:
            s.dbg("acc", acc, acc_r, c.T)
            s.dbg("rstd", rstd, rstd_r, c.T)
        for dc in range(c.DC):
            xt, xr = s.next_xt()
            emit(s.sq, lambda e: e.dma_start(out=xt, in_=s.xs[dc * 128:(dc + 1) * 128, :]), [s.xs_r[dc]], [xr])
            emit(s.dve, lambda e: e.tensor_tensor(out=xt, in0=xt, in1=rstd, op=ALU.mult), [xr] + rstd_r, [xr])
            bias = sh[:, dc:dc + 1] if sh is not None else 0.0
            if out_fn is not None:
                out_fn(dc, xt, xr, gs)
            elif hook is not None:
                tq, tr = s.tmp[dc % 2], s.tmp_r[dc % 2]
                emit(s.act, lambda e: e.activation(out=tq, in_=xt, func=AF.Identity, bias=bias, scale=gs[:, dc:dc + 1]),
                     [xr, s.mod_r], [tr])
                emit(s.dve, lambda e: e.tensor_copy(out=s.hT[:, dc, :], in_=tq), [tr], [s.hT_r[dc]])
                hook(dc, tq, tr)
            else:
                emit(s.act, lambda e: e.activation(out=s.hT[:, dc, :], in_=xt, func=AF.Identity, bias=bias, scale=gs[:, dc:dc + 1]),
                     [xr, s.mod_r], [s.hT_r[dc]])
                if dc == 0 and "h0" not in s.dbg_map:
                    s.dbg("h0", s.hT[:, 0, :], [s.hT_r[0]], c.T)

    def hx(s, kc, n0, n1):
        return s.hT[:, kc, n0:n1]

    def midx(s, kc, n0, n1):
        return s.mid[:, kc, HALO + n0:HALO + n1]

    def midc(s, dc):
        return s.mid[:, dc, HALO:HALO + s.c.T]

    def halo_exchange(s, tag):
        c = s.c
        loc = s.dram(f"halo_l{tag}", [c.D, HALO], BF16)
        pair = s.dram(f"halo_p{tag}", [2 * c.D, HALO], BF16)
        rl, rp = Res("hl"), Res("hp")
        emit(s.pq, lambda e: e.dma_start(out=loc.rearrange("(dc p) h -> p dc h", p=128), in_=s.mid[:, :, c.T:c.T + HALO]), s.mid_r, [rl])
        s.pair_exchange(loc, rl, pair, rp)
        emit(s.sq, lambda e: e.dma_start(out=s.mid[:, :, 0:HALO], in_=pair[0:c.D, :].rearrange("(dc p) h -> p dc h", p=128)), [rp], s.mid_r)
        emit(s.dve, lambda e: e.tensor_scalar(out=s.mid[:, :, 0:HALO], in0=s.mid[:, :, 0:HALO], scalar1=s.flag[:, 0:1], scalar2=None, op0=ALU.mult),
             s.mid_r + [s.const_r], s.mid_r)

    def dwconv(s, dc, cw, K, out, out_r, bias=None):
        c = s.c
        for k in range(K):
            off = HALO - (K - 1) + k
            src = s.mid[:, dc, off:off + c.T]
            wk = cw[:, k * c.DC + dc:k * c.DC + dc + 1]
            if k == 0:
                if bias is not None:
                    emit(s.dve, lambda e, src=src, wk=wk: e.tensor_scalar(out=out, in0=src, scalar1=wk, scalar2=bias[:, dc:dc + 1],
                                                                        op0=ALU.mult, op1=ALU.add), [s.mid_r[dc], s.cv_r], out_r)
                else:
                    emit(s.dve, lambda e, src=src, wk=wk: e.tensor_scalar(out=out, in0=src, scalar1=wk, scalar2=None, op0=ALU.mult),
                         [s.mid_r[dc], s.cv_r], out_r)
            else:
                emit(s.dve, lambda e, src=src, wk=wk: e.scalar_tensor_tensor(out=out, in0=src, scalar=wk, in1=out,
                                                                           op0=ALU.mult, op1=ALU.add), [s.mid_r[dc], s.cv_r] + out_r, out_r)

    def load_small(s, dst, src, res):
        emit(s.sq, lambda e: e.dma_start(out=dst, in_=src), [], [res])

    def build(s):
        c = s.c
        nc = s.nc
        es = s.es
        D, T, DC = c.D, c.T, c.DC
        s.pe = Q(nc, "pe", [s.sem()], False)
        s.act = Q(nc, "act", [s.sem()], False)
        s.dve = Q(nc, "dve", [s.sem()], False)
        s.sq = Q(nc, "sq", [s.sem() for _ in range(8)], True)
        s.pq = Q(nc, "pq", [s.sem() for _ in range(8)], True)
        s.wts = {}
        s.wcount = s.pcount = s.xcount = 0

        xT_in = s.inp("xT", [D, T])
        out_d = nc.dram_tensor("outT", [D, T], F32, kind="ExternalOutput").ap()
        s.dbg_d = nc.dram_tensor("dbg", [128, 8192], F32, kind="ExternalOutput").ap() if s.debug else None
        s.dbg_off = 0
        s.dbg_map = {}
        cT_in = s.inp("cT", [128, DC * 4])
        adaw_in = s.inp("ada_w", [D, 6 * D // 8])
        adab_in = s.inp("ada_b", [128, 6 * DC // 8])
        adat_in = s.inp("ada_t", [128, c.DEPTH * 6 * DC])
        nmix_in = s.inp("norm_mix", [128, c.DEPTH * DC])
        nffn_in = s.inp("norm_ffn", [128, c.DEPTH * DC])
        nfin_in = s.inp("norm_final", [128, DC])
        bsel_in = s.inp("bsel", [128, 4])
        flag_in = s.inp("flag", [128, 2])
        ident_in = s.inp("ident", [128, 128])
        tri_in = s.inp("tri", [128, 128])
        s.ohs_in = s.inp("ohs", [128, 2 * 32 * 128])
        s.rbb_in = s.inp("rbb", [128, 32 * c.NH])
        s.gcq_in = s.inp("g_cq", [128, c.QL // 128])
        s.gckv_in = s.inp("g_ckv", [128, c.KVL // 128])
        s.scw_in = s.inp("sconv_cw", [128, 3 * DC])
        s.cfw_in = s.inp("conf_cw", [128, 31 * DC])
        s.cfv_in = s.inp("conf_vec", [128, 3 * DC])
        s.lrw_in = s.inp("lru_cw", [128, 4 * DC])
        s.lrv_in = s.inp("lru_vec", [128, 4 * DC])
        s.rt_in = s.inp("router", [128, 2 * DC * 8])

        s.xs = s.dram("xs", [D, T], F32)
        s.xs_r = [Res(f"xs{i}") for i in range(DC)]

        s.full = (c.D == 4096)
        s.hT = s.sb("hT", [128, DC, T], BF16)
        s.hT_r = [Res(f"hT{i}") for i in range(DC)]
        s.mid = s.sb("mid", [128, DC, T + HALO], BF16)
        s.mid_r = [Res(f"mid{i}") for i in range(DC)]
        NW = 2 if s.full else 3
        s.wt = [s.sb(f"wt{i}", [128, DC, 128], BF16) for i in range(NW)]
        s.wt_r = [Res(f"wt{i}") for i in range(NW)]
        s.scr = s.sb("scr", [128, 7 * T], F32)
        s.xt = [s.scr[:, i * T:(i + 1) * T] for i in range(2)]
        s.xt_r = [Res(f"xt{i}") for i in range(2)]
        s.tmp = [s.scr[:, (2 + i) * T:(3 + i) * T] for i in range(2)]
        s.tmp_r = [Res(f"tmp{i}") for i in range(2)]
        s.st = [s.scr[:, (4 + i) * T:(5 + i) * T] for i in range(2)]
        s.st_r = [Res(f"st{i}") for i in range(2)]
        s.px = s.scr[:, 6 * T:7 * T]
        s.px_r = Res("px")
        s.xt = s.xt + [s.px]
        s.xt_r = s.xt_r + [s.px_r]
        psum = es.enter_context(nc.psum_tensor("psum", [128, 4096], F32))
        s.bank = [psum[:, i * 512:(i + 1) * 512] for i in range(8)]
        s.bank_r = [Res(f"bank{i}") for i in range(8)]
        s.psum = psum
        s.const_r = Res("const")
        s.ones32 = s.sb("ones32", [128, 128], F32)
        s.ident32 = s.sb("ident32", [128, 128], F32)
        s.identb = s.sb("identb", [128, 128], BF16)
        s.onesb = s.sb("onesb", [128, 128], BF16)
        s.tri = s.sb("tri", [128, 128], F32)
        s.flag = s.sb("flag", [128, 2], F32)
        bsel = s.sb("bsel", [128, 4], F32)
        modsel = s.sb("modsel", [128, 6 * DC], F32)
        s.modL = s.sb("modL", [128, c.DEPTH * 6 * DC], F32)
        nmix = s.sb("nmix", [128, c.DEPTH * DC], F32)
        nffn = s.sb("nffn", [128, c.DEPTH * DC], F32)
        s.nfin = s.sb("nfin", [128, DC], F32)
        s.rtf = s.sb("rtf", [128, DC * 8], F32)
        s.rtf_r = Res("rtf")
        s.gtok = s.sb("gtok", [128, c.TB, 8], F32)
        s.gtok_r = Res("gtok")
        s.sm = [s.sb(f"sm{i}", [128, 16], F32) for i in range(3)]
        s.sm_r = Res("sm")
        s.gb = s.sb("gb", [128, 128], F32)
        s.gb_r = Res("gb")
        s.EB = s.sb("EB", [128, c.NH * 256], BF16)
        s.EB_r = Res("EB")
        s.cvw = s.sb("cvw", [128, 31 * DC], F32)
        s.cvv = s.sb("cvv", [128, 4 * DC], F32)
        s.cv_r = Res("cv")
        s.vec = s.sb("vec", [128, 4 * DC], F32)
        s.vec_r = Res("vec")
        s.mod_r = Res("modL")
        small_r = Res("small")

        for dst, src in ((s.ident32[:, :], ident_in), (s.tri[:, :], tri_in), (s.flag[:, :], flag_in), (bsel[:, :], bsel_in),
                         (s.modL[:, :], adat_in), (nmix[:, :], nmix_in), (nffn[:, :], nffn_in), (s.nfin[:, :], nfin_in)):
            s.load_small(dst, src, small_r)
        emit(s.dve, lambda e: e.memset(s.ones32[:, :], 1.0), [], [s.const_r])
        emit(s.dve, lambda e: e.memset(s.onesb[:, :], 1.0), [s.const_r], [s.const_r])
        emit(s.dve, lambda e: e.tensor_copy(out=s.identb[:, :], in_=s.ident32[:, :]), [small_r, s.const_r], [s.const_r])

        for dc in range(DC):
            emit(s.sq, lambda e, dc=dc: e.dma_start(out=s.xs[dc * 128:(dc + 1) * 128, :], in_=xT_in[dc * 128:(dc + 1) * 128, :]), [], [s.xs_r[dc]])

        W = {}
        s.W = W
        decls = [("att_w_in", D, c.ATT_IN), ("att_w_qidx", c.QL, c.IH * 128), ("att_w_uq", c.QL, c.AW), ("att_w_ukT", c.AW, c.KVL),
                 ("att_w_uv", c.KVL, c.AW), ("att_w_out", c.AW, D), ("ffn_w13_0", D, 2 * c.DFF), ("ffn_w2_0", c.DFF, D),
                 ("sconv_w_in", D, 3 * D), ("sconv_w_out", D, D), ("moe_w13_0", D, c.NE * 2 * c.DFE), ("moe_w2_0", c.DFE, c.NE * D),
                 ("conf_w_in", D, 2 * D), ("conf_w_out", D, D), ("ffn_w13_1", D, 2 * c.DFF), ("ffn_w2_1", c.DFF, D),
                 ("lru_w_in", D, 2 * D), ("lru_w_a", D, c.LB), ("lru_w_x", D, c.LB), ("lru_w_out", D, D),
                 ("moe_w13_1", D, c.NE * 2 * c.DFE), ("moe_w2_1", c.DFE, c.NE * D)]
        for (name, K, Fd) in decls:
            W[name] = s.weight(name, K, Fd)
        order = [d[0] for d in decls]
        gi = [0]

        def gather_upto(name):
            while gi[0] < len(order) and gi[0] <= order.index(name):
                s.gather(order[gi[0]])
                gi[0] += 1

        s.cast_upto = gather_upto

        NCL = 6 * DC // 8
        cT = s.st[0]
        emit(s.sq, lambda e: e.dma_start(out=cT[:, 0:DC * 4], in_=cT_in), [], [s.st_r[0]])
        scT = s.sb("scT", [128, DC * 4], BF16)
        scT_r = Res("scT")
        emit(s.act, lambda e: e.activation(out=scT[:, :], in_=cT[:, 0:DC * 4], func=AF.Silu), [s.st_r[0]], [scT_r])
        adab = s.sb("adab", [128, NCL], F32)
        s.load_small(adab[:, :], adab_in, small_r)
        modp = s.sb("modp", [128, NCL * 4], F32)
        modp_r = Res("modp")
        for j in range(NCL):
            sl = s.wcount % len(s.wt)
            s.wcount += 1
            wt, wr = s.wt[sl], s.wt_r[sl]
            emit(s.pq, lambda e, wt=wt, j=j: e.dma_start(out=wt[:, 0:DC, :], in_=adaw_in[:, j * 128:(j + 1) * 128].rearrange("(kc p) f -> p kc f", p=128)),
                 [], [wr])
            ps, pr = s.bank[6 + j % 2], [s.bank_r[6 + j % 2]]
            for kc in range(DC):
                emit(s.pe, lambda e, ps=ps, wt=wt, kc=kc: e.matmul(ps[:, 0:4], lhsT=wt[:, kc, :], rhs=scT[:, kc * 4:(kc + 1) * 4],
                                                                   start=(kc == 0), stop=(kc == DC - 1)), [wr, scT_r], pr)
            emit(s.dve, lambda e, ps=ps, j=j: e.tensor_scalar(out=modp[:, j * 4:(j + 1) * 4], in0=ps[:, 0:4], scalar1=adab[:, j:j + 1], scalar2=None,
                                                              op0=ALU.add), pr + [small_r], [modp_r])
        modl_d = s.dram("modl", [NCL * 128, 4], F32)
        modf_d = s.dram("modf", [6 * D, 4], F32)
        rl, rf = Res("modl"), Res("modf")
        emit(s.pq, lambda e: e.dma_start(out=modl_d.rearrange("(j p) b -> p j b", p=128), in_=modp[:, :].rearrange("p (j b) -> p j b", b=4)), [modp_r], [rl])
        s.gather2(modl_d, [rl], modf_d, rf, NCL * 128, 4, F32, "modg")
        modall = s.st[1]
        m3 = modall[:, 0:6 * DC * 4].rearrange("p (j b) -> p j b", b=4)
        emit(s.sq, lambda e: e.dma_start(out=m3, in_=modf_d.rearrange("(j p) b -> p j b", p=128)), [rf], [s.st_r[1]])
        ms_r = Res("modsel")
        emit(s.dve, lambda e: e.tensor_scalar(out=modsel[:, :], in0=m3[:, :, 0], scalar1=bsel[:, 0:1], scalar2=None, op0=ALU.mult), [s.st_r[1], small_r], [ms_r])
        for b in range(1, 4):
            emit(s.dve, lambda e, b=b: e.scalar_tensor_tensor(out=modsel[:, :], in0=m3[:, :, b], scalar=bsel[:, b:b + 1], in1=modsel[:, :],
                                                              op0=ALU.mult, op1=ALU.add), [s.st_r[1], ms_r], [ms_r])
        mod_r = s.mod_r
        for i in range(c.DEPTH):
            o = i * 6 * DC
            emit(s.dve, lambda e, o=o: e.tensor_tensor(out=s.modL[:, o:o + 6 * DC], in0=s.modL[:, o:o + 6 * DC],
                                                       in1=modsel[:, :], op=ALU.add), [ms_r, small_r, mod_r], [mod_r])
            for (j, nw) in ((1, nmix), (4, nffn)):
                emit(s.dve, lambda e, o=o, j=j, nw=nw, i=i: e.scalar_tensor_tensor(
                    out=s.modL[:, o + j * DC:o + (j + 1) * DC], in0=s.modL[:, o + j * DC:o + (j + 1) * DC], scalar=1.0,
                    in1=nw[:, i * DC:(i + 1) * DC], op0=ALU.add, op1=ALU.mult), [mod_r, small_r], [mod_r])

        def M(i, j):
            o = i * 6 * DC + j * DC
            return s.modL[:, o:o + DC]

        gather_upto("att_w_out")

        s.dbg("modL", s.modL[:, :], [mod_r], c.DEPTH * 6 * DC)
        s.dbg("modsel", modsel[:, :], [ms_r], 6 * DC)
        s.dbg("modp", modp[:, :], [modp_r], NCL * 4)

        done = False
        try:
            s.layers(M, gather_upto)
        except StopBuild:
            done = True
        for i in range(0):
            if s.stop_after is not None and s.stop_after[0] == "pre":
                done = True
                break
            kind = i % 4
            if kind == 0:
                s.attention(M(i, 1), M(i, 0), M(i, 2))
                gather_upto("moe_w2_0")
            elif kind == 1:
                s.sconv(M(i, 1), M(i, 0), M(i, 2))
                gather_upto("ffn_w2_1")
            elif kind == 2:
                s.conformer(M(i, 1), M(i, 0), M(i, 2))
                gather_upto("moe_w2_1")
            else:
                s.rglru(M(i, 1), M(i, 0), M(i, 2))
            if s.stop_after == ("mix", i):
                done = True
                break
            if i % 2 == 0:
                s.norm(M(i, 4), M(i, 3))
                s.ffn(W[f"ffn_w13_{i // 2}"], W[f"ffn_w2_{i // 2}"], M(i, 5))
            else:
                s.moe(W[f"moe_w13_{i // 2}"], W[f"moe_w2_{i // 2}"], M(i, 4), M(i, 3), M(i, 5), i // 2)
            if s.stop_after == ("ffn", i):
                done = True
                break

        s.out_r = Res("out")

        def fin(dc, xt, xr, gs):
            emit(s.act, lambda e: e.activation(out=xt, in_=xt, func=AF.Identity, scale=gs[:, dc:dc + 1]), [xr, small_r], [xr])
            emit(s.sq, lambda e: e.dma_start(out=out_d[dc * 128:(dc + 1) * 128, :], in_=xt), [xr], [s.out_r])

        if not done:
            s.norm(s.nfin, None, out_fn=fin)
        else:
            for dc in range(DC):
                xt, xr = s.next_xt()
                emit(s.sq, lambda e: e.dma_start(out=xt, in_=s.xs[dc * 128:(dc + 1) * 128, :]), [s.xs_r[dc]], [xr])
                emit(s.pq, lambda e: e.dma_start(out=out_d[dc * 128:(dc + 1) * 128, :], in_=xt), [xr], [s.out_r])
        for tok in getattr(s.pq, "cc_toks", []):
            s.pq.wait_tok(tok)
        for q in (s.pe, s.act, s.dve):
            if q.n > 0:
                s.pq.wait_tok(("c", q, q.n))
        for q in (s.pq, s.sq):
            K = len(q.sems)
            for i in range(max(0, q.n - K), q.n):
                q.wait_tok(("d", q.sems[i % K], 16 * (i // K + 1), (q.name, i % K)))

        with nc.Block() as block:
            @block.tensor
            def _(e):
                for f in s.pe.prog:
                    f(e)

            @block.scalar
            def _(e):
                for f in s.act.prog:
                    f(e)

            @block.vector
            def _(e):
                for f in s.dve.prog:
                    f(e)

            @block.sync
            def _(e):
                for f in s.sq.prog:
                    f(e)

            @block.gpsimd
            def _(e):
                for f in s.pq.prog:
                    f(e)
        es.close()
        return nc

    def dbg(s, name, ap, res, n):
        if not s.debug or s.dbg_off + n > 8192:
            return
        o = s.dbg_off
        s.dbg_off += n
        s.dbg_map[name] = (o, n)
        emit(s.pq, lambda e: e.dma_start(out=s.dbg_d[:, o:o + n], in_=ap), res, [Res("dbg")])

    def chk(s, tag, kind="att"):
        if s.stop_after == (kind, tag):
            raise StopBuild()

    def layers(s, M, gather_upto):
        c = s.c
        W = s.W
        for i in range(c.DEPTH):
            if s.stop_after is not None and s.stop_after[0] == "pre":
                raise StopBuild()
            kind = i % 4
            if s.only is None or f"mix{i}" in s.only:
                gather_upto(["att_w_out", "sconv_w_out", "conf_w_out", "lru_w_out"][kind])
                if kind == 0:
                    s.attention(M(i, 1), M(i, 0), M(i, 2))
                elif kind == 1:
                    s.sconv(M(i, 1), M(i, 0), M(i, 2))
                elif kind == 2:
                    s.conformer(M(i, 1), M(i, 0), M(i, 2))
                else:
                    s.rglru(M(i, 1), M(i, 0), M(i, 2))
            if s.stop_after == ("mix", i):
                raise StopBuild()
            if s.only is None or f"ffn{i}" in s.only:
                gather_upto(["ffn_w2_0", "moe_w2_0", "ffn_w2_1", "moe_w2_1"][i])
                if i % 2 == 0:
                    s.norm(M(i, 4), M(i, 3))
                    s.ffn(W[f"ffn_w13_{i // 2}"], W[f"ffn_w2_{i // 2}"], M(i, 5))
                else:
                    s.moe(W[f"moe_w13_{i // 2}"], W[f"moe_w2_{i // 2}"], M(i, 4), M(i, 3), M(i, 5), i // 2)
            if s.stop_after == ("ffn", i):
                raise StopBuild()

    def swiglu_epi(s, gbc=None, gbc_r=None):
        c = s.c

        def epi(idx, ps, pr, w):
            jj, which = idx // 2, idx % 2
            tq, tr = s.tmp[jj % 2], s.tmp_r[jj % 2]
            if which == 0:
                emit(s.act, lambda e: e.activation(out=tq, in_=ps[:, 0:c.T], func=AF.Silu), pr, [tr])
                if gbc is not None:
                    emit(s.dve, lambda e: e.tensor_tensor(out=tq, in0=tq, in1=gbc, op=ALU.mult), [tr] + gbc_r, [tr])
            else:
                emit(s.dve, lambda e: e.tensor_tensor(out=s.midc(jj), in0=ps[:, 0:c.T], in1=tq, op=ALU.mult), pr + [tr], [s.mid_r[jj]])
        return epi

    def ffn(s, W13, W2, gate):
        c = s.c
        (w13, r13), (w2, r2) = W13, W2
        G = c.DC
        HC = c.DFF // 128
        for g in range(HC // G):
            cols = []
            for jj in range(G):
                j = g * G + jj
                cols.append((j * 128, 128))
                cols.append((c.DFF + j * 128, 128))
            s.linear(w13, r13, 0, c.DC, cols, s.hx, s.hT_r, s.swiglu_epi(), c.T)
            if "act0" not in s.dbg_map:
                s.dbg("act0", s.midc(0), [s.mid_r[0]], c.T)
            pre, epi2 = s.resid(gate)
            s.linear(w2, r2, g * G * 128, G, [(d * 128, 128) for d in range(c.DC)], s.midx, s.mid_r, epi2, c.T, pre=pre)

    def moe(s, W13, W2, gs, sh, gate, li):
        c = s.c
        (w13, r13), (w2, r2) = W13, W2
        DC, T = c.DC, c.T
        rtf = s.rtf
        emit(s.sq, lambda e: e.dma_start(out=rtf[:, :], in_=s.rt_in[:, li * DC * 8:(li + 1) * DC * 8]), [], [s.rtf_r])
        lgps = s.psum[:, 2048:3072]
        lgps_r = [s.bank_r[4], s.bank_r[5]]

        def hook(dc, tq, tr):
            for n0 in range(0, T, 512):
                n1 = min(T, n0 + 512)
                emit(s.pe, lambda e, n0=n0, n1=n1: e.matmul(lgps[0:8, n0:n1], lhsT=rtf[:, dc * 8:(dc + 1) * 8], rhs=tq[:, n0:n1],
                                                            start=(dc == 0), stop=(dc == DC - 1)), [tr, s.rtf_r], lgps_r)

        s.norm(gs, sh, hook=hook)
        s.chk(1, "moe")
        lgT = s.st[0]
        emit(s.dve, lambda e: e.tensor_copy(out=lgT[0:8, :], in_=lgps[0:8, 0:T]), lgps_r, [s.st_r[0]])
        gtok = s.gtok
        l8, m8, ex = s.sm[0], s.sm[1], s.sm[2]
        r8 = [s.sm_r]
        for tb in range(c.TB):
            ps, pr = s.bank[6], [s.bank_r[6]]
            emit(s.pe, lambda e, tb=tb: e.matmul(ps[:, 0:8], lhsT=lgT[0:8, tb * 128:(tb + 1) * 128], rhs=s.ident32[0:8, 0:8], start=True, stop=True),
                 [s.st_r[0], s.const_r], pr)
            emit(s.dve, lambda e: e.tensor_copy(out=l8[:, 0:8], in_=ps[:, 0:8]), pr, r8)
            emit(s.dve, lambda e: e.max(out=m8[:, 0:8], in_=l8[:, 0:8]), r8, r8)
            emit(s.dve, lambda e: e.tensor_scalar(out=ex[:, 8:9], in0=m8[:, 0:1], scalar1=-1.0, scalar2=None, op0=ALU.mult), r8, r8)
            emit(s.act, lambda e: e.activation(out=ex[:, 0:8], in_=l8[:, 0:8], func=AF.Exp, bias=ex[:, 8:9], scale=1.0), r8, r8)
            emit(s.dve, lambda e: e.scalar_tensor_tensor(out=ex[:, 0:8], in0=l8[:, 0:8], scalar=m8[:, 1:2], in1=ex[:, 0:8], op0=ALU.is_ge, op1=ALU.mult),
                 r8, r8)
            emit(s.dve, lambda e: e.tensor_reduce(out=ex[:, 9:10], in_=ex[:, 0:8], axis=AX.X, op=ALU.add), r8, r8)
            emit(s.dve, lambda e: e.reciprocal(out=ex[:, 9:10], in_=ex[:, 9:10]), r8, r8)
            emit(s.dve, lambda e, tb=tb: e.tensor_scalar(out=gtok[:, tb, :], in0=ex[:, 0:8], scalar1=ex[:, 9:10], scalar2=None, op0=ALU.mult),
                 r8, [s.gtok_r])
        s.chk(2, "moe")
        HCE = c.DFE // 128
        for ei in range(c.NE):
            gps = s.psum[:, 2048:3072]
            gpr = [s.bank_r[4], s.bank_r[5]]
            for tb in range(c.TB):
                emit(s.dve, lambda e, tb=tb: e.tensor_copy(out=s.gb[:, :], in_=gtok[:, tb, ei:ei + 1].to_broadcast([128, 128])), [s.gtok_r], [s.gb_r])
                emit(s.pe, lambda e, tb=tb: e.matmul(gps[:, tb * 128:(tb + 1) * 128], lhsT=s.gb[:, :], rhs=s.ident32[:, :], start=True, stop=True),
                     [s.gb_r, s.const_r], gpr)
            gbc, gbc_r = s.st[1], [s.st_r[1]]
            emit(s.act, lambda e: e.activation(out=gbc, in_=gps[:, 0:T], func=AF.Identity), gpr, gbc_r)
            s.chk(3, "moe")
            cols = []
            for jj in range(HCE):
                cols.append((ei * 2 * c.DFE + jj * 128, 128))
                cols.append((ei * 2 * c.DFE + c.DFE + jj * 128, 128))
            s.linear(w13, r13, 0, DC, cols, s.hx, s.hT_r, s.swiglu_epi(gbc, gbc_r), T)
            s.chk(4, "moe")
            pre, epi2 = s.resid(gate)
            s.linear(w2, r2, 0, HCE, [(ei * c.D + d * 128, 128) for d in range(DC)], s.midx, s.mid_r[0:HCE], epi2, T, pre=pre)

    def sconv(s, gs, sh, gate):
        c = s.c
        D, DC, T = c.D, c.DC, c.T
        (w_in, r_in), (w_out, r_out) = s.W["sconv_w_in"], s.W["sconv_w_out"]
        s.load_small(s.cvw[:, 0:3 * DC], s.scw_in, s.cv_r)
        s.norm(gs, sh)
        cols = []
        for dc in range(DC):
            cols += [(D + dc * 128, 128), (2 * D + dc * 128, 128)]

        def epi1(idx, ps, pr, w):
            dc, which = idx // 2, idx % 2
            tq, tr = s.tmp[dc % 2], s.tmp_r[dc % 2]
            if which == 0:
                emit(s.act, lambda e: e.activation(out=tq, in_=ps[:, 0:T], func=AF.Identity), pr, [tr])
            else:
                emit(s.dve, lambda e: e.tensor_tensor(out=s.midc(dc), in0=ps[:, 0:T], in1=tq, op=ALU.mult), pr + [tr], [s.mid_r[dc]])

        s.linear(w_in, r_in, 0, DC, cols, s.hx, s.hT_r, epi1, T)
        s.halo_exchange("sc")
        if s.only is None:
            s.cast_upto("conf_w_out")

        def epi2(dc, ps, pr, w):
            tq, tr = s.tmp[dc % 2], [s.tmp_r[dc % 2]]
            s.dwconv(dc, s.cvw, 3, tq, tr)
            emit(s.dve, lambda e: e.tensor_tensor(out=s.midc(dc), in0=ps[:, 0:T], in1=tq, op=ALU.mult), pr + tr, [s.mid_r[dc]])

        s.linear(w_in, r_in, 0, DC, [(dc * 128, 128) for dc in range(DC)], s.hx, s.hT_r, epi2, T)
        pre, epi3 = s.resid(gate)
        s.linear(w_out, r_out, 0, DC, [(d * 128, 128) for d in range(DC)], s.midx, s.mid_r, epi3, T, pre=pre)

    def conformer(s, gs, sh, gate):
        c = s.c
        D, DC, T = c.D, c.DC, c.T
        (w_in, r_in), (w_out, r_out) = s.W["conf_w_in"], s.W["conf_w_out"]
        s.load_small(s.cvw[:, 0:31 * DC], s.cfw_in, s.cv_r)
        s.load_small(s.cvv[:, 0:3 * DC], s.cfv_in, s.cv_r)
        s.norm(gs, sh)
        cols = []
        for dc in range(DC):
            cols += [(D + dc * 128, 128), (dc * 128, 128)]

        def epi1(idx, ps, pr, w):
            dc, which = idx // 2, idx % 2
            tq, tr = s.tmp[dc % 2], s.tmp_r[dc % 2]
            if which == 0:
                emit(s.act, lambda e: e.activation(out=tq, in_=ps[:, 0:T], func=AF.Sigmoid), pr, [tr])
            else:
                emit(s.dve, lambda e: e.tensor_tensor(out=s.midc(dc), in0=ps[:, 0:T], in1=tq, op=ALU.mult), pr + [tr], [s.mid_r[dc]])

        s.linear(w_in, r_in, 0, DC, cols, s.hx, s.hT_r, epi1, T)
        s.halo_exchange("cf")
        if s.only is None:
            s.cast_upto("lru_w_out")
        acc1, acc2 = s.st[0], s.st[1]
        a1r, a2r = [s.st_r[0]], [s.st_r[1]]
        for dc in range(DC):
            tq, tr = s.tmp[dc % 2], [s.tmp_r[dc % 2]]
            s.dwconv(dc, s.cvw, 31, tq, tr, bias=s.cvv[:, 0:DC])
            sq, sqr = s.next_xt()
            emit(s.act, lambda e: e.activation(out=sq, in_=tq, func=AF.Square), tr, [sqr])
            emit(s.act, lambda e: e.activation(out=s.hT[:, dc, :], in_=tq, func=AF.Identity), tr, [s.hT_r[dc]])
            if dc == 0:
                emit(s.dve, lambda e: e.tensor_copy(out=acc1, in_=tq), tr, a1r)
                emit(s.dve, lambda e: e.tensor_copy(out=acc2, in_=sq), [sqr], a2r)
            else:
                emit(s.dve, lambda e: e.tensor_tensor(out=acc1, in0=acc1, in1=tq, op=ALU.add), tr + a1r, a1r)
                emit(s.dve, lambda e: e.tensor_tensor(out=acc2, in0=acc2, in1=sq, op=ALU.add), [sqr] + a2r, a2r)
        mean, mr = s.xt[0], [s.xt_r[0]]
        rstd, rr = s.xt[1], [s.xt_r[1]]
        s.colsum_bc(acc1, a1r, mean, mr, 1.0 / D, 0.0, None)
        s.colsum_bc(acc2, a2r, rstd, rr, 1.0 / D, 0.0, None)
        m2, m2r = s.px, [s.px_r]
        emit(s.dve, lambda e: e.tensor_tensor(out=m2, in0=mean, in1=mean, op=ALU.mult), mr, m2r)
        emit(s.dve, lambda e: e.tensor_tensor(out=rstd, in0=rstd, in1=m2, op=ALU.subtract), rr + m2r, rr)
        emit(s.dve, lambda e: e.tensor_scalar(out=rstd, in0=rstd, scalar1=1e-6, scalar2=None, op0=ALU.add), rr, rr)
        s.rpow(rstd, rr, -0.5)
        for dc in range(DC):
            tq, tr = s.tmp[dc % 2], [s.tmp_r[dc % 2]]
            emit(s.dve, lambda e: e.tensor_tensor(out=tq, in0=s.hT[:, dc, :], in1=mean, op=ALU.subtract), [s.hT_r[dc]] + mr, tr)
            emit(s.dve, lambda e: e.tensor_tensor(out=tq, in0=tq, in1=rstd, op=ALU.mult), tr + rr, tr)
            emit(s.act, lambda e: e.activation(out=s.midc(dc), in_=tq, func=AF.Silu, bias=s.cvv[:, 2 * DC + dc:2 * DC + dc + 1],
                                               scale=s.cvv[:, DC + dc:DC + dc + 1]), tr + [s.cv_r], [s.mid_r[dc]])
        pre, epi3 = s.resid(gate)
        s.linear(w_out, r_out, 0, DC, [(d * 128, 128) for d in range(DC)], s.midx, s.mid_r, epi3, T, pre=pre)

    def rglru(s, gs, sh, gate):
        c = s.c
        D, DC, T = c.D, c.DC, c.T
        CH = c.LB // 128
        (w_in, r_in), (w_out, r_out) = s.W["lru_w_in"], s.W["lru_w_out"]
        (w_a, rw_a), (w_x, rw_x) = s.W["lru_w_a"], s.W["lru_w_x"]
        s.load_small(s.cvw[:, 0:4 * DC], s.lrw_in, s.cv_r)
        s.load_small(s.cvv[:, 0:4 * DC], s.lrv_in, s.cv_r)
        vec, vr = s.vec, [s.vec_r]
        emit(s.act, lambda e: e.activation(out=vec[:, 0:DC], in_=s.cvv[:, 3 * DC:4 * DC], func=AF.Exp, scale=-1.0), [s.cv_r], vr)
        emit(s.dve, lambda e: e.tensor_scalar(out=vec[:, 0:DC], in0=vec[:, 0:DC], scalar1=1.0, scalar2=None, op0=ALU.add), vr, vr)
        emit(s.act, lambda e: e.activation(out=vec[:, 0:DC], in_=vec[:, 0:DC], func=AF.Ln), vr, vr)
        emit(s.dve, lambda e: e.tensor_scalar(out=vec[:, DC:2 * DC], in0=vec[:, 0:DC], scalar1=-16.0, scalar2=None, op0=ALU.mult), vr, vr)
        emit(s.dve, lambda e: e.tensor_scalar(out=vec[:, 0:DC], in0=vec[:, 0:DC], scalar1=-8.0, scalar2=None, op0=ALU.mult), vr, vr)
        s.chk(1, "lru")
        s.norm(gs, sh)

        def epi1(dc, ps, pr, w):
            emit(s.act, lambda e: e.activation(out=s.midc(dc), in_=ps[:, 0:T], func=AF.Identity), pr, [s.mid_r[dc]])

        s.linear(w_in, r_in, 0, DC, [(D + dc * 128, 128) for dc in range(DC)], s.hx, s.hT_r, epi1, T)
        s.chk(2, "lru")
        s.halo_exchange("lr")
        for dc in range(DC):
            tq, tr = s.tmp[dc % 2], [s.tmp_r[dc % 2]]
            s.dwconv(dc, s.cvw, 4, tq, tr, bias=s.cvv[:, 0:DC])
            emit(s.act, lambda e: e.activation(out=s.midc(dc), in_=tq, func=AF.Identity), tr, [s.mid_r[dc]])

        t_r, t_a2, t_a, t_u, t_g = s.xt[0], s.xt[1], s.tmp[0], s.tmp[1], s.st[0]
        r_r, r_a2, r_a, r_u, r_g = [s.xt_r[0]], [s.xt_r[1]], [s.tmp_r[0]], [s.tmp_r[1]], [s.st_r[0]]
        ob = s.st[1].bitcast(BF16)
        ob_r = [s.st_r[1]]
        assert CH <= 2

        def lru_pass(final_only):
            for hh in range(c.LH):
                for jc in range(CH):
                    j = hh * CH + jc
                    xfn = lambda kc, n0, n1, hh=hh: s.mid[:, hh * CH + kc, HALO + n0:HALO + n1]
                    xres = s.mid_r[hh * CH:(hh + 1) * CH]

                    def epi_r(_, ps, pr, w):
                        emit(s.act, lambda e: e.activation(out=t_r, in_=ps[:, 0:T], func=AF.Sigmoid, bias=s.cvv[:, DC + j:DC + j + 1], scale=1.0),
                             pr + [s.cv_r], r_r)
                        emit(s.act, lambda e: e.activation(out=t_a, in_=t_r, func=AF.Exp, scale=vec[:, j:j + 1]), r_r + vr, r_a)
                        emit(s.act, lambda e: e.activation(out=t_a2, in_=t_r, func=AF.Exp, scale=vec[:, DC + j:DC + j + 1]), r_r + vr, r_a2)
                        emit(s.dve, lambda e: e.tensor_scalar(out=t_a2, in0=t_a2, scalar1=-1.0, scalar2=1.0, op0=ALU.mult, op1=ALU.add), r_a2, r_a2)
                        emit(s.act, lambda e: e.activation(out=t_a2, in_=t_a2, func=AF.Sqrt), r_a2, r_a2)

                    def epi_i(_, ps, pr, w):
                        emit(s.act, lambda e: e.activation(out=t_u, in_=ps[:, 0:T], func=AF.Sigmoid, bias=s.cvv[:, 2 * DC + j:2 * DC + j + 1], scale=1.0),
                             pr + [s.cv_r], r_u)
                        emit(s.dve, lambda e: e.tensor_tensor(out=t_u, in0=t_u, in1=s.midc(j), op=ALU.mult), r_u + [s.mid_r[j]], r_u)
                        emit(s.dve, lambda e: e.tensor_tensor(out=t_u, in0=t_u, in1=t_a2, op=ALU.mult), r_u + r_a2, r_u)
                        init = 0.0 if final_only else vec[:, 3 * DC + j:3 * DC + j + 1]
                        emit(s.dve, lambda e: e.tensor_tensor_scan(out=t_r, data0=t_a, data1=t_u, initial=init, op0=ALU.mult, op1=ALU.add),
                             r_a + r_u + vr + r_r, r_r)
                        if final_only:
                            emit(s.dve, lambda e: e.tensor_copy(out=vec[:, 2 * DC + j:2 * DC + j + 1], in_=t_r[:, T - 1:T]), r_r + vr, vr)

                    s.linear(w_a, rw_a, hh * c.LB, CH, [(jc * 128, 128)], xfn, xres, epi_r, T)
                    s.linear(w_x, rw_x, hh * c.LB, CH, [(jc * 128, 128)], xfn, xres, epi_i, T)
                    if not final_only:
                        def epi_g(_, ps, pr, w):
                            emit(s.act, lambda e: e.activation(out=t_g, in_=ps[:, 0:T], func=AF.Square), pr, r_g)
                            emit(s.dve, lambda e: e.tensor_scalar(out=t_g, in0=t_g, scalar1=0.044715, scalar2=1.0, op0=ALU.mult, op1=ALU.add), r_g, r_g)
                            emit(s.dve, lambda e: e.tensor_tensor(out=t_g, in0=t_g, in1=ps[:, 0:T], op=ALU.mult), r_g + pr, r_g)
                            emit(s.act, lambda e: e.activation(out=t_g, in_=t_g, func=AF.Sigmoid, scale=1.5957691216057308), r_g, r_g)
                            emit(s.dve, lambda e: e.tensor_tensor(out=t_g, in0=t_g, in1=ps[:, 0:T], op=ALU.mult), r_g + pr, r_g)
                            emit(s.dve, lambda e: e.tensor_tensor(out=ob[:, jc * T:(jc + 1) * T], in0=t_g, in1=t_r, op=ALU.mult), r_g + r_r, ob_r)
                        s.linear(w_in, r_in, 0, DC, [(j * 128, 128)], s.hx, s.hT_r, epi_g, T)
                if not final_only:
                    for jc in range(CH):
                        j = hh * CH + jc
                        emit(s.act, lambda e, j=j, jc=jc: e.activation(out=s.midc(j), in_=ob[:, jc * T:(jc + 1) * T], func=AF.Identity), ob_r, [s.mid_r[j]])

        s.chk(3, "lru")
        lru_pass(True)
        s.chk(4, "lru")
        loc = s.dram("lru_l", [128, DC], F32)
        pair = s.dram("lru_p", [256, DC], F32)
        rl, rp = Res("ll"), Res("lp")
        emit(s.pq, lambda e: e.dma_start(out=loc, in_=vec[:, 2 * DC:3 * DC]), vr, [rl])
        s.pair_exchange(loc, rl, pair, rp)
        if s.only is None:
            s.cast_upto("moe_w2_1")
        emit(s.sq, lambda e: e.dma_start(out=vec[:, 3 * DC:4 * DC], in_=pair[0:128, :]), [rp], vr)
        emit(s.dve, lambda e: e.tensor_scalar(out=vec[:, 3 * DC:4 * DC], in0=vec[:, 3 * DC:4 * DC], scalar1=s.flag[:, 0:1], scalar2=None, op0=ALU.mult),
             vr + [s.const_r], vr)
        s.chk(5, "lru")
        lru_pass(False)
        s.chk(6, "lru")
        pre, epi3 = s.resid(gate)
        s.linear(w_out, r_out, 0, DC, [(d * 128, 128) for d in range(DC)], s.midx, s.mid_r, epi3, T, pre=pre)

    def attention(s, gs, sh, gate):
        c = s.c
        D, DC, T, S, TB = c.D, c.DC, c.T, c.S, c.TB
        QC, KVC, NH, IH = c.QL // 128, c.KVL // 128, c.NH, c.IH
        SB = S // 128
        W = s.W
        (w_in, r_in), (w_qidx, r_qidx), (w_uq, r_uq) = W["att_w_in"], W["att_w_qidx"], W["att_w_uq"]
        (w_ukT, r_ukT), (w_uv, r_uv), (w_out, r_out) = W["att_w_ukT"], W["att_w_uv"], W["att_w_out"]
        s.load_small(s.cvv[:, 0:QC], s.gcq_in, s.cv_r)
        s.load_small(s.cvv[:, QC:QC + KVC], s.gckv_in, s.cv_r)

        if s.full:
            midflat = s.mid[:, :, :].rearrange("p a b -> p (a b)")
            ohs = midflat[:, 0:2 * 32 * 128]
            ohs_r = s.mid_r
            rbb = s.st[0]
            rbb_r = [s.st_r[0]]
        else:
            ohs = s.sb("ohs_sb", [128, 2 * 32 * 128], BF16)[:, :]
            ohs_r = [Res("ohs")]
            rbb = s.sb("rbb_sb", [128, 32 * NH], F32)[:, :]
            rbb_r = [Res("rbb")]
        emit(s.pq, lambda e: e.dma_start(out=ohs, in_=s.ohs_in), [], ohs_r)
        emit(s.sq, lambda e: e.dma_start(out=rbb[:, 0:32 * NH], in_=s.rbb_in), [], rbb_r)
        assert NH <= 32
        negb = s.gb[:, 0:NH]
        nb_r = [s.gb_r]
        emit(s.dve, lambda e: e.tensor_scalar(out=negb, in0=rbb[:, 31 * NH:32 * NH], scalar1=-1.0, scalar2=None, op0=ALU.mult), rbb_r, nb_r)
        eacc = s.tmp[0][:, 0:128]
        ea_r = [s.tmp_r[0]]
        for h in range(NH):
            for dl in range(2):
                for b in range(32):
                    src = ohs[:, (dl * 32 + b) * 128:(dl * 32 + b + 1) * 128]
                    sc = rbb[:, b * NH + h:b * NH + h + 1]
                    if b == 0:
                        emit(s.dve, lambda e, src=src, sc=sc: e.tensor_scalar(out=eacc, in0=src, scalar1=sc, scalar2=None, op0=ALU.mult),
                             ohs_r + rbb_r, ea_r)
                    else:
                        emit(s.dve, lambda e, src=src, sc=sc: e.scalar_tensor_tensor(out=eacc, in0=src, scalar=sc, in1=eacc, op0=ALU.mult, op1=ALU.add),
                             ohs_r + rbb_r + ea_r, ea_r)
                o = (h * 2 + dl) * 128
                emit(s.act, lambda e, o=o, h=h: e.activation(out=s.EB[:, o:o + 128], in_=eacc, func=AF.Exp, bias=negb[:, h:h + 1], scale=1.0),
                     ea_r + nb_r, [s.EB_r])

        s.chk(1)
        s.norm(gs, sh)
        s.chk(2)
        accq, aq_r = s.st[0], [s.st_r[0]]
        acck, ak_r = s.st[1], [s.st_r[1]]

        def epi_in(j, ps, pr, w):
            if j < QC + KVC:
                acc, ar = (accq, aq_r) if j < QC else (acck, ak_r)
                first = (j == 0) or (j == QC)
                if first:
                    emit(s.act, lambda e: e.activation(out=acc, in_=ps[:, 0:T], func=AF.Square), pr, ar)
                else:
                    tq, tr = s.tmp[j % 2], [s.tmp_r[j % 2]]
                    emit(s.act, lambda e: e.activation(out=tq, in_=ps[:, 0:T], func=AF.Square), pr, tr)
                    emit(s.dve, lambda e: e.tensor_tensor(out=acc, in0=acc, in1=tq, op=ALU.add), tr + ar, ar)
            emit(s.act, lambda e: e.activation(out=s.midc(j), in_=ps[:, 0:T], func=AF.Identity), pr, [s.mid_r[j]])

        s.linear(w_in, r_in, 0, DC, [(j * 128, 128) for j in range(QC + KVC + 1)], s.hx, s.hT_r, epi_in, T)

        s.chk(3)
        if s.full:
            hflat = s.hT[:, :, :].rearrange("p a b -> p (a b)")
        else:
            hflat = s.sb("att_scratch", [128, QC * T + KVC * S + SB * c.KVL + S + 16 * 128 + TB * IH * 2 + KVC * 256 + 128 + 256 + 64 + 4 * KVC * 128], BF16)[:, :]
        off = [0]

        def carve(n):
            a = hflat[:, off[0]:off[0] + n]
            off[0] += n
            return a

        assert TB * IH <= 31 * DC
        wtok = s.cvw[:, 0:TB * IH]
        wtok_r = [s.cv_r]
        sl = s.wcount % len(s.wt)
        s.wcount += 1
        wt, wr = s.wt[sl], s.wt_r[sl]
        wcol = c.QL + c.KVL + 128
        emit(s.sq, lambda e: e.dma_start(out=wt[:, 0:DC, 0:IH], in_=w_in[wcol:wcol + 128, :].rearrange("p (kc f) -> p kc f", f=128)[:, :, 0:IH]), list(r_in[1]), [wr])
        for tb in range(TB):
            ps, pr = s.bank[7], [s.bank_r[7]]
            for kc in range(DC):
                emit(s.pe, lambda e, tb=tb, kc=kc: e.matmul(ps[:, 0:IH], lhsT=s.hT[:, kc, tb * 128:(tb + 1) * 128], rhs=wt[:, kc, 0:IH],
                                                            start=(kc == 0), stop=(kc == DC - 1)), [wr, s.hT_r[kc]], pr)
            emit(s.dve, lambda e, tb=tb: e.tensor_copy(out=wtok[:, tb * IH:(tb + 1) * IH], in_=ps[:, 0:IH]), pr, wtok_r)

        cqn = carve(QC * T)
        ckv = carve(KVC * S)
        kvt = carve(SB * c.KVL)
        kidx = carve(S)
        GI = min(16, IH)
        qig = carve(GI * 128)
        qh = carve(128)
        qlat = carve(KVC * 128)
        olat = carve(KVC * 128)
        wukb = [carve(KVC * 128).rearrange("p (cc f) -> p cc f", f=128) for _ in range(2)]
        wuvb = [carve(KVC * 128).rearrange("p (cc f) -> p cc f", f=128) for _ in range(2)]
        wukb_r = [[Res("wuk0")], [Res("wuk1")]]
        wuvb_r = [[Res("wuv0")], [Res("wuv1")]]
        views = {n: [Res(n)] for n in ("cqn", "ckv", "kvt", "kidx", "qig", "qh", "qlat", "olat")}
        if s.full:
            base = []
            for r in s.hT_r:
                base += r.r + ([r.w] if r.w is not None else [])
            for v in views.values():
                v[0].r = list(base)
        rinv = carve(256).bitcast(F32)
        rinv_r = [Res("rinv")]
        views["rinv"] = rinv_r
        for i_ in range(2):
            views[f"wuk{i_}"] = wukb_r[i_]
            views[f"wuv{i_}"] = wuvb_r[i_]
        if s.full:
            rinv_r[0].r = list(base)
            for i_ in range(2):
                wukb_r[i_][0].r = list(base)
                wuvb_r[i_][0].r = list(base)

        rq, rq_r = s.xt[0], [s.xt_r[0]]
        rk, rk_r = s.xt[1], [s.xt_r[1]]
        s.colsum_bc(accq, aq_r, rq, rq_r, 1.0 / c.QL, 1e-6, -0.5)
        s.colsum_bc(acck, ak_r, rk, rk_r, 1.0 / c.KVL, 1e-6, -0.5)
        for j in range(QC + KVC):
            tq, tr = s.tmp[j % 2], [s.tmp_r[j % 2]]
            rs, rs_r = (rq, rq_r) if j < QC else (rk, rk_r)
            emit(s.dve, lambda e: e.tensor_tensor(out=tq, in0=s.midc(j), in1=rs, op=ALU.mult), [s.mid_r[j]] + rs_r, tr)
            if j < QC:
                dst, dr = cqn[:, j * T:(j + 1) * T], views["cqn"]
            else:
                cc = j - QC
                dst, dr = ckv[:, cc * S + T:cc * S + 2 * T], views["ckv"]
            emit(s.act, lambda e, dst=dst: e.activation(out=dst, in_=tq, func=AF.Identity, scale=s.cvv[:, j:j + 1]), tr + [s.cv_r], dr)
        emit(s.dve, lambda e: e.tensor_copy(out=kidx[:, T:2 * T], in_=s.midc(QC + KVC)), [s.mid_r[QC + KVC]], views["kidx"])

        s.chk(4)
        NR = (KVC + 1) * 128
        loc = s.dram("kv_l", [NR, T], BF16)
        pair = s.dram("kv_p", [2 * NR, T], BF16)
        rl, rp = Res("kvl"), Res("kvp")
        ckv3 = ckv.rearrange("p (a b) -> p a b", b=S)
        emit(s.pq, lambda e: e.dma_start(out=loc[0:KVC * 128, :].rearrange("(cc p) t -> p cc t", p=128), in_=ckv3[:, :, T:2 * T]), views["ckv"], [rl])
        emit(s.pq, lambda e: e.dma_start(out=loc[KVC * 128:NR, :], in_=kidx[:, T:2 * T]), views["kidx"], [rl])
        s.pair_exchange(loc, rl, pair, rp)
        if s.only is None:
            s.cast_upto("sconv_w_out")
        emit(s.sq, lambda e: e.dma_start(out=ckv3[:, :, 0:T], in_=pair[0:KVC * 128, :].rearrange("(cc p) t -> p cc t", p=128)), [rp], views["ckv"])
        emit(s.sq, lambda e: e.dma_start(out=kidx[:, 0:T], in_=pair[KVC * 128:NR, :]), [rp], views["kidx"])

        s.chk(5)
        for sb_ in range(SB):
            ps, pr = s.bank[6 + sb_ % 2], [s.bank_r[6 + sb_ % 2]]
            for cc in range(KVC):
                emit(s.pe, lambda e, ps=ps, cc=cc, sb_=sb_: e.matmul(ps[:, cc * 128:(cc + 1) * 128], lhsT=ckv[:, cc * S + sb_ * 128:cc * S + (sb_ + 1) * 128],
                                                                  rhs=s.identb[:, :], start=True, stop=True), views["ckv"] + [s.const_r], pr)
            emit(s.act, lambda e, ps=ps, sb_=sb_: e.activation(out=kvt[:, sb_ * c.KVL:(sb_ + 1) * c.KVL], in_=ps[:, 0:c.KVL], func=AF.Identity), pr, views["kvt"])

        s.chk(6)
        big = s.psum[:, 0:2048]
        big_r = s.bank_r[0:4]
        Sc = s.scr[:, 0:S]
        Sc_r = [s.xt_r[0], s.xt_r[1]]
        Sw = s.scr[:, 2 * T:2 * T + S]
        Sw_r = [s.tmp_r[0], s.tmp_r[1]]
        Mb = s.scr[:, 2 * T:3 * T].bitcast(BF16)
        PTs = [s.st[0].bitcast(BF16), s.px.bitcast(BF16)]
        PT_rs = [[s.st_r[0]], [s.px_r]]
        MT = s.st[1].bitcast(BF16)
        MT_r = [s.st_r[1]]
        m8, thr = s.sm[0], s.sm[1]
        sm_r = [s.sm_r]
        scale = 128 ** -0.5

        for qb in range(TB):
            NKC = TB + qb + 1
            NK = NKC * 128
            t0 = qb * 128
            xq = lambda kc, n0, n1: cqn[:, kc * T + t0 + n0:kc * T + t0 + n1]
            first = True
            for g0 in range(0, IH, GI):
                def epi_qi(jl, ps, pr, w):
                    emit(s.act, lambda e: e.activation(out=qig[:, jl * 128:(jl + 1) * 128], in_=ps[:, 0:128], func=AF.Identity), pr, views["qig"])
                s.linear(w_qidx, r_qidx, 0, QC, [((g0 + jl) * 128, 128) for jl in range(GI)], xq, views["cqn"], epi_qi, 128)
                for jl in range(GI):
                    hh = g0 + jl
                    for n0 in range(0, NK, 512):
                        n1 = min(NK, n0 + 512)
                        emit(s.pe, lambda e, jl=jl, n0=n0, n1=n1: e.matmul(big[:, n0:n1], lhsT=qig[:, jl * 128:(jl + 1) * 128], rhs=kidx[:, n0:n1],
                                                                          start=True, stop=True), views["qig"] + views["kidx"], big_r)
                    emit(s.act, lambda e: e.activation(out=Sw[:, 0:NK], in_=big[:, 0:NK], func=AF.Relu), big_r, Sw_r)
                    wsc = wtok[:, qb * IH + hh:qb * IH + hh + 1]
                    if first:
                        emit(s.dve, lambda e, wsc=wsc: e.tensor_scalar(out=Sc[:, 0:NK], in0=Sw[:, 0:NK], scalar1=wsc, scalar2=None, op0=ALU.mult),
                             Sw_r + wtok_r, Sc_r)
                        first = False
                    else:
                        emit(s.dve, lambda e, wsc=wsc: e.scalar_tensor_tensor(out=Sc[:, 0:NK], in0=Sw[:, 0:NK], scalar=wsc, in1=Sc[:, 0:NK],
                                                                             op0=ALU.mult, op1=ALU.add), Sw_r + wtok_r + Sc_r, Sc_r)
            s.chk(7)
            emit(s.dve, lambda e: e.tensor_scalar(out=Sc[:, 0:T], in0=Sc[:, 0:T], scalar1=s.flag[:, 1:2], scalar2=None, op0=ALU.add), Sc_r + [s.const_r], Sc_r)
            emit(s.dve, lambda e: e.tensor_tensor(out=Sc[:, NK - 128:NK], in0=Sc[:, NK - 128:NK], in1=s.tri[:, :], op=ALU.add), Sc_r + [s.const_r], Sc_r)
            rounds = c.KSEL // 8
            for r in range(rounds):
                srcv = Sc if r == 0 else Sw
                src_r = Sc_r if r == 0 else Sw_r
                emit(s.dve, lambda e, srcv=srcv: e.max(out=m8[:, 0:8], in_=srcv[:, 0:NK]), src_r + sm_r, sm_r)
                if r < rounds - 1:
                    emit(s.dve, lambda e, srcv=srcv: e.match_replace(out=Sw[:, 0:NK], in_to_replace=m8[:, 0:8], in_values=srcv[:, 0:NK], imm_value=NEG),
                         src_r + sm_r + Sw_r, Sw_r)
            emit(s.dve, lambda e: e.tensor_scalar(out=thr[:, 0:1], in0=m8[:, 7:8], scalar1=-1.0e29, scalar2=None, op0=ALU.max), sm_r, sm_r)
            emit(s.dve, lambda e: e.tensor_scalar(out=Mb[:, 0:NK], in0=Sc[:, 0:NK], scalar1=thr[:, 0:1], scalar2=None, op0=ALU.is_ge), Sc_r + sm_r + Sw_r, Sw_r)
            s.chk(8)
            for j0 in range(0, NKC, 4):
                j1 = min(NKC, j0 + 4)
                ps, pr = s.bank[7], [s.bank_r[7]]
                for j in range(j0, j1):
                    emit(s.pe, lambda e, j=j, j0=j0: e.matmul(ps[:, (j - j0) * 128:(j - j0 + 1) * 128], lhsT=Mb[:, j * 128:(j + 1) * 128], rhs=s.identb[:, :],
                                                             start=True, stop=True), Sw_r + [s.const_r], pr)
                emit(s.act, lambda e, j0=j0, j1=j1: e.activation(out=MT[:, j0 * 128:j1 * 128], in_=ps[:, 0:(j1 - j0) * 128], func=AF.Identity), pr, MT_r)
            s.chk(9)
            for h in range(NH):
                PT, PT_r = PTs[h % 2], PT_rs[h % 2]

                def epi_q(_, ps, pr, w):
                    emit(s.act, lambda e: e.activation(out=qh, in_=ps[:, 0:128], func=AF.Identity), pr, views["qh"])
                s.linear(w_uq, r_uq, 0, QC, [(h * 128, 128)], xq, views["cqn"], epi_q, 128)
                wuk, wuk_r = wukb[h % 2], wukb_r[h % 2]
                emit(s.sq, lambda e, wuk=wuk: e.dma_start(out=wuk, in_=w_ukT[:, h * 128:(h + 1) * 128].rearrange("(cc p) f -> p cc f", p=128)),
                     list(r_ukT[1]), wuk_r)
                wuv, wuv_r = wuvb[h % 2], wuvb_r[h % 2]
                emit(s.sq, lambda e, wuv=wuv: e.dma_start(out=wuv, in_=w_uv[h * 128:(h + 1) * 128, :].rearrange("p (kc f) -> p kc f", f=128)),
                     list(r_uv[1]), wuv_r)
                ps6, pr6 = s.bank[6], [s.bank_r[6]]
                ps7, pr7 = s.bank[7], [s.bank_r[7]]
                for cc in range(KVC):
                    emit(s.pe, lambda e, cc=cc, wuk=wuk: e.matmul(ps6[:, cc * 128:(cc + 1) * 128], lhsT=wuk[:, cc, :], rhs=qh, start=True, stop=True),
                         wuk_r + views["qh"], pr6)
                emit(s.dve, lambda e: e.tensor_copy(out=qlat, in_=ps6[:, 0:KVC * 128]), pr6, views["qlat"])
                for j in range(NKC):
                    for cc in range(KVC):
                        emit(s.pe, lambda e, j=j, cc=cc: e.matmul(big[:, j * 128:(j + 1) * 128], lhsT=ckv[:, cc * S + j * 128:cc * S + (j + 1) * 128],
                                                                 rhs=qlat[:, cc * 128:(cc + 1) * 128], start=(cc == 0), stop=(cc == KVC - 1)),
                             views["ckv"] + views["qlat"], big_r)
                emit(s.act, lambda e, PT=PT: e.activation(out=PT[:, 0:NK], in_=big[:, 0:NK], func=AF.Exp, scale=scale), big_r, PT_r)
                emit(s.dve, lambda e, PT=PT: e.tensor_tensor(out=PT[:, 0:NK], in0=PT[:, 0:NK], in1=MT[:, 0:NK], op=ALU.mult), PT_r + MT_r, PT_r)
                emit(s.dve, lambda e, PT=PT, h=h: e.tensor_tensor(out=PT[:, NK - 256:NK], in0=PT[:, NK - 256:NK], in1=s.EB[:, h * 256:(h + 1) * 256], op=ALU.mult),
                     PT_r + [s.EB_r], PT_r)
                for j in range(NKC):
                    emit(s.pe, lambda e, j=j, PT=PT: e.matmul(ps7[:, 0:128], lhsT=s.onesb[:, :], rhs=PT[:, j * 128:(j + 1) * 128], start=(j == 0), stop=(j == NKC - 1)),
                         PT_r + [s.const_r], pr7)
                emit(s.dve, lambda e: e.reciprocal(out=rinv, in_=ps7[:, 0:128]), pr7, rinv_r)
                for cc in range(KVC):
                    for j in range(NKC):
                        emit(s.pe, lambda e, j=j, cc=cc, PT=PT: e.matmul(ps6[:, cc * 128:(cc + 1) * 128], lhsT=kvt[:, j * c.KVL + cc * 128:j * c.KVL + (cc + 1) * 128],
                                                                        rhs=PT[:, j * 128:(j + 1) * 128], start=(j == 0), stop=(j == NKC - 1)),
                             PT_r + views["kvt"], pr6)
                emit(s.act, lambda e: e.activation(out=olat, in_=ps6[:, 0:KVC * 128], func=AF.Identity), pr6, views["olat"])
                for cc in range(KVC):
                    emit(s.pe, lambda e, cc=cc, wuv=wuv: e.matmul(ps7[:, 128:256], lhsT=wuv[:, cc, :], rhs=olat[:, cc * 128:(cc + 1) * 128],
                                                                 start=(cc == 0), stop=(cc == KVC - 1)), wuv_r + views["olat"], pr7)
                emit(s.dve, lambda e, h=h: e.tensor_tensor(out=s.mid[:, h, HALO + t0:HALO + t0 + 128], in0=ps7[:, 128:256], in1=rinv, op=ALU.mult),
                     pr7 + rinv_r, [s.mid_r[h]])
        s.chk(10)
        if s.full:
            allt = []
            for v in views.values():
                allt += v[0].r + ([v[0].w] if v[0].w is not None else [])
            for r in s.hT_r:
                r.r = r.r + allt
        pre, epi3 = s.resid(gate)
        s.linear(w_out, r_out, 0, c.AW // 128, [(d * 128, 128) for d in range(DC)], s.midx, s.mid_r[0:NH], epi3, T, pre=pre)


def _rel_bucket(dist):
    import math
    dist = np.maximum(dist, 0)
    max_exact = 16
    large = max_exact + (np.log(np.maximum(dist, 1).astype(np.float32) / max_exact) / math.log(128 / max_exact)
                         * (32 - max_exact)).astype(np.int32)
    large = np.minimum(large, 31)
    return np.where(dist < max_exact, dist, large)


def _pvec(v, DC):
    v = np.asarray(v, np.float32)
    lead = int(np.prod(v.shape[:-1])) if v.ndim > 1 else 1
    return np.ascontiguousarray(v.reshape(lead, DC, 128).transpose(2, 0, 1).reshape(128, lead * DC))


def make_in_maps(cfg, inp):
    c = cfg
    D, DC, T = c.D, c.DC, c.T
    f = lambda a: np.asarray(a, np.float32)
    shared = {}
    shared["cT"] = np.ascontiguousarray(f(inp["c"]).reshape(4, DC, 128).transpose(2, 1, 0).reshape(128, DC * 4))
    shared["ada_t"] = _pvec(f(inp["ada_table"]), DC)
    shared["norm_mix"] = _pvec(f(inp["norm_mix"]), DC)
    shared["norm_ffn"] = _pvec(f(inp["norm_ffn"]), DC)
    shared["norm_final"] = _pvec(f(inp["norm_final"]), DC)
    shared["ident"] = np.eye(128, dtype=np.float32)
    ss, tt = np.meshgrid(np.arange(128), np.arange(128), indexing="ij")
    shared["tri"] = np.where(ss.T >= tt.T, 0.0, 0.0).astype(np.float32)
    tq, sk = np.meshgrid(np.arange(128), np.arange(128), indexing="ij")
    shared["tri"] = np.where(sk <= tq, 0.0, NEG).astype(np.float32)
    ohs = np.zeros((128, 2, 32, 128), np.float32)
    s_i, t_i = np.meshgrid(np.arange(128), np.arange(128), indexing="ij")
    for dl in range(2):
        delta = 1 - dl
        bk = _rel_bucket(delta * 128 + t_i - s_i)
        for b in range(32):
            ohs[:, dl, b, :] = (bk == b)
    shared["ohs"] = ohs.reshape(128, -1)
    shared["rbb"] = np.ascontiguousarray(np.broadcast_to(f(inp["rel_bias"]).reshape(1, 32 * c.NH), (128, 32 * c.NH)))
    shared["g_cq"] = _pvec(f(inp["att_g_cq"])[0], c.QL // 128)
    shared["g_ckv"] = _pvec(f(inp["att_g_ckv"])[0], c.KVL // 128)
    shared["sconv_cw"] = _pvec(f(inp["sconv_conv_w"])[0], DC)
    shared["conf_cw"] = _pvec(f(inp["conf_conv_w"])[0], DC)
    shared["conf_vec"] = _pvec(np.stack([f(inp["conf_conv_b"])[0], f(inp["conf_ln_g"])[0], f(inp["conf_ln_b"])[0]]), DC)
    shared["lru_cw"] = _pvec(f(inp["lru_conv_w"])[0], DC)
    shared["lru_vec"] = _pvec(np.stack([f(inp["lru_conv_b"])[0], f(inp["lru_b_a"])[0], f(inp["lru_b_x"])[0], f(inp["lru_lambda"])[0]]), DC)
    rt = f(inp["moe_router"])
    shared["router"] = np.ascontiguousarray(rt.reshape(rt.shape[0], DC, 128, 8).transpose(2, 0, 1, 3).reshape(128, rt.shape[0] * DC * 8))
    if shared["router"].shape[1] < 2 * DC * 8:
        shared["router"] = np.concatenate([shared["router"], np.zeros((128, 2 * DC * 8 - shared["router"].shape[1]), np.float32)], 1)

    wfull = {
        "att_w_in": f(inp["att_w_in"])[0],
        "att_w_qidx": f(inp["att_w_qidx"])[0],
        "att_w_uq": f(inp["att_w_uq"])[0],
        "att_w_ukT": np.ascontiguousarray(f(inp["att_w_uk"])[0].transpose(1, 2, 0)).reshape(c.AW, c.KVL),
        "att_w_uv": f(inp["att_w_uv"])[0].reshape(c.KVL, c.AW),
        "att_w_out": f(inp["att_w_out"])[0],
        "sconv_w_in": f(inp["sconv_w_in"])[0], "sconv_w_out": f(inp["sconv_w_out"])[0],
        "conf_w_in": f(inp["conf_w_in"])[0], "conf_w_out": f(inp["conf_w_out"])[0],
        "lru_w_in": f(inp["lru_w_in"])[0], "lru_w_out": f(inp["lru_w_out"])[0],
        "lru_w_a": f(inp["lru_w_a"])[0].reshape(D, c.LB), "lru_w_x": f(inp["lru_w_x"])[0].reshape(D, c.LB),
    }
    for l in range(2):
        wfull[f"ffn_w13_{l}"] = f(inp["ffn_w13"])[l]
        wfull[f"ffn_w2_{l}"] = f(inp["ffn_w2"])[l]
        wfull[f"moe_w13_{l}"] = np.ascontiguousarray(f(inp["moe_w13"])[l].transpose(1, 0, 2)).reshape(D, c.NE * 2 * c.DFE)
        wfull[f"moe_w2_{l}"] = np.ascontiguousarray(f(inp["moe_w2"])[l].transpose(1, 0, 2)).reshape(c.DFE, c.NE * D)
    for name in list(wfull.keys()):
        w = wfull[name]
        K, Fd = w.shape
        Fp = ((Fd + 127) // 128) * 128
        if Fp != Fd:
            w = np.concatenate([w, np.zeros((K, Fp - Fd), np.float32)], axis=1)
        wfull[name] = np.ascontiguousarray(w.reshape(K // 128, 128, Fp // 128, 128).transpose(2, 1, 0, 3)).reshape(Fp, K)
    ada_w = f(inp["ada_w"])
    ada_b = f(inp["ada_b"])
    x = f(inp["x"])
    NCOL = 6 * D // 8
    NCL = 6 * DC // 8
    maps = []
    for core in range(8):
        b, half = core // 2, core % 2
        m = dict(shared)
        m["xT"] = np.ascontiguousarray(x[b, half * T:(half + 1) * T, :].T)
        m["ada_w"] = np.ascontiguousarray(ada_w[:, core * NCOL:(core + 1) * NCOL])
        m["ada_b"] = np.ascontiguousarray(ada_b[core * NCOL:(core + 1) * NCOL].reshape(NCL, 128).T)
        bs = np.zeros((128, 4), np.float32)
        bs[:, b] = 1.0
        m["bsel"] = bs
        fl = np.zeros((128, 2), np.float32)
        fl[:, 0] = float(half)
        fl[:, 1] = 0.0 if half == 1 else NEG
        m["flag"] = fl
        for name, w in wfull.items():
            m[name] = w
        maps.append(m)
    return maps


_CACHE = {}


def run(cfg, inp, stop_after=None, only=None):
    key = (cfg.D, cfg.SEQ, stop_after, tuple(sorted(only)) if only else None)
    if key not in _CACHE:
        bld = B(cfg, stop_after, only)
        _CACHE[key] = (bld.build(), bld)
    nc = _CACHE[key][0]
    maps = make_in_maps(cfg, inp)
    res = run_bass_kernel_spmd(nc, maps, core_ids=list(range(8)))
    out = np.zeros((4, cfg.SEQ, cfg.D), np.float32)
    global LAST
    LAST = (res, _CACHE.get(key))
    for core in range(8):
        b, half = core // 2, core % 2
        out[b, half * cfg.T:(half + 1) * cfg.T, :] = np.asarray(res.results[core]["outT"]).T
    return out


def kernel(**inputs):
    return run(Cfg(), inputs)
```
